# Optimizing a Trainium2 kernel written in Bass

```python
import math
import jax, jax.numpy as jnp
from jax import lax
import numpy as np

D_MODEL = 1024
BATCH = 16
SEQ = 4096
DEPTH = 1

MEM_LEN = 256
HEAD_DIM = 64
DIFF_HEADS = 4
DIFF_QK = HEAD_DIM
DIFF_V = 2 * HEAD_DIM
FOX_HEADS = 8
FOX_DIM = HEAD_DIM
MEM_HEADS = 4
MEM_DIM = 128
N_BRANCHES = 3
ROPE_THETA = 500000.0
ROPE_DIM = HEAD_DIM // 4
Q_BLOCK = 128
N_GROUPS = 4
EXPERTS_PER_GROUP = 8
N_EXPERTS = N_GROUPS * EXPERTS_PER_GROUP
EXPERT_TOP_K = 2
EXPERT_FF = 512
MOE_BLOCK = 128
EPS = 1e-6
NEG_INF = -1e30
IN_SPLITS = (DIFF_HEADS * 2 * DIFF_QK, DIFF_HEADS * 2 * DIFF_QK, DIFF_HEADS * DIFF_V,
             FOX_HEADS * FOX_DIM, FOX_HEADS * FOX_DIM, FOX_HEADS * FOX_DIM, FOX_HEADS,
             MEM_HEADS * MEM_DIM, N_BRANCHES * D_MODEL)
IN_COLS = sum(IN_SPLITS)

kernel_name = "hybrid_diff_fox_mem_hiermoe_layer"


def _rms(x, g):
    xf = x.astype(jnp.float32)
    y = xf * lax.rsqrt(jnp.mean(xf * xf, axis=-1, keepdims=True) + EPS)
    return (y * g.astype(jnp.float32)).astype(x.dtype)


def _rope_tables(positions):
    inv = ROPE_THETA ** (-jnp.arange(0, ROPE_DIM, 2, dtype=jnp.float32) / ROPE_DIM)
    ang = positions.astype(jnp.float32)[..., None] * inv
    return jnp.cos(ang), jnp.sin(ang)


def _partial_rope(t, cos, sin):
    shape = cos.shape[:2] + (1,) * (t.ndim - 3) + cos.shape[-1:]
    c = cos.reshape(shape).astype(t.dtype)
    s = sin.reshape(shape).astype(t.dtype)
    half = ROPE_DIM // 2
    t1, t2, rest = t[..., :half], t[..., half:ROPE_DIM], t[..., ROPE_DIM:]
    return jnp.concatenate([t1 * c - t2 * s, t2 * c + t1 * s, rest], axis=-1)


def _causal_mask(s0, s1):
    return jnp.arange(s1)[None, :] <= jnp.arange(s0, s1)[:, None]


def _diff_attention(q, k, v, cos, sin, qn_g, kn_g, lam_params, subln_g, lambda_init):
    B, S, _ = q.shape
    q = q.reshape(B, S, DIFF_HEADS, 2, DIFF_QK)
    k = k.reshape(B, S, DIFF_HEADS, 2, DIFF_QK)
    v = v.reshape(B, S, DIFF_HEADS, DIFF_V).transpose(0, 2, 1, 3)
    q = (_partial_rope(_rms(q, qn_g), cos, sin) * DIFF_QK ** -0.5).transpose(0, 2, 3, 1, 4)
    k = _partial_rope(_rms(k, kn_g), cos, sin).transpose(0, 2, 3, 1, 4)
    lp = lam_params.astype(jnp.float32)
    lam = jnp.exp(jnp.sum(lp[0] * lp[1])) - jnp.exp(jnp.sum(lp[2] * lp[3])) + lambda_init
    outs = []
    for i in range(S // Q_BLOCK):
        s0, s1 = i * Q_BLOCK, (i + 1) * Q_BLOCK
        sc = jnp.einsum('bhmqd,bhmkd->bhmqk', q[:, :, :, s0:s1], k[:, :, :, :s1]).astype(jnp.float32)
        p = jax.nn.softmax(jnp.where(_causal_mask(s0, s1), sc, NEG_INF), axis=-1)
        pd = p[:, :, 0] - lam * p[:, :, 1]
        outs.append(jnp.einsum('bhqk,bhkd->bhqd', pd.astype(v.dtype), v[:, :, :s1]))
    o = _rms(jnp.concatenate(outs, axis=2), subln_g) * (1.0 - lambda_init)
    return o.transpose(0, 2, 1, 3).reshape(B, S, DIFF_HEADS * DIFF_V)


def _forgetting_attention(q, k, v, f_logit, f_bias, qn_g, kn_g):
    B, S, _ = q.shape
    q = (_rms(q.reshape(B, S, FOX_HEADS, FOX_DIM), qn_g) * FOX_DIM ** -0.5).transpose(0, 2, 1, 3)
    k = _rms(k.reshape(B, S, FOX_HEADS, FOX_DIM), kn_g).transpose(0, 2, 1, 3)
    v = v.reshape(B, S, FOX_HEADS, FOX_DIM).transpose(0, 2, 1, 3)
    log_f = jax.nn.log_sigmoid(f_logit.astype(jnp.float32) + f_bias.astype(jnp.float32))
    cum = jnp.cumsum(log_f, axis=1).transpose(0, 2, 1)
    outs = []
    for i in range(S // Q_BLOCK):
        s0, s1 = i * Q_BLOCK, (i + 1) * Q_BLOCK
        sc = jnp.einsum('bhqd,bhkd->bhqk', q[:, :, s0:s1], k[:, :, :s1]).astype(jnp.float32)
        sc = sc + cum[:, :, s0:s1, None] - cum[:, :, None, :s1]
        p = jax.nn.softmax(jnp.where(_causal_mask(s0, s1), sc, NEG_INF), axis=-1)
        outs.append(jnp.einsum('bhqk,bhkd->bhqd', p.astype(v.dtype), v[:, :, :s1]))
    o = jnp.concatenate(outs, axis=2)
    return o.transpose(0, 2, 1, 3).reshape(B, S, FOX_HEADS * FOX_DIM)


def _memory_attention(q, mem_n, w_kv, qn_g, kn_g):
    B, S, _ = q.shape
    M = mem_n.shape[1]
    q = (_rms(q.reshape(B, S, MEM_HEADS, MEM_DIM), qn_g) * MEM_DIM ** -0.5).transpose(0, 2, 1, 3)
    kv = jnp.einsum('bmd,dc->bmc', mem_n, w_kv).reshape(B, M, 2, MEM_HEADS, MEM_DIM)
    k = _rms(kv[:, :, 0], kn_g).transpose(0, 2, 1, 3)
    v = kv[:, :, 1].transpose(0, 2, 1, 3)
    p = jax.nn.softmax(jnp.einsum('bhsd,bhmd->bhsm', q, k).astype(jnp.float32), axis=-1)
    o = jnp.einsum('bhsm,bhmd->bhsd', p.astype(v.dtype), v)
    return o.transpose(0, 2, 1, 3).reshape(B, S, MEM_HEADS * MEM_DIM)


def _hier_moe(h, w_rg, b_rg, w_re, b_re, w_up, w_down):
    B, S, D = h.shape
    T = B * S
    ht = h.reshape(T, D)
    g_logits = (ht @ w_rg).astype(jnp.float32) + b_rg.astype(jnp.float32)
    g_idx = jnp.argmax(g_logits, axis=-1).astype(jnp.int32)
    g_gate = jnp.take_along_axis(jax.nn.softmax(g_logits, axis=-1), g_idx[:, None], axis=-1)
    e_logits = ((ht @ w_re).astype(jnp.float32) + b_re.astype(jnp.float32)).reshape(T, N_GROUPS, EXPERTS_PER_GROUP)
    e_logits = jnp.take_along_axis(e_logits, g_idx[:, None, None], axis=1)[:, 0]
    top_logit, top_local = lax.top_k(e_logits, EXPERT_TOP_K)
    weights = g_gate * jax.nn.softmax(top_logit, axis=-1)
    expert_id = g_idx[:, None] * EXPERTS_PER_GROUP + top_local.astype(jnp.int32)
    TK = T * EXPERT_TOP_K
    e_flat = expert_id.reshape(TK)
    tok = jnp.repeat(jnp.arange(T, dtype=jnp.int32), EXPERT_TOP_K)
    w_flat = weights.reshape(TK)
    order = jnp.argsort(e_flat)
    e_s, tok_s, w_s = e_flat[order], tok[order], w_flat[order]
    counts = jnp.bincount(e_flat, length=N_EXPERTS)
    starts = jnp.cumsum(counts) - counts
    padded = (counts + MOE_BLOCK - 1) // MOE_BLOCK * MOE_BLOCK
    pends = jnp.cumsum(padded)
    pstarts = pends - padded
    dest = pstarts[e_s] + jnp.arange(TK, dtype=jnp.int32) - starts[e_s]
    n_blocks = (TK + N_EXPERTS * (MOE_BLOCK - 1) + MOE_BLOCK - 1) // MOE_BLOCK
    rows = jnp.zeros((n_blocks * MOE_BLOCK, D), h.dtype).at[dest].set(ht[tok_s])
    block_expert = jnp.minimum(
        jnp.searchsorted(pends, jnp.arange(n_blocks, dtype=jnp.int32) * MOE_BLOCK, side='right'),
        N_EXPERTS - 1)

    def expert_block(args):
        xb, e = args
        a, b = jnp.split(xb @ w_up[e], 2, axis=-1)
        return (jax.nn.silu(a) * b) @ w_down[e]

    ys = lax.map(expert_block, (rows.reshape(n_blocks, MOE_BLOCK, D), block_expert)).reshape(-1, D)
    contrib = (w_s[:, None] * ys[dest].astype(jnp.float32)).astype(h.dtype)
    out = jnp.zeros((T, D), h.dtype).at[tok_s].add(contrib)
    return out.reshape(B, S, D)


def setup_inputs(seed: int = 0) -> dict:
    key = jax.random.key(seed)
    ks = jax.random.split(key, 32)

    def nrm(k, shape, scale):
        return jax.random.normal(k, shape, jnp.float32) * scale

    def gain(k, shape):
        return 1.0 + 0.02 * jax.random.normal(k, shape, jnp.float32)

    L, D = DEPTH, D_MODEL
    positions = (jnp.arange(SEQ, dtype=jnp.int32)[None, :]
                 + jax.random.randint(ks[2], (BATCH, 1), 0, 1024, dtype=jnp.int32))
    return {
        "x": nrm(ks[0], (BATCH, SEQ, D), 1.0),
        "mem": nrm(ks[1], (BATCH, MEM_LEN, D), 1.0),
        "positions": positions,
        "attn_norm_g": gain(ks[3], (L, D)),
        "w_in": nrm(ks[4], (L, D, IN_COLS), D ** -0.5),
        "diff_qnorm_g": gain(ks[5], (L, DIFF_QK)),
        "diff_knorm_g": gain(ks[6], (L, DIFF_QK)),
        "diff_lambda": nrm(ks[7], (L, 4, DIFF_QK), 0.1),
        "diff_subln_g": gain(ks[8], (L, DIFF_V)),
        "fox_qnorm_g": gain(ks[9], (L, FOX_DIM)),
        "fox_knorm_g": gain(ks[10], (L, FOX_DIM)),
        "fox_forget_b": 3.0 + 0.5 * jax.random.normal(ks[11], (L, FOX_HEADS), jnp.float32),
        "mem_norm_g": gain(ks[12], (L, D)),
        "w_mem_kv": nrm(ks[13], (L, D, 2 * MEM_HEADS * MEM_DIM), D ** -0.5),
        "mem_qnorm_g": gain(ks[14], (L, MEM_DIM)),
        "mem_knorm_g": gain(ks[15], (L, MEM_DIM)),
        "w_o_diff": nrm(ks[16], (L, DIFF_HEADS * DIFF_V, D), (DIFF_HEADS * DIFF_V) ** -0.5),
        "w_o_fox": nrm(ks[17], (L, FOX_HEADS * FOX_DIM, D), (FOX_HEADS * FOX_DIM) ** -0.5),
        "w_o_mem": nrm(ks[18], (L, MEM_HEADS * MEM_DIM, D), (MEM_HEADS * MEM_DIM) ** -0.5),
        "w_out": nrm(ks[19], (L, D, D), D ** -0.5),
        "ffn_norm_g": gain(ks[20], (L, D)),
        "w_router_group": nrm(ks[21], (L, D, N_GROUPS), D ** -0.5),
        "b_router_group": nrm(ks[22], (L, N_GROUPS), 0.01),
        "w_router_expert": nrm(ks[23], (L, D, N_EXPERTS), D ** -0.5),
        "b_router_expert": nrm(ks[24], (L, N_EXPERTS), 0.01),
        "w_up": nrm(ks[25], (L, N_EXPERTS, D, 2 * EXPERT_FF), D ** -0.5),
        "w_down": nrm(ks[26], (L, N_EXPERTS, EXPERT_FF, D), EXPERT_FF ** -0.5),
    }


def reference(x, mem, positions, attn_norm_g, w_in, diff_qnorm_g, diff_knorm_g, diff_lambda,
              diff_subln_g, fox_qnorm_g, fox_knorm_g, fox_forget_b, mem_norm_g, w_mem_kv,
              mem_qnorm_g, mem_knorm_g, w_o_diff, w_o_fox, w_o_mem, w_out, ffn_norm_g,
              w_router_group, b_router_group, w_router_expert, b_router_expert, w_up, w_down):
    B, S, D = x.shape
    cos, sin = _rope_tables(positions)
    split_points = [int(i) for i in np.cumsum(IN_SPLITS)[:-1]]
    for l in range(DEPTH):
        lambda_init = 0.8 - 0.6 * math.exp(-0.3 * l)
        h = _rms(x, attn_norm_g[l])
        proj = jnp.einsum('bsd,dc->bsc', h, w_in[l])
        qa, ka, va, qb, kb, vb, fb, qc, gates = jnp.split(proj, split_points, axis=-1)
        ya = _diff_attention(qa, ka, va, cos, sin, diff_qnorm_g[l], diff_knorm_g[l],
                             diff_lambda[l], diff_subln_g[l], lambda_init)
        yb = _forgetting_attention(qb, kb, vb, fb, fox_forget_b[l], fox_qnorm_g[l], fox_knorm_g[l])
        yc = _memory_attention(qc, _rms(mem, mem_norm_g[l]), w_mem_kv[l], mem_qnorm_g[l], mem_knorm_g[l])
        g = jax.nn.sigmoid(gates.astype(jnp.float32)).astype(x.dtype).reshape(B, S, N_BRANCHES, D)
        merged = (g[:, :, 0] * (ya @ w_o_diff[l])
                  + g[:, :, 1] * (yb @ w_o_fox[l])
                  + g[:, :, 2] * (yc @ w_o_mem[l]))
        x = x + merged @ w_out[l]
        x = x + _hier_moe(_rms(x, ffn_norm_g[l]), w_router_group[l], b_router_group[l],
                          w_router_expert[l], b_router_expert[l], w_up[l], w_down[l])
    return x
```

```python
import math
from contextlib import ExitStack

import numpy as np
import concourse.bass as bass
import concourse.mybir as mybir
from concourse.bass_utils import run_bass_kernel_spmd

F32 = mybir.dt.float32
BF16 = mybir.dt.bfloat16
I32 = mybir.dt.int32
AF = mybir.ActivationFunctionType
ALU = mybir.AluOpType
AX = mybir.AxisListType

D = 1024
KC = 8
MEM_LEN = 256
EPS = 1e-6
N_EXP = 32
FF = 512
IN_COLS = 6664
OFF_QA, OFF_KA, OFF_VA, OFF_QB, OFF_KB, OFF_VB, OFF_FB, OFF_QC, OFF_G = 0, 512, 1024, 1536, 2048, 2560, 3072, 3080, 3592
ROPE_THETA = 500000.0
LAMBDA_INIT = 0.8 - 0.6 * math.exp(0.0)

C_ID, C_MASK, C_TLE, C_TLT, C_B64, C_ROPE, C_INVF, C_ONE = 0, 128, 256, 384, 512, 640, 768, 769
C_EB = 769 + 128
NCONST = C_EB + 32
R_AG, R_MG, R_FG, R_BR, R_SUB, R_FB, R_LAM = 0, 1024, 2048, 3072, 3108, 3236, 3244
NROWB = 3244 + 256
P_DQ, P_DK, P_FQ, P_FK, P_MQ, P_MK, P_SUB = 0, 1, 2, 3, 4, 5, 6
NCOLP = 7

ND_SEMS = 20


class Buf:
    __slots__ = ("w", "cw", "r")

    def __init__(self):
        self.w = {}
        self.cw = {}
        self.r = {}


class T:
    def __init__(self, h):
        self.h = h
        self.b = Buf()
        self.subs = {}

    def __getitem__(self, k):
        return self.h[k]

    def sub(self, key):
        b = self.subs.get(key)
        if b is None:
            b = self.subs[key] = Buf()
        return b


def _b(x):
    return x.b if isinstance(x, T) else x


class Eng:
    def __init__(self, k, eng, name, skip_self=False):
        self.eng = eng
        self.name = name
        self.key = k.newsem(name)
        self.cnt = 0
        self.waited = {}
        self.skip_self = skip_self
        self.dkeys = None
        self.dn = 0


class K:
    def __init__(self, nc):
        self.nc = nc
        self.es = ExitStack()
        self.sems = []
        self.PE = Eng(self, nc.tensor, "pe", skip_self=True)
        self.ACT = Eng(self, nc.scalar, "act")
        self.DVE = Eng(self, nc.vector, "dve")
        self.POOL = Eng(self, nc.gpsimd, "pool")
        self.SP = Eng(self, nc.sync, "sp")
        self.engs = [self.PE, self.ACT, self.DVE, self.POOL, self.SP]
        for q in (self.SP, self.POOL, self.ACT):
            q.dkeys = [self.newsem(f"d{q.name}{i}") for i in range(ND_SEMS)]
        self.uid = 0

    def newsem(self, name):
        h = self.es.enter_context(self.nc.semaphore(name))
        self.sems.append(h)
        return len(self.sems) - 1

    def name(self, base):
        self.uid += 1
        return f"{base}_{self.uid}"

    def sb(self, sc, name, shape, dt):
        return T(sc.enter_context(self.nc.sbuf_tensor(self.name(name), list(shape), dt)))

    def ps(self, sc, name, shape, dt):
        return T(sc.enter_context(self.nc.psum_tensor(self.name(name), list(shape), dt)))

    def _wait(self, E, deps):
        for k, v in deps.items():
            if E.skip_self and k == E.key:
                continue
            if E.waited.get(k, 0) >= v:
                continue
            E.eng.wait_ge(self.sems[k], v)
            E.waited[k] = v

    @staticmethod
    def _deps(r, w, cw):
        d = {}

        def add(m):
            for k, v in m.items():
                if d.get(k, 0) < v:
                    d[k] = v

        for b in r:
            add(b.w)
            add(b.cw)
        for b in w:
            add(b.w)
            add(b.cw)
            add(b.r)
        for b in cw:
            add(b.w)
            add(b.r)
        return d

    @staticmethod
    def _record(ev, r, w, cw):
        k, v = ev
        for b in r:
            if b.r.get(k, 0) < v:
                b.r[k] = v
        for b in w:
            b.w = {k: v}
            b.cw = {}
            b.r = {}
        for b in cw:
            if b.cw.get(k, 0) < v:
                b.cw[k] = v

    def op(self, E, fn, r=(), w=(), cw=()):
        r = [_b(x) for x in r]
        w = [_b(x) for x in w]
        cw = [_b(x) for x in cw]
        self._wait(E, self._deps(r, w, cw))
        inst = fn(E.eng)
        E.cnt += 1
        inst.then_inc(self.sems[E.key], 1)
        self._record((E.key, E.cnt), r, w, cw)

    def dma(self, Q, out, in_, r=(), w=(), cw=(), scatter_idx=None, gather_idx=None, bounds=None, **kw):
        r = [_b(x) for x in r]
        w = [_b(x) for x in w]
        cw = [_b(x) for x in cw]
        self._wait(Q, self._deps(r, w, cw))
        i = Q.dn
        Q.dn += 1
        slot, use = i % ND_SEMS, i // ND_SEMS
        k = Q.dkeys[slot]
        if use > 0 and Q.waited.get(k, 0) < 16 * use:
            Q.eng.wait_ge(self.sems[k], 16 * use)
            Q.waited[k] = 16 * use
        if bounds is not None:
            if not hasattr(self, "_breg"):
                self._breg = {}
            if bounds not in self._breg:
                self._breg[bounds] = Q.eng.to_reg(bounds)
            bounds = self._breg[bounds]
        if scatter_idx is not None:
            inst = Q.eng.indirect_dma_start(out=out, out_offset=bass.IndirectOffsetOnAxis(ap=scatter_idx, axis=0),
                                            in_=in_, in_offset=None, bounds_check=bounds, oob_is_err=False)
        elif gather_idx is not None:
            inst = Q.eng.indirect_dma_start(out=out, out_offset=None, in_=in_,
                                            in_offset=bass.IndirectOffsetOnAxis(ap=gather_idx, axis=0),
                                            bounds_check=bounds, oob_is_err=False)
        else:
            inst = Q.eng.dma_start(out=out, in_=in_, **kw)
        inst.then_inc(self.sems[k], 16)
        self._record((k, 16 * (use + 1)), r, w, cw)

    def barrier(self, engs=None):
        tot = {}
        for E in self.engs:
            if E.cnt:
                tot[E.key] = E.cnt
            if E.dkeys:
                for s, k in enumerate(E.dkeys):
                    uses = (E.dn - s + ND_SEMS - 1) // ND_SEMS if E.dn > s else 0
                    if uses:
                        tot[k] = 16 * uses
        for E in (engs or self.engs):
            d = dict(tot)
            d.pop(E.key, None)
            sk = E.skip_self
            E.skip_self = False
            self._wait(E, d)
            E.skip_self = sk


def build_program(S, NSEQ, CAP, dbg=None):
    nc = bass.Bass("TRN2", target_bir_lowering=False)
    k = K(nc)
    k.es.enter_context(nc.allow_low_precision("bf16 operands / fp32 accumulation by design"))
    PE, ACT, DVE, POOL, SP = k.PE, k.ACT, k.DVE, k.POOL, k.SP
    NT = S // 128
    NB = S // 512
    NTT = NSEQ * NT
    NSLOT = N_EXP * CAP
    EB = 384 if CAP % 384 == 0 else 128
    dt = nc.dram_tensor

    x_d = dt("x", [NSEQ, S, D], F32, kind="ExternalInput").ap()
    mem_d = dt("mem", [NSEQ, MEM_LEN, D], F32, kind="ExternalInput").ap()
    pos_d = dt("pos", [NSEQ, S], I32, kind="ExternalInput").ap()
    consts_d = dt("consts", [128, NCONST], F32, kind="ExternalInput").ap()
    rowb_d = dt("rowb", [128, NROWB], F32, kind="ExternalInput").ap()
    colp_d = dt("colp", [128, NCOLP], F32, kind="ExternalInput").ap()
    w_in_d = dt("w_in", [D, IN_COLS], F32, kind="ExternalInput").ap()
    w_kv_d = dt("w_mem_kv", [D, 1024], F32, kind="ExternalInput").ap()
    w_o_d = [dt(n, [512, D], F32, kind="ExternalInput").ap() for n in ("w_o_diff", "w_o_fox", "w_o_mem")]
    w_out_d = dt("w_out", [D, D], F32, kind="ExternalInput").ap()
    w_r_d = dt("w_router", [D, 36], F32, kind="ExternalInput").ap()
    w_up_d = dt("w_up", [N_EXP, D, 2 * FF], F32, kind="ExternalInput").ap()
    w_dn_d = dt("w_down", [N_EXP, FF, D], F32, kind="ExternalInput").ap()
    out_d = dt("out", [NSEQ, S, D], F32, kind="ExternalOutput").ap()
    yT_d = dt("yT_scr", [NSEQ, 12, 128, S], BF16).ap()
    g_d = dt("g_scr", [NSEQ, 24, 128, S], BF16).ap()
    xs_d = dt("xs_scr", [NSLOT, D], BF16).ap()
    ys_d = dt("ys_scr", [NSLOT, D], BF16).ap()
    B_yT, B_g, B_xs, B_ys, B_out = Buf(), Buf(), Buf(), Buf(), Buf()
    dbg_d = {}
    if dbg:
        for n, (shape, dty) in dbg.items():
            dbg_d[n] = dt(n, list(shape), dty, kind="ExternalOutput").ap()

    top = k.es
    cf = k.sb(top, "cf", [128, NCONST], F32)
    cb = k.sb(top, "cb", [128, NCONST], BF16)
    rowb = k.sb(top, "rowb", [128, NROWB], F32)
    colp = k.sb(top, "colp", [128, NCOLP], F32)
    lam = k.sb(top, "lam", [128, 4], F32)
    wr = k.sb(top, "wr", [128, KC, 36], F32)
    rt_d1 = k.sb(top, "rt_d1", [128, NTT], I32)
    rt_d2 = k.sb(top, "rt_d2", [128, NTT], I32)
    rt_w = k.sb(top, "rt_w", [128, NTT, 2], F32)
    cnt = k.sb(top, "cnt", [128, N_EXP], F32)
    banks = [k.ps(top, f"bank{i}", [128, 512], F32) for i in range(8)]

    k.dma(SP, cf[:], consts_d[:, :], w=[cf])
    k.dma(SP, rowb[:], rowb_d[:, :], w=[rowb])
    k.dma(SP, colp[:], colp_d[:, :], w=[colp])
    k.dma(SP, wr[:], w_r_d.rearrange("(kc p) c -> p kc c", p=128), w=[wr])
    k.op(DVE, lambda e: e.tensor_copy(out=cb[:], in_=cf[:]), r=[cf], w=[cb])
    k.op(POOL, lambda e: e.memset(cnt[:], 0.0), w=[cnt])
    ident_b = cb[:, C_ID:C_ID + 128]
    ident_f = cf[:, C_ID:C_ID + 128]

    with ExitStack() as sc:
        t = k.sb(sc, "lt", [128, 2, 64], F32)
        s2 = k.sb(sc, "ls", [128, 2], F32)
        lp = rowb[:, R_LAM:R_LAM + 256].rearrange("p (a b c) -> p a b c", a=2, b=2)
        k.op(DVE, lambda e: e.tensor_tensor(out=t[:], in0=lp[:, :, 0, :], in1=lp[:, :, 1, :], op=ALU.mult), r=[rowb], w=[t])
        k.op(DVE, lambda e: e.reduce_sum(out=s2[:], in_=t[:], axis=AX.X), r=[t], w=[s2])
        k.op(ACT, lambda e: e.activation(out=s2[:], in_=s2[:], func=AF.Exp), r=[s2], w=[s2])
        k.op(DVE, lambda e: e.scalar_tensor_tensor(out=lam[:, 0:1], in0=s2[:, 1:2], scalar=-LAMBDA_INIT, in1=s2[:, 0:1],
                                                   op0=ALU.add, op1=ALU.subtract), r=[s2], w=[lam])
        k.op(DVE, lambda e: e.tensor_scalar(out=lam[:, 1:2], in0=colp[:, P_SUB:P_SUB + 1], scalar1=1.0 - LAMBDA_INIT,
                                            scalar2=None, op0=ALU.mult), r=[colp, lam], w=[lam])
        k.barrier()

    bank_rr = [0]

    def rms_rstd(E_ok, out_ap, in_ap, n, rbufs, wbufs):
        k.op(ACT, lambda e: e.activation(out=out_ap, in_=in_ap, func=AF.Ln, scale=1.0 / n, bias=EPS),
             r=rbufs, w=wbufs)
        k.op(ACT, lambda e: e.activation(out=out_ap, in_=out_ap, func=AF.Exp, scale=-0.5), r=wbufs, w=wbufs)

    def load_w(sc_t, src_ap, rows_kc, ncols):
        k.dma(POOL, sc_t[:, 0:rows_kc, 0:ncols], src_ap.rearrange("(kc p) c -> p kc c", p=128), w=[sc_t])

    sc_cache = {}

    def norm_tokens_to_T(sc, src_ap_fn, ntiles, gcol0, dstT, tag, every4=None):
        if "nb" not in sc_cache:
            sc_cache["nb"] = ([k.sb(sc, f"xt{i}", [128, D], F32) for i in range(4)],
                              [k.sb(sc, f"xn{i}", [128, D], BF16) for i in range(4)],
                              [k.sb(sc, f"st{i}", [128, 2], F32) for i in range(4)],
                              k.sb(sc, "junk", [128, D], BF16))
        xts, xn, st, junk = sc_cache["nb"]
        for t in range(ntiles):
            xt, xb, s_ = xts[t % 4], xn[t % 4], st[t % 4]
            if every4 is not None and t % 4 == 0:
                every4(t // 4)
            k.dma(SP, xt[:], src_ap_fn(t), w=[xt])
            k.op(ACT, lambda e: e.activation(out=junk[:], in_=xt[:], func=AF.Square, accum_out=s_[:, 0:1]), r=[xt], w=[junk, s_])
            rms_rstd(None, s_[:, 1:2], s_[:, 0:1], D, [s_], [s_])
            k.op(DVE, lambda e: e.scalar_tensor_tensor(out=xb[:], in0=xt[:], scalar=s_[:, 1:2], in1=rowb[:, gcol0:gcol0 + D],
                                                       op0=ALU.mult, op1=ALU.mult), r=[xt, s_, rowb], w=[xb])
            bk = banks[6 + (t % 2)]
            bkb = bk.h.bitcast(BF16)

            def tr(e):
                for c in range(KC):
                    i = e.transpose(out=bkb[:, c * 128:(c + 1) * 128], in_=xb[:, c * 128:(c + 1) * 128], identity=ident_b)
                return i
            k.op(PE, tr, r=[xb, cb], w=[bk])
            k.op(DVE, lambda e: e.tensor_copy(out=dstT[:, :, t * 128:(t + 1) * 128],
                                              in_=bkb[:, 0:1024].rearrange("p (c t) -> p c t", c=KC)), r=[bk], w=[dstT.sub(t // 4)])

    def proj_fm(hT, hbuf, ws, wcol0, M, tok0, ntok, bank):
        def f(e):
            for c in range(KC):
                i = e.matmul(out=bank[0:M, 0:ntok], lhsT=ws[:, c, wcol0:wcol0 + M], rhs=hT[:, c, tok0:tok0 + ntok],
                             start=(c == 0), stop=(c == KC - 1))
            return i
        k.op(PE, f, r=[hbuf, ws], w=[bank])

    for seq in range(NSEQ):
        with ExitStack() as sc:
            hT = k.sb(sc, "hT", [128, KC, S], BF16)
            memT = k.sb(sc, "memT", [128, KC, MEM_LEN], BF16)
            ropeC = k.sb(sc, "ropeC", [128, S], BF16)
            ropeS = k.sb(sc, "ropeS", [128, S], BF16)
            with ExitStack() as s1:
                sc_cache.clear()
                RW = 512
                posi = k.sb(s1, "posi", [128, RW], I32)
                ang = k.sb(s1, "ang", [128, RW], F32)
                kk = k.sb(s1, "kk", [128, RW], F32)
                ki = k.sb(s1, "ki", [128, RW], I32)
                ang2 = k.sb(s1, "ang2", [128, RW], F32)
                TWO_PI = 2.0 * math.pi

                def wrap(a, shift):
                    k.op(DVE, lambda e: e.tensor_scalar(out=kk[:], in0=a[:], scalar1=shift, scalar2=1.0 / TWO_PI,
                                                        op0=ALU.add, op1=ALU.mult), r=[a], w=[kk])
                    k.op(DVE, lambda e: e.tensor_copy(out=ki[:], in_=kk[:]), r=[kk], w=[ki])
                    k.op(DVE, lambda e: e.tensor_copy(out=kk[:], in_=ki[:]), r=[ki], w=[kk])
                    k.op(DVE, lambda e: e.scalar_tensor_tensor(out=kk[:], in0=kk[:], scalar=-TWO_PI, in1=a[:],
                                                               op0=ALU.mult, op1=ALU.add), r=[kk, a], w=[kk])
                    if shift != 0.0:
                        k.op(DVE, lambda e: e.tensor_scalar(out=kk[:], in0=kk[:], scalar1=shift, scalar2=None, op0=ALU.add),
                             r=[kk], w=[kk])
                    k.op(DVE, lambda e: e.tensor_scalar(out=a[:], in0=kk[:], scalar1=math.pi, scalar2=-TWO_PI,
                                                        op0=ALU.is_gt, op1=ALU.mult), r=[kk], w=[a])
                    k.op(DVE, lambda e: e.tensor_tensor(out=kk[:], in0=kk[:], in1=a[:], op=ALU.add), r=[kk, a], w=[kk])
                    k.op(DVE, lambda e: e.tensor_scalar(out=a[:], in0=kk[:], scalar1=-math.pi, scalar2=TWO_PI,
                                                        op0=ALU.is_lt, op1=ALU.mult), r=[kk], w=[a])
                    k.op(DVE, lambda e: e.tensor_tensor(out=a[:], in0=kk[:], in1=a[:], op=ALU.add), r=[kk, a], w=[a])
                    k.op(DVE, lambda e: e.tensor_scalar(out=a[:], in0=a[:], scalar1=3.14159, scalar2=-3.14159,
                                                        op0=ALU.min, op1=ALU.max), r=[a], w=[a])

                def rope_block(bi):
                    r0_ = bi * RW
                    k.dma(SP, posi[:], pos_d[seq:seq + 1, r0_:r0_ + RW].partition_broadcast(128), w=[posi])
                    k.op(DVE, lambda e: e.tensor_copy(out=ang[:], in_=posi[:]), r=[posi], w=[ang])
                    k.op(DVE, lambda e: e.tensor_scalar(out=ang[:], in0=ang[:], scalar1=cf[:, C_INVF:C_INVF + 1], scalar2=None,
                                                        op0=ALU.mult), r=[ang, cf], w=[ang])
                    wrap(ang, 0.0)
                    k.op(ACT, lambda e: e.activation(out=ropeS[:, r0_:r0_ + RW], in_=ang[:], func=AF.Sin), r=[ang], w=[ropeS])
                    k.op(ACT, lambda e: e.activation(out=ang2[:], in_=ang[:], func=AF.Abs), r=[ang], w=[ang2])
                    k.op(DVE, lambda e: e.tensor_scalar(out=ang2[:], in0=ang2[:], scalar1=-1.0, scalar2=math.pi / 2.0,
                                                        op0=ALU.mult, op1=ALU.add), r=[ang2], w=[ang2])
                    k.op(ACT, lambda e: e.activation(out=ropeC[:, r0_:r0_ + RW], in_=ang2[:], func=AF.Sin), r=[ang2], w=[ropeC])
                norm_tokens_to_T(s1, lambda t: x_d[seq, t * 128:(t + 1) * 128, :], NT, R_AG, hT, "x", every4=rope_block)
                norm_tokens_to_T(s1, lambda t: mem_d[seq, t * 128:(t + 1) * 128, :], MEM_LEN // 128, R_MG, memT, "m")
                k.barrier()
            QB = k.sb(sc, "QB", [128, S], BF16)
            KB = k.sb(sc, "KB", [128, S], BF16)
            QB2 = k.sb(sc, "QB2", [128, S], BF16)
            dacc = [k.sb(sc, f"dacc{i}", [128, 512], F32) for i in range(4)]
            VB = k.sb(sc, "VB", [128, NT, 128], BF16)
            WS = [k.sb(sc, f"WS{i}", [128, KC, 512], BF16) for i in range(3)]
            PT = [k.sb(sc, f"PT{i}", [128, 512], BF16) for i in range(4)]
            sq = [k.sb(sc, f"sq{i}", [128, 512], BF16) for i in range(3)]
            rstd = [k.sb(sc, f"rstd{i}", [128, 512], F32) for i in range(3)]
            qn = [k.sb(sc, f"qn{i}", [128, 512], BF16) for i in range(3)]
            tmpf = [k.sb(sc, f"tmpf{i}", [128, 512], F32) for i in range(3)]
            ytb = [k.sb(sc, f"ytb{i}", [128, 512], BF16) for i in range(2)]
            rr = [k.sb(sc, f"rr{i}", [128, 2, 2, 2], F32) for i in range(4)]
            ncum = k.sb(sc, "ncum", [128, NT, 8], F32)
            ncum8 = k.sb(sc, "ncum8", [128, 64 + NT * 8], BF16)
            nl = k.sb(sc, "nl", [128, NT, 8], F32)
            offs = k.sb(sc, "offs", [128, NT, 8], F32)
            memK = k.sb(sc, "memK", [128, 4, MEM_LEN], BF16)
            memV = k.sb(sc, "memV", [128, 2, 4, 128], BF16)
            dtm = [k.sb(sc, f"dtm{i}", [128, 512], F32) for i in range(3)]

            hbufs = [hT.sub(i) for i in range((NT + 3) // 4)]

            def qk_proj(ws, wcol0, M, src, sbufs, ntok_total, dst, dbuf, gcolidx, blkn, rope, ctr=[0], split=None):
                for t0 in range(0, ntok_total, 512):
                    n = min(512, ntok_total - t0)
                    i = ctr[0] % 3
                    ctr[0] += 1
                    bk = banks[(6, 7, 3)[i]]
                    bk2 = banks[(4, 5, 2)[i]]
                    proj_fm(src, sbufs[t0 // 512] if len(sbufs) > 1 else sbufs[0], ws, wcol0, M, t0, n, bk)
                    k.op(ACT, lambda e: e.activation(out=sq[i][0:M, 0:n], in_=bk[0:M, 0:n], func=AF.Square), r=[bk], w=[sq[i]])
                    ones = cb[0:M, C_B64:C_B64 + M] if blkn == 64 else cb[0:M, C_ONE:C_ONE + M]
                    k.op(PE, lambda e: e.matmul(out=bk2[0:M, 0:n], lhsT=ones, rhs=sq[i][0:M, 0:n], start=True, stop=True),
                         r=[sq[i], cb], w=[bk2])
                    rms_rstd(None, rstd[i][0:M, 0:n], bk2[0:M, 0:n], blkn, [bk2], [rstd[i]])
                    out_ap = dst[0:M, t0:t0 + n] if not rope else qn[i][0:M, 0:n]
                    obuf = [dbuf] if not rope else [qn[i]]
                    k.op(DVE, lambda e: e.scalar_tensor_tensor(out=out_ap, in0=bk[0:M, 0:n], scalar=colp[0:M, gcolidx:gcolidx + 1],
                                                               in1=rstd[i][0:M, 0:n], op0=ALU.mult, op1=ALU.mult),
                         r=[bk, colp, rstd[i]], w=obuf)
                    if rope:
                        k.op(PE, lambda e: e.matmul(out=bk2[0:M, 0:n], lhsT=cb[0:M, C_ROPE:C_ROPE + M], rhs=qn[i][0:M, 0:n],
                                                    start=True, stop=True), r=[qn[i], cb], w=[bk2])
                        k.op(DVE, lambda e: e.tensor_tensor(out=tmpf[i][0:M, 0:n], in0=bk2[0:M, 0:n], in1=ropeS[0:M, t0:t0 + n],
                                                            op=ALU.mult), r=[bk2, ropeS], w=[tmpf[i]])
                        k.op(DVE, lambda e: e.tensor_tensor(out=rstd[i][0:M, 0:n], in0=qn[i][0:M, 0:n], in1=ropeC[0:M, t0:t0 + n],
                                                            op=ALU.mult), r=[qn[i], ropeC], w=[rstd[i]])
                        if split is None:
                            k.op(DVE, lambda e: e.tensor_tensor(out=dst[0:M, t0:t0 + n], in0=rstd[i][0:M, 0:n], in1=tmpf[i][0:M, 0:n],
                                                                op=ALU.add), r=[rstd[i], tmpf[i]], w=[dbuf])
                        else:
                            for (dd, lo) in ((dst, 0), (split, 64)):
                                k.op(DVE, lambda e: e.tensor_tensor(out=dd[lo:lo + 64, t0:t0 + n], in0=rstd[i][lo:lo + 64, 0:n],
                                                                     in1=tmpf[i][lo:lo + 64, 0:n], op=ALU.add),
                                     r=[rstd[i], tmpf[i]], w=[dd])

            def v_proj(ws, wcol0, dv, src, sbufs, ntiles, dstV, vbuf, ones_col, vctr=[0]):
                if ones_col is not None:
                    k.op(POOL, lambda e: e.memset(dstV[:, 0:ntiles, ones_col:ones_col + 1], 1.0), w=[vbuf])
                for t0 in range(0, ntiles, 4):
                    nt_ = min(4, ntiles - t0)
                    bk = banks[6 + vctr[0] % 2]
                    vctr[0] += 1

                    def f(e):
                        for tt in range(nt_):
                            for c in range(KC):
                                i = e.matmul(out=bk[:, tt * 128:tt * 128 + dv], lhsT=src[:, c, (t0 + tt) * 128:(t0 + tt + 1) * 128],
                                             rhs=ws[:, c, wcol0:wcol0 + dv], start=(c == 0), stop=(c == KC - 1))
                        return i
                    k.op(PE, f, r=[sbufs[min(t0 // 4, len(sbufs) - 1)], ws], w=[bk])
                    k.op(DVE, lambda e: e.tensor_copy(out=dstV[:, t0:t0 + nt_, 0:dv],
                                                      in_=bk[:, 0:nt_ * 128].rearrange("p (t c) -> p t c", c=128)[:, :, 0:dv]),
                         r=[bk], w=[vbuf])

            sctr = [0]
            pctr = [0]
            pend = []

            def flush_pv(keep=0):
                while len(pend) > keep:
                    pend.pop(0)()

            def attn_fm(qb, Kr, krow0, kT, kbuf, qT, qbuf, V, vbuf, nk_total, causal, scale, bias_fn, bias_bufs, numb, denb):
                q0 = qb * 512
                nk = 4 * qb + 4 if causal else nk_total
                for j in range(nk):
                    r_ = j - 4 * qb if causal else -1
                    c0 = 128 * r_ if r_ > 0 else 0
                    sbk = banks[sctr[0] % 3]
                    sctr[0] += 1
                    pt = PT[pctr[0] % 4]
                    pctr[0] += 1

                    def qk(e, j=j, r_=r_, c0=c0, sbk=sbk):
                        i = e.matmul(out=sbk[:, c0:512], lhsT=kT[krow0:krow0 + Kr, j * 128:(j + 1) * 128],
                                     rhs=qT[krow0:krow0 + Kr, q0 + c0:q0 + 512], start=True, stop=True)
                        if r_ >= 0:
                            i = e.matmul(out=sbk[:, c0:c0 + 128], lhsT=ident_b, rhs=cb[:, C_MASK:C_MASK + 128],
                                         start=False, stop=True, skip_group_check=True)
                        return i
                    k.op(PE, qk, r=[kbuf, qbuf, cb], w=[sbk])
                    if bias_fn is None:
                        k.op(ACT, lambda e: e.activation(out=pt[:, c0:512], in_=sbk[:, c0:512], func=AF.Exp, scale=scale),
                             r=[sbk], w=[pt])
                    else:
                        k.op(ACT, lambda e: e.activation(out=pt[:, c0:512], in_=sbk[:, c0:512], func=AF.Exp, scale=scale,
                                                         bias=bias_fn(j)), r=[sbk] + bias_bufs, w=[pt])

                    def pv(e, j=j, c0=c0, pt=pt, nk=nk):
                        i = e.matmul(out=numb[:, c0:512], lhsT=V(j), rhs=pt[:, c0:512], start=(j == 0), stop=(j == nk - 1),
                                     skip_group_check=True)
                        if denb is not None:
                            i = e.matmul(out=denb[:, c0:512], lhsT=cb[:, C_ONE:C_ONE + 128], rhs=pt[:, c0:512], start=(j == 0),
                                         stop=(j == nk - 1), skip_group_check=True)
                        return i
                    wb = [numb] + ([denb] if denb is not None else [])
                    pend.append(lambda pv=pv, pt=pt, wb=wb: k.op(PE, pv, r=[pt, vbuf, cb], w=wb))
                    flush_pv(2)

            def store_yT(ytile, chunk_idx, qb):
                k.dma(SP, yT_d[seq, chunk_idx, :, qb * 512:(qb + 1) * 512], ytile[:], r=[ytile], cw=[B_yT])

            load_w(WS[0], w_in_d[:, OFF_QA:OFF_QA + 512], KC, 512)
            load_w(WS[1], w_in_d[:, OFF_KA:OFF_KA + 512], KC, 512)
            load_w(WS[2], w_in_d[:, OFF_VA:OFF_VA + 512], KC, 512)
            uc = 0
            dtail = []
            k.op(POOL, lambda e: e.memset(QB[64:128, :], 0.0), w=[QB])
            k.op(POOL, lambda e: e.memset(QB2[0:64, :], 0.0), w=[QB2])
            for h in range(4):
                qk_proj(WS[0], h * 128, 128, hT, hbufs, S, QB, QB, P_DQ, 64, True, split=QB2)
                qk_proj(WS[1], h * 128, 128, hT, hbufs, S, KB, KB, P_DK, 64, True)
                v_proj(WS[2], h * 128, 128, hT, hbufs, NT, VB, VB, None)
                for qb in range(NB):
                    nb_ = [banks[3], banks[5]]
                    db_ = [banks[4], banks[6]]
                    for m in range(2):
                        qsel = QB if m == 0 else QB2
                        attn_fm(qb, 128, 0, KB, KB, qsel, qsel, lambda j: VB[:, j, 0:128], VB, NT, True, 0.125, None, [],
                                nb_[m], db_[m])
                    flush_pv(0)
                    if dtail:
                        dtail.pop(0)()
                    t0_, t1_ = dtm[uc % 2], dtm[2]
                    yt_ = ytb[uc % 2]
                    rs_ = rstd[uc % 2]
                    sq_ = sq[uc % 2]
                    uc += 1
                    k.op(DVE, lambda e: e.tensor_copy(out=dacc[0][:], in_=nb_[0][:]), r=[nb_[0]], w=[dacc[0]])
                    k.op(DVE, lambda e: e.tensor_copy(out=dacc[1][:], in_=db_[0][:]), r=[db_[0]], w=[dacc[1]])
                    k.op(DVE, lambda e: e.tensor_copy(out=dacc[2][:], in_=nb_[1][:]), r=[nb_[1]], w=[dacc[2]])
                    k.op(DVE, lambda e: e.tensor_copy(out=dacc[3][:], in_=db_[1][:]), r=[db_[1]], w=[dacc[3]])
                    k.op(DVE, lambda e: e.reciprocal(out=dacc[1][:], in_=dacc[1][:]), r=[dacc[1]], w=[dacc[1]])
                    k.op(DVE, lambda e: e.tensor_tensor(out=t0_[:], in0=dacc[0][:], in1=dacc[1][:], op=ALU.mult), r=[dacc[0], dacc[1]], w=[t0_])
                    k.op(DVE, lambda e: e.reciprocal(out=dacc[3][:], in_=dacc[3][:]), r=[dacc[3]], w=[dacc[3]])
                    k.op(DVE, lambda e: e.tensor_tensor(out=t1_[:], in0=dacc[2][:], in1=dacc[3][:], op=ALU.mult), r=[dacc[2], dacc[3]], w=[t1_])
                    k.op(DVE, lambda e: e.scalar_tensor_tensor(out=t0_[:], in0=t1_[:], scalar=lam[:, 0:1], in1=t0_[:],
                                                               op0=ALU.mult, op1=ALU.add), r=[t1_, t0_, lam], w=[t0_])
                    k.op(POOL, lambda e: e.tensor_tensor(out=sq_[:], in0=t0_[:], in1=t0_[:], op=ALU.mult), r=[t0_], w=[sq_])

                    def tail(t0_=t0_, yt_=yt_, rs_=rs_, sq_=sq_, h=h, qb=qb):
                        bk7 = banks[7]
                        k.op(PE, lambda e: e.matmul(out=bk7[:], lhsT=cb[:, C_ONE:C_ONE + 128], rhs=sq_[:], start=True, stop=True),
                             r=[sq_, cb], w=[bk7])
                        rms_rstd(None, rs_[:], bk7[:], 128, [bk7], [rs_])
                        k.op(DVE, lambda e: e.scalar_tensor_tensor(out=yt_[:], in0=t0_[:], scalar=lam[:, 1:2], in1=rs_[:],
                                                                   op0=ALU.mult, op1=ALU.mult), r=[t0_, rs_, lam], w=[yt_])
                        store_yT(yt_, 0 + h, qb)
                    dtail.append(tail)
                while dtail:
                    dtail.pop(0)()

            load_w(WS[0], w_in_d[:, OFF_QB:OFF_QB + 512], KC, 512)
            load_w(WS[1], w_in_d[:, OFF_KB:OFF_KB + 512], KC, 512)
            load_w(WS[2], w_in_d[:, OFF_VB:OFF_VB + 512], KC, 512)
            with ExitStack() as s2:
                wfb = k.sb(s2, "wfb", [128, KC, 8], BF16)
                load_w(wfb, w_in_d[:, OFF_FB:OFF_FB + 8], KC, 8)
                for t in range(NT):
                    bk = banks[6 + t % 2]

                    def f(e):
                        for c in range(KC):
                            i = e.matmul(out=bk[:, 0:8], lhsT=hT[:, c, t * 128:(t + 1) * 128], rhs=wfb[:, c, :],
                                         start=(c == 0), stop=(c == KC - 1))
                        return i
                    k.op(PE, f, r=[hbufs[t // 4], wfb], w=[bk])
                    k.op(DVE, lambda e: e.tensor_tensor(out=nl[:, t, :], in0=bk[:, 0:8], in1=rowb[:, R_FB:R_FB + 8], op=ALU.add),
                         r=[bk, rowb], w=[nl.sub(t)])
                nlb = [nl.sub(t) for t in range(NT)]
                k.op(ACT, lambda e: e.activation(out=nl[:], in_=nl[:], func=AF.Exp, scale=-1.0), r=nlb, w=[nl] + nlb)
                k.op(ACT, lambda e: e.activation(out=nl[:], in_=nl[:], func=AF.Ln, bias=1.0), r=[nl], w=[nl])
                nlf = nl[:].rearrange("p t h -> p (t h)")
                bkA, bkB = banks[6], banks[7]
                for c0 in range(0, NT * 8, 512):
                    n = min(512, NT * 8 - c0)
                    k.op(PE, lambda e: e.matmul(out=bkA[:, 0:n], lhsT=cf[:, C_TLE:C_TLE + 128], rhs=nlf[:, c0:c0 + n], start=True, stop=True),
                         r=[nl, cf], w=[bkA])
                    k.op(PE, lambda e: e.matmul(out=bkB[:, 0:n], lhsT=cf[:, C_ONE:C_ONE + 128], rhs=nlf[:, c0:c0 + n], start=True, stop=True),
                         r=[nl, cf], w=[bkB])
                    k.op(DVE, lambda e: e.tensor_copy(out=ncum[:].rearrange("p t h -> p (t h)")[:, c0:c0 + n], in_=bkA[:, 0:n]),
                         r=[bkA], w=[ncum])
                    k.op(DVE, lambda e: e.tensor_copy(out=offs[:].rearrange("p t h -> p (t h)")[:, c0:c0 + n], in_=bkB[:, 0:n]),
                         r=[bkB], w=[offs])
                k.op(POOL, lambda e: e.memset(nl[:, 0, :], 0.0), w=[nl])
                for t in range(1, NT):
                    k.op(DVE, lambda e: e.tensor_tensor(out=nl[:, t, :], in0=nl[:, t - 1, :], in1=offs[:, t - 1, :], op=ALU.add),
                         r=[nl, offs], w=[nl])
                k.op(DVE, lambda e: e.tensor_tensor(out=ncum[:], in0=ncum[:], in1=nl[:], op=ALU.add), r=[ncum, nl], w=[ncum])
                k.op(POOL, lambda e: e.memset(ncum8[:, 0:64], 0.0), w=[ncum8])
                k.op(DVE, lambda e: e.tensor_scalar(out=ncum8[:, 64:64 + NT * 8], in0=ncum[:].rearrange("p t h -> p (t h)"),
                                                    scalar1=-8.0, scalar2=None, op0=ALU.mult), r=[ncum], w=[ncum8])
                k.barrier()
            QP, KP = QB2, ropeC
            VPv = ropeS[:].rearrange("p (t c) -> p t c", c=128)
            for pr in range(4):
                qk_proj(WS[0], pr * 128, 128, hT, hbufs, S, QP, QP, P_FQ, 64, False)
                qk_proj(WS[1], pr * 128, 128, hT, hbufs, S, KP, KP, P_FK, 64, False)
                for hh in range(2):
                    h = 2 * pr + hh
                    vlo, olo = (0, 64) if hh == 0 else (64, 0)
                    k.dma(SP, QB[0:64, :], QP[64 * hh:64 * hh + 64, :], r=[QP], w=[QB])
                    k.dma(SP, KB[0:64, :], KP[64 * hh:64 * hh + 64, :], r=[KP], w=[KB])
                    k.op(POOL, lambda e: e.memset(KB[64:65, :], 1.0), w=[KB])
                    for t0 in range(0, NT, 4):
                        bk = banks[6 + (t0 // 4) % 2]

                        def f(e):
                            for tt in range(4):
                                c = (t0 + tt) * 8 + h
                                i = e.matmul(out=bk[0:65, tt * 128:(tt + 1) * 128], lhsT=ncum8[:, c:c + 65], rhs=ident_b,
                                             start=True, stop=True)
                            return i
                        k.op(PE, f, r=[ncum8, cb], w=[bk])
                        k.op(DVE, lambda e: e.tensor_copy(out=QB[64:65, t0 * 128:(t0 + 4) * 128], in_=bk[64:65, 0:512]), r=[bk], w=[QB])
                    if hh == 0:
                        v_proj(WS[2], pr * 128, 128, hT, hbufs, NT, VPv, ropeS, None)
                    k.op(DVE, lambda e: e.tensor_copy(out=VB[:, :, vlo:vlo + 64], in_=VPv[:, :, hh * 64:(hh + 1) * 64]), r=[ropeS], w=[VB])
                    k.op(POOL, lambda e: e.memset(VB[:, :, olo:olo + 64], 1.0), w=[VB])
                    for qb in range(NB):
                        accb = banks[3 + (qb % 2)]
                        attn_fm(qb, 65, 0, KB, KB, QB, QB, lambda j: VB[:, j, 0:128], VB, NT, True, 0.125,
                                lambda j: ncum[:, j, h:h + 1], [ncum], accb, None)
                        flush_pv(0)
                        dsb = dacc[qb % 2]
                        dsh = dacc[2 + qb % 2]
                        nsb = tmpf[qb % 2]
                        k.op(DVE, lambda e: e.tensor_copy(out=dsb[olo:olo + 64, :], in_=accb[olo:olo + 64, :]), r=[accb], w=[dsb])
                        k.op(DVE, lambda e: e.tensor_copy(out=nsb[vlo:vlo + 64, :], in_=accb[vlo:vlo + 64, :]), r=[accb], w=[nsb])
                        k.dma(SP, dsh[vlo:vlo + 64, :], dsb[olo:olo + 64, :], r=[dsb], w=[dsh])
                        k.op(DVE, lambda e: e.reciprocal(out=dsh[vlo:vlo + 64, :], in_=dsh[vlo:vlo + 64, :]), r=[dsh], w=[dsh])
                        yt_ = ytb[uc % 2]
                        uc += 1
                        k.op(DVE, lambda e: e.tensor_tensor(out=yt_[vlo:vlo + 64, :], in0=nsb[vlo:vlo + 64, :],
                                                            in1=dsh[vlo:vlo + 64, :], op=ALU.mult), r=[nsb, dsh], w=[yt_])
                        k.dma(SP, yT_d[seq, 4 + pr, vlo:vlo + 64, qb * 512:(qb + 1) * 512], yt_[vlo:vlo + 64, :], r=[yt_], cw=[B_yT])

            load_w(WS[0], w_in_d[:, OFF_QC:OFF_QC + 512], KC, 512)
            load_w(WS[1], w_kv_d[:, 0:512], KC, 512)
            load_w(WS[2], w_kv_d[:, 512:1024], KC, 512)
            mbufs = [memT.sub(0)]
            for h in range(4):
                qk_proj(WS[1], h * 128, 128, memT, mbufs, MEM_LEN, memK[:, h, :], memK.sub(h), P_MK, 128, False)
            for h in range(4):
                v_proj(WS[2], h * 128, 128, memT, mbufs, MEM_LEN // 128, memV[:, :, h, :], memV.sub(h), None)
            for h in range(4):
                qk_proj(WS[0], h * 128, 128, hT, hbufs, S, QB, QB, P_MQ, 128, False)
                for qb in range(NB):
                    numb, denb = banks[3 + 2 * (qb % 2)], banks[4 + 2 * (qb % 2)]
                    attn_fm(qb, 128, 0, memK[:, h, :], memK.sub(h), QB, QB, lambda j: memV[:, j, h, 0:128], memV.sub(h),
                            MEM_LEN // 128, False, 128.0 ** -0.5, None, [], numb, denb)
                    flush_pv(0)
                    rd = tmpf[qb % 2]
                    yt_ = ytb[uc % 2]
                    uc += 1
                    k.op(DVE, lambda e: e.reciprocal(out=rd[:], in_=denb[:]), r=[denb], w=[rd])
                    k.op(DVE, lambda e: e.tensor_tensor(out=yt_[:], in0=numb[:], in1=rd[:], op=ALU.mult), r=[numb, rd], w=[yt_])
                    store_yT(yt_, 8 + h, qb)

            gctr = 0
            for grp in range(6):
                ws = WS[grp % 3]
                load_w(ws, w_in_d[:, OFF_G + grp * 512:OFF_G + (grp + 1) * 512], KC, 512)
                for c4 in range(4):
                    for tb in range(NB):
                        i = gctr % 2
                        gctr += 1
                        bk = banks[6 + i]
                        proj_fm(hT, hbufs[tb], ws, c4 * 128, 128, tb * 512, 512, bk)
                        k.op(ACT, lambda e: e.activation(out=qn[i][:], in_=bk[:], func=AF.Sigmoid), r=[bk], w=[qn[i]])
                        k.dma(SP, g_d[seq, grp * 4 + c4, :, tb * 512:(tb + 1) * 512], qn[i][:], r=[qn[i]], cw=[B_g])
            if dbg and "dbg_yT" in dbg_d and seq == 0:
                pass
            k.barrier()

        phase_merge(k, nc, seq, S, NT, NB, NSEQ, CAP, banks, cf, cb, rowb, wr, rt_d1, rt_d2, rt_w, cnt,
                    x_d, out_d, yT_d, g_d, xs_d, w_o_d, w_out_d, B_yT, B_g, B_xs, B_out, ident_f, load_w)

    phase_experts(k, nc, CAP, EB, banks, cb, ident_b, xs_d, ys_d, w_up_d, w_dn_d, B_xs, B_ys, load_w)
    phase_combine(k, nc, S, NT, NSEQ, CAP, rt_d1, rt_d2, rt_w, out_d, ys_d, B_ys, B_out)
    k.barrier([SP])
    k.es.close()
    return nc


def phase_merge(k, nc, seq, S, NT, NB, NSEQ, CAP, banks, cf, cb, rowb, wr, rt_d1, rt_d2, rt_w, cnt,
                x_d, out_d, yT_d, g_d, xs_d, w_o_d, w_out_d, B_yT, B_g, B_xs, B_out, ident_f, load_w):
    PE, ACT, DVE, POOL, SP = k.PE, k.ACT, k.DVE, k.POOL, k.SP
    NSLOT = N_EXP * CAP
    with ExitStack() as sc:
        Wo = [k.sb(sc, f"Wo{b}", [128, 4, D], BF16) for b in range(3)]
        Wout = k.sb(sc, "Wout", [128, KC, D], BF16)
        for b in range(3):
            load_w(Wo[b], w_o_d[b][:, :], 4, D)
        load_w(Wout, w_out_d[:, :], KC, D)
        ytb = [k.sb(sc, f"mytb{i}", [128, 12, 512], BF16) for i in range(2)]
        gts = [k.sb(sc, f"gts{i}", [128, 3, 512], BF16) for i in range(2)]
        mt = [k.sb(sc, f"mt{i}", [128, 512], F32) for i in range(3)]
        mT = [k.sb(sc, f"mT{i}", [128, KC, 512], BF16) for i in range(2)]
        xt = [k.sb(sc, f"mxt{i}", [128, D], F32) for i in range(4)]
        x1 = [k.sb(sc, f"x1{i}", [128, D], F32) for i in range(4)]
        h2f = [k.sb(sc, f"h2f{i}", [128, D], F32) for i in range(4)]
        h2b = [k.sb(sc, f"h2b{i}", [128, D], BF16) for i in range(10)]
        h2T = k.sb(sc, "h2T", [128, KC, 128], F32)
        junk = k.sb(sc, "mjunk", [128, D], BF16)
        st = [k.sb(sc, f"mst{i}", [128, 8], F32) for i in range(4)]
        lgs = [k.sb(sc, f"lg{i}", [128, 36], F32) for i in range(2)]
        sm = k.sb(sc, "sm", [128, 16], F32)
        m8 = k.sb(sc, "m8", [128, 8], F32)
        esel = k.sb(sc, "esel", [128, 8], F32)
        hot = k.sb(sc, "hot", [128, 2, 8], F32)
        e48 = k.sb(sc, "e48", [128, 4, 8], F32)
        M12 = k.sb(sc, "M12", [128, 2, 32], F32)
        Msum = k.sb(sc, "Msum", [128, 32], BF16)
        posb = k.sb(sc, "posb", [128, 32], F32)
        dsc = k.sb(sc, "dsc", [128, 2, 32], F32)
        dfl = k.sb(sc, "dfl", [128, 2], F32)
        lg4 = k.sb(sc, "lg4", [128, 4, 36], F32)
        sm4 = k.sb(sc, "sm4", [128, 5, 4], F32)
        gh4 = k.sb(sc, "gh4", [128, 4, 4], F32)
        ge4 = k.sb(sc, "ge4", [128, 4, 4], F32)
        e448 = k.sb(sc, "e448", [128, 4, 4, 8], F32)
        esel4 = k.sb(sc, "esel4", [128, 4, 8], F32)
        m84 = k.sb(sc, "m84", [128, 4, 8], F32)
        hot4 = k.sb(sc, "hot4", [128, 2, 4, 8], F32)
        M124 = k.sb(sc, "M124", [128, 2, 4, 32], F32)
        Msum4 = k.sb(sc, "Msum4", [128, 4, 32], BF16)
        pos4 = k.sb(sc, "pos4", [128, 4, 32], F32)
        dsc4 = k.sb(sc, "dsc4", [128, 2, 4, 32], F32)
        dfl4 = k.sb(sc, "dfl4", [128, 2, 4], F32)
        hb4 = [None] * 4
        zc = 0
        pend_back = []
        pendB = []
        for tb in range(NB):
            yb = ytb[tb % 2]
            k.dma(SP, yb[:], yT_d[seq, :, :, tb * 512:(tb + 1) * 512].rearrange("c p t -> p c t"), r=[B_yT], w=[yb])
            mTb = mT[tb % 2]
            for t in range(4):
                k.dma(SP, xt[t][:], x_d[seq, tb * 512 + t * 128:tb * 512 + (t + 1) * 128, :], w=[xt[t]])
            for j in range(KC):
                gt_ = gts[j % 2]
                k.dma(SP, gt_[:], g_d[seq].rearrange("(b j) p t -> p b j t", b=3)[:, :, j, tb * 512:(tb + 1) * 512], r=[B_g], w=[gt_])
                zb = [banks[3 * (zc % 2) + b] for b in range(3)]
                zc += 1
                for b in range(3):
                    def f(e, b=b):
                        for c in range(4):
                            i = e.matmul(out=zb[b][:], lhsT=Wo[b][:, c, j * 128:(j + 1) * 128], rhs=yb[:, b * 4 + c, :],
                                         start=(c == 0), stop=(c == 3))
                        return i
                    k.op(PE, f, r=[Wo[b], yb], w=[zb[b]])
                    k.op(DVE, lambda e: e.tensor_tensor(out=mt[b][:], in0=zb[b][:], in1=gt_[:, b, :], op=ALU.mult),
                         r=[zb[b], gt_], w=[mt[b]])
                k.op(DVE, lambda e: e.tensor_tensor(out=mt[0][:], in0=mt[0][:], in1=mt[1][:], op=ALU.add), r=[mt[0], mt[1]], w=[mt[0]])
                k.op(DVE, lambda e: e.tensor_tensor(out=mTb[:, j, :], in0=mt[0][:], in1=mt[2][:], op=ALU.add),
                     r=[mt[0], mt[2]], w=[mTb])
            if pend_back:
                pend_back.pop(0)()
            for t in range(4):
                gt = seq * NT + tb * 4 + t
                tok0 = tb * 512 + t * 128
                xt_, x1_, hf, hb, s_ = xt[t], x1[t], h2f[t], h2b[(tb * 4 + t) % 10], st[t]
                for half in range(2):
                    bk = banks[6 + half]

                    def f(e, half=half):
                        for c in range(KC):
                            i = e.matmul(out=bk[:], lhsT=mTb[:, c, t * 128:(t + 1) * 128], rhs=Wout[:, c, half * 512:(half + 1) * 512],
                                         start=(c == 0), stop=(c == KC - 1))
                        return i
                    k.op(PE, f, r=[mTb, Wout], w=[bk])
                    k.op(DVE, lambda e: e.tensor_tensor(out=x1_[:, half * 512:(half + 1) * 512], in0=bk[:],
                                                        in1=xt_[:, half * 512:(half + 1) * 512], op=ALU.add), r=[bk, xt_], w=[x1_])
                k.dma(ACT, out_d[seq, tok0:tok0 + 128, :], x1_[:], r=[x1_], cw=[B_out])
                k.op(ACT, lambda e: e.activation(out=junk[:], in_=x1_[:], func=AF.Square, accum_out=s_[:, 0:1]), r=[x1_], w=[junk, s_])
                k.op(ACT, lambda e: e.activation(out=s_[:, 1:2], in_=s_[:, 0:1], func=AF.Ln, scale=1.0 / D, bias=EPS), r=[s_], w=[s_])
                k.op(ACT, lambda e: e.activation(out=s_[:, 1:2], in_=s_[:, 1:2], func=AF.Exp, scale=-0.5), r=[s_], w=[s_])
            for t in range(4):
                x1_, hf, hb, s_ = x1[t], h2f[t], h2b[(tb * 4 + t) % 10], st[t]
                k.op(DVE, lambda e: e.scalar_tensor_tensor(out=hf[:], in0=x1_[:], scalar=s_[:, 1:2], in1=rowb[:, R_FG:R_FG + D],
                                                           op0=ALU.mult, op1=ALU.mult), r=[x1_, s_, rowb], w=[hf])
                k.op(ACT, lambda e: e.activation(out=hb[:], in_=hf[:], func=AF.Copy), r=[hf], w=[hb])
            for t in range(4):
                gt = seq * NT + tb * 4 + t
                hf, hb, s_ = h2f[t], h2b[(tb * 4 + t) % 10], st[t]
                lg = lgs[t % 2]

                def stageB(gt=gt, hb=hb, s_=s_, lg=lg, hf=hf):
                    for hh in range(2):
                        bk = banks[hh]

                        def tr(e, hh=hh):
                            for c in range(4):
                                cc = hh * 4 + c
                                i = e.transpose(out=bk[:, c * 128:(c + 1) * 128], in_=hf[:, cc * 128:(cc + 1) * 128], identity=ident_f)
                            return i
                        k.op(PE, tr, r=[hf, cf], w=[bk])
                        k.op(ACT, lambda e: e.activation(out=h2T[:, hh * 4:(hh + 1) * 4, :], in_=bk[:].rearrange("p (c t) -> p c t", c=4),
                                                         func=AF.Copy), r=[bk], w=[h2T])
                    bkl = banks[2]

                    def fl(e):
                        for c in range(KC):
                            i = e.matmul(out=bkl[:, 0:36], lhsT=h2T[:, c, :], rhs=wr[:, c, :], start=(c == 0), stop=(c == KC - 1))
                        return i
                    k.op(PE, fl, r=[h2T, wr], w=[bkl])
                    k.op(DVE, lambda e: e.tensor_tensor(out=lg[:], in0=bkl[:, 0:36], in1=rowb[:, R_BR:R_BR + 36], op=ALU.add),
                         r=[bkl, rowb], w=[lg])
                    k.op(DVE, lambda e: e.tensor_copy(out=lg4[:, gt % 4, :], in_=lg[:]), r=[lg], w=[lg4])
                    hb4[gt % 4] = hb
                    if gt % 4 != 3:
                        return
                    g0 = gt - 3
                    hbs = list(hb4)

                    def back(g0=g0, hbs=hbs):
                        GL = lg4[:, :, 0:4]
                        k.op(DVE, lambda e: e.reduce_max(out=sm4[:, 0, :], in_=GL, axis=AX.X), r=[lg4], w=[sm4])
                        k.op(DVE, lambda e: e.tensor_tensor(out=gh4[:], in0=GL, in1=sm4[:, 0, :].unsqueeze(2).to_broadcast([128, 4, 4]),
                                                            op=ALU.subtract), r=[lg4, sm4], w=[gh4])
                        k.op(ACT, lambda e: e.activation(out=ge4[:], in_=gh4[:], func=AF.Exp), r=[gh4], w=[ge4])
                        k.op(DVE, lambda e: e.reduce_sum(out=sm4[:, 1, :], in_=ge4[:], axis=AX.X), r=[ge4], w=[sm4])
                        k.op(DVE, lambda e: e.tensor_scalar(out=gh4[:], in0=gh4[:], scalar1=0.0, scalar2=None, op0=ALU.is_equal),
                             r=[gh4, ge4], w=[gh4])
                        k.op(DVE, lambda e: e.tensor_tensor(out=e448[:], in0=lg4[:, :, 4:36].rearrange("p t (g j) -> p t g j", g=4),
                                                            in1=gh4[:].unsqueeze(3).to_broadcast([128, 4, 4, 8]), op=ALU.mult),
                             r=[lg4, gh4], w=[e448])
                        k.op(DVE, lambda e: e.reduce_sum(out=esel4[:], in_=e448[:].rearrange("p t g j -> p t j g"), axis=AX.X),
                             r=[e448], w=[esel4])
                        for t_ in range(4):
                            k.op(DVE, lambda e: e.max(out=m84[:, t_, :], in_=esel4[:, t_, :]), r=[esel4], w=[m84])
                        for q in range(2):
                            k.op(DVE, lambda e: e.tensor_tensor(out=hot4[:, q, :, :], in0=esel4[:],
                                                                in1=m84[:, :, q:q + 1].to_broadcast([128, 4, 8]), op=ALU.is_equal),
                                 r=[esel4, m84], w=[hot4])
                        k.op(DVE, lambda e: e.tensor_tensor(out=sm4[:, 2, :], in0=m84[:, :, 1], in1=m84[:, :, 0], op=ALU.subtract),
                             r=[m84, sm4], w=[sm4])
                        k.op(ACT, lambda e: e.activation(out=sm4[:, 3, :], in_=sm4[:, 2, :], func=AF.Exp), r=[sm4], w=[sm4])
                        k.op(DVE, lambda e: e.scalar_tensor_tensor(out=sm4[:, 4, :], in0=sm4[:, 3, :], scalar=1.0, in1=sm4[:, 1, :],
                                                                   op0=ALU.add, op1=ALU.mult), r=[sm4], w=[sm4])
                        k.op(DVE, lambda e: e.reciprocal(out=rt_w[:, g0:g0 + 4, 0], in_=sm4[:, 4, :]), r=[sm4], w=[rt_w.sub(g0)])
                        k.op(DVE, lambda e: e.tensor_tensor(out=rt_w[:, g0:g0 + 4, 1], in0=rt_w[:, g0:g0 + 4, 0], in1=sm4[:, 3, :], op=ALU.mult),
                             r=[sm4, rt_w.sub(g0)], w=[rt_w.sub(g0)])
                        for q in range(2):
                            k.op(DVE, lambda e: e.tensor_tensor(out=M124[:, q, :, :].rearrange("p t (g j) -> p t g j", g=4),
                                                                in0=gh4[:].unsqueeze(3).to_broadcast([128, 4, 4, 8]),
                                                                in1=hot4[:, q, :, :].unsqueeze(2).to_broadcast([128, 4, 4, 8]), op=ALU.mult),
                                 r=[gh4, hot4], w=[M124])
                        k.op(DVE, lambda e: e.tensor_tensor(out=Msum4[:], in0=M124[:, 0, :, :], in1=M124[:, 1, :, :], op=ALU.add),
                             r=[M124], w=[Msum4])
                        bkp = banks[3]
                        MS = Msum4[:].rearrange("p t e -> p (t e)")

                        def fp(e):
                            e.matmul(out=bkp[:, 0:128], lhsT=cb[:, C_TLT:C_TLT + 128], rhs=MS, start=True, stop=True)
                            return e.matmul(out=bkp[:, 128:256], lhsT=cb[:, C_ONE:C_ONE + 128], rhs=MS, start=False, stop=True,
                                            skip_group_check=True)
                        k.op(PE, fp, r=[Msum4, cb], w=[bkp])
                        for t_ in range(4):
                            k.op(DVE, lambda e: e.tensor_tensor(out=pos4[:, t_, :], in0=bkp[:, t_ * 32:(t_ + 1) * 32], in1=cnt[:], op=ALU.add),
                                 r=[bkp, cnt], w=[pos4])
                            k.op(DVE, lambda e: e.tensor_tensor(out=cnt[:], in0=bkp[:, 128 + t_ * 32:128 + (t_ + 1) * 32], in1=cnt[:], op=ALU.add),
                                 r=[bkp, cnt], w=[cnt])
                        k.op(DVE, lambda e: e.tensor_scalar(out=pos4[:], in0=pos4[:], scalar1=float(CAP - 1), scalar2=None, op0=ALU.min),
                             r=[pos4], w=[pos4])
                        k.op(DVE, lambda e: e.tensor_tensor(out=pos4[:], in0=pos4[:], in1=cf[:, C_EB:C_EB + 32].unsqueeze(1).to_broadcast([128, 4, 32]),
                                                            op=ALU.add), r=[pos4, cf], w=[pos4])
                        for q in range(2):
                            k.op(DVE, lambda e: e.tensor_tensor(out=dsc4[:, q, :, :], in0=M124[:, q, :, :], in1=pos4[:], op=ALU.mult),
                                 r=[M124, pos4], w=[dsc4])
                        k.op(DVE, lambda e: e.reduce_sum(out=dfl4[:], in_=dsc4[:], axis=AX.X), r=[dsc4], w=[dfl4])
                        k.op(DVE, lambda e: e.tensor_copy(out=rt_d1[:, g0:g0 + 4], in_=dfl4[:, 0, :]), r=[dfl4], w=[rt_d1.sub(g0)])
                        k.op(DVE, lambda e: e.tensor_copy(out=rt_d2[:, g0:g0 + 4], in_=dfl4[:, 1, :]), r=[dfl4], w=[rt_d2.sub(g0)])
                        for t_ in range(4):
                            g_ = g0 + t_
                            k.dma(POOL, xs_d[:, :], hbs[t_][:], r=[hbs[t_], rt_d1.sub(g0)], cw=[B_xs], scatter_idx=rt_d1[:, g_:g_ + 1],
                                  bounds=NSLOT - 1)
                            k.dma(POOL, xs_d[:, :], hbs[t_][:], r=[hbs[t_], rt_d2.sub(g0)], cw=[B_xs], scatter_idx=rt_d2[:, g_:g_ + 1],
                                  bounds=NSLOT - 1)
                    pend_back.append(back)
                stageB()
        while pendB:
            pendB.pop(0)()
        while pend_back:
            pend_back.pop(0)()
        k.barrier()


def phase_experts(k, nc, CAP, EB, banks, cb, ident_b, xs_d, ys_d, w_up_d, w_dn_d, B_xs, B_ys, load_w):
    PE, ACT, DVE, POOL, SP = k.PE, k.ACT, k.DVE, k.POOL, k.SP
    NTB = EB // 128
    with ExitStack() as sc:
        wup = [k.sb(sc, f"wup{i}", [128, KC, 2 * FF], BF16) for i in range(3)]
        wdn = [k.sb(sc, f"wdn{i}", [128, 4, D], BF16) for i in range(3)]
        xr = [k.sb(sc, f"xr{i}", [128, NTB, D], BF16) for i in range(2)]
        xsT = [k.sb(sc, f"xsT{i}", [128, KC, EB], BF16) for i in range(2)]
        actT = [k.sb(sc, f"actT{i}", [128, 4, EB], BF16) for i in range(2)]
        sil = [k.sb(sc, f"sil{i}", [128, EB], F32) for i in range(2)]
        yt = [k.sb(sc, f"eyt{i}", [128, D], BF16) for i in range(2)]
        it = 0
        cc = 0
        blocks = [(ex, blk) for ex in range(N_EXP) for blk in range(CAP // EB)]

        def load_rows(i):
            ex_, blk_ = blocks[i]
            s0_ = ex_ * CAP + blk_ * EB
            k.dma(SP, xr[i % 2][:], xs_d[s0_:s0_ + EB, :].rearrange("(t p) d -> p t d", p=128), r=[B_xs], w=[xr[i % 2]])
        load_rows(0)
        for ex in range(N_EXP):
            wu, wd = wup[ex % 3], wdn[ex % 3]
            if ex == 0:
                for e0 in range(2):
                    load_w(wup[e0], w_up_d[e0], KC, 2 * FF)
                    load_w(wdn[e0], w_dn_d[e0], 4, D)
            if ex + 2 < N_EXP:
                load_w(wup[(ex + 2) % 3], w_up_d[ex + 2], KC, 2 * FF)
                load_w(wdn[(ex + 2) % 3], w_dn_d[ex + 2], 4, D)
            for blk in range(CAP // EB):
                s0 = ex * CAP + blk * EB
                xr_, xT, aT = xr[it % 2], xsT[it % 2], actT[it % 2]
                it += 1
                for t in range(NTB):
                    bk = banks[6 + t % 2]
                    bkb = bk.h.bitcast(BF16)

                    def tr(e, t=t):
                        for c in range(KC):
                            i = e.transpose(out=bkb[:, c * 128:(c + 1) * 128], in_=xr_[:, t, c * 128:(c + 1) * 128], identity=ident_b)
                        return i
                    k.op(PE, tr, r=[xr_, cb], w=[bk])
                    k.op(DVE, lambda e: e.tensor_copy(out=xT[:, :, t * 128:(t + 1) * 128],
                                                      in_=bkb[:, 0:1024].rearrange("p (c t) -> p c t", c=KC)), r=[bk], w=[xT])
                if it < len(blocks):
                    load_rows(it)
                for fc in range(4):
                    ba, bb = banks[(cc % 2) * 2], banks[(cc % 2) * 2 + 1]
                    cc += 1
                    for (bk, col0) in ((ba, fc * 128), (bb, FF + fc * 128)):
                        def f(e, bk=bk, col0=col0):
                            for c in range(KC):
                                i = e.matmul(out=bk[:, 0:EB], lhsT=wu[:, c, col0:col0 + 128], rhs=xT[:, c, :], start=(c == 0), stop=(c == KC - 1))
                            return i
                        k.op(PE, f, r=[wu, xT], w=[bk])
                    sl = sil[fc % 2]
                    k.op(ACT, lambda e: e.activation(out=sl[:], in_=ba[:, 0:EB], func=AF.Silu), r=[ba], w=[sl])
                    k.op(DVE, lambda e: e.tensor_tensor(out=aT[:, fc, :], in0=bb[:, 0:EB], in1=sl[:], op=ALU.mult), r=[bb, sl], w=[aT])
                for t in range(NTB):
                    y_ = yt[t % 2]
                    for half in range(2):
                        bk = banks[4 + half]

                        def f(e, half=half, t=t):
                            for c in range(4):
                                i = e.matmul(out=bk[:], lhsT=aT[:, c, t * 128:(t + 1) * 128], rhs=wd[:, c, half * 512:(half + 1) * 512],
                                             start=(c == 0), stop=(c == 3))
                            return i
                        k.op(PE, f, r=[aT, wd], w=[bk])
                        if half == 0:
                            k.op(ACT, lambda e: e.activation(out=y_[:, 0:512], in_=bk[:], func=AF.Copy), r=[bk], w=[y_])
                        else:
                            k.op(DVE, lambda e: e.tensor_copy(out=y_[:, 512:1024], in_=bk[:]), r=[bk], w=[y_])
                    k.dma(SP, ys_d[s0 + t * 128:s0 + (t + 1) * 128, :], y_[:], r=[y_], cw=[B_ys])
        k.barrier()


def phase_combine(k, nc, S, NT, NSEQ, CAP, rt_d1, rt_d2, rt_w, out_d, ys_d, B_ys, B_out):
    PE, ACT, DVE, POOL, SP = k.PE, k.ACT, k.DVE, k.POOL, k.SP
    NSLOT = N_EXP * CAP
    B_fin = Buf()
    with ExitStack() as sc:
        xt = [k.sb(sc, f"cxt{i}", [128, D], F32) for i in range(4)]
        y1 = [k.sb(sc, f"cy1{i}", [128, D], BF16) for i in range(4)]
        y2 = [k.sb(sc, f"cy2{i}", [128, D], BF16) for i in range(4)]
        o1 = [k.sb(sc, f"co1{i}", [128, D], F32) for i in range(4)]
        NTT_ = NSEQ * NT

        def fetch(g):
            sq_, t_ = divmod(g, NT)
            ii = g % 4
            k.dma(SP, xt[ii][:], out_d[sq_, t_ * 128:(t_ + 1) * 128, :], r=[B_out], w=[xt[ii]])
            k.dma(POOL, y1[ii][:], ys_d[:, :], r=[B_ys], w=[y1[ii]], gather_idx=rt_d1[:, g:g + 1], bounds=NSLOT - 1)
            k.dma(POOL, y2[ii][:], ys_d[:, :], r=[B_ys], w=[y2[ii]], gather_idx=rt_d2[:, g:g + 1], bounds=NSLOT - 1)
        for g in range(min(3, NTT_)):
            fetch(g)
        for gt in range(NTT_):
            seq, t = divmod(gt, NT)
            i = gt % 4
            if gt + 3 < NTT_:
                fetch(gt + 3)
            k.op(DVE, lambda e: e.scalar_tensor_tensor(out=o1[i][:], in0=y1[i][:], scalar=rt_w[:, gt, 0:1], in1=xt[i][:],
                                                       op0=ALU.mult, op1=ALU.add), r=[y1[i], xt[i], rt_w.sub(gt)], w=[o1[i]])
            k.op(DVE, lambda e: e.scalar_tensor_tensor(out=o1[i][:], in0=y2[i][:], scalar=rt_w[:, gt, 1:2], in1=o1[i][:],
                                                       op0=ALU.mult, op1=ALU.add), r=[y2[i], o1[i], rt_w.sub(gt)], w=[o1[i]])
            k.dma(ACT, out_d[seq, t * 128:(t + 1) * 128, :], o1[i][:], r=[o1[i]], cw=[B_fin])
        k.barrier()


def make_consts(CAP):
    c = np.zeros((128, NCONST), np.float32)
    p = np.arange(128)[:, None]
    q = np.arange(128)[None, :]
    c[:, C_ID:C_ID + 128] = (p == q)
    c[:, C_MASK:C_MASK + 128] = np.where(p > q, -30000.0, 0.0)
    c[:, C_TLE:C_TLE + 128] = (p <= q)
    c[:, C_TLT:C_TLT + 128] = (p < q)
    c[:, C_B64:C_B64 + 128] = (p // 64 == q // 64)
    rope = np.zeros((128, 128), np.float32)
    for base in (0, 64):
        for d in range(8):
            rope[base + d + 8, base + d] = -1.0
            rope[base + d, base + d + 8] = 1.0
    c[:, C_ROPE:C_ROPE + 128] = rope
    dd = np.arange(128) % 64
    inv = np.where(dd < 16, ROPE_THETA ** (-(2.0 * (dd % 8)) / 16.0), 0.0)
    c[:, C_INVF] = inv.astype(np.float32)
    c[:, C_ONE:C_ONE + 128] = 1.0
    c[:, C_EB:C_EB + 32] = (np.arange(32) * CAP)[None, :]
    return c


def make_inmaps(inputs, n_cores, NSEQ, CAP):
    f = lambda a: np.ascontiguousarray(np.asarray(a, dtype=np.float32))
    row = np.concatenate([f(inputs["attn_norm_g"])[0], f(inputs["mem_norm_g"])[0], f(inputs["ffn_norm_g"])[0],
                          f(inputs["b_router_group"])[0], f(inputs["b_router_expert"])[0], f(inputs["diff_subln_g"])[0],
                          f(inputs["fox_forget_b"])[0], f(inputs["diff_lambda"])[0].ravel()])
    assert row.shape[0] == NROWB
    rowb = np.ascontiguousarray(np.broadcast_to(row[None, :], (128, NROWB)))
    i64 = np.arange(128) % 64
    colp = np.stack([f(inputs["diff_qnorm_g"])[0][i64], f(inputs["diff_knorm_g"])[0][i64], f(inputs["fox_qnorm_g"])[0][i64],
                     f(inputs["fox_knorm_g"])[0][i64], f(inputs["mem_qnorm_g"])[0], f(inputs["mem_knorm_g"])[0],
                     f(inputs["diff_subln_g"])[0]], axis=1)
    colp = np.ascontiguousarray(colp.astype(np.float32))
    shared = {
        "consts": make_consts(CAP), "rowb": rowb, "colp": colp,
        "w_in": f(inputs["w_in"])[0], "w_mem_kv": f(inputs["w_mem_kv"])[0],
        "w_o_diff": f(inputs["w_o_diff"])[0], "w_o_fox": f(inputs["w_o_fox"])[0], "w_o_mem": f(inputs["w_o_mem"])[0],
        "w_out": f(inputs["w_out"])[0],
        "w_router": np.ascontiguousarray(np.concatenate([f(inputs["w_router_group"])[0], f(inputs["w_router_expert"])[0]], axis=1)),
        "w_up": f(inputs["w_up"])[0], "w_down": f(inputs["w_down"])[0],
    }
    x = f(inputs["x"])
    mem = f(inputs["mem"])
    pos = np.ascontiguousarray(np.asarray(inputs["positions"], dtype=np.int32))
    maps = []
    for c in range(n_cores):
        m = dict(shared)
        m["x"] = np.ascontiguousarray(x[c * NSEQ:(c + 1) * NSEQ])
        m["mem"] = np.ascontiguousarray(mem[c * NSEQ:(c + 1) * NSEQ])
        m["pos"] = np.ascontiguousarray(pos[c * NSEQ:(c + 1) * NSEQ])
        maps.append(m)
    return maps


def kernel(**inputs):
    B, S, _ = np.asarray(inputs["x"]).shape
    n_cores = 8
    NSEQ = B // n_cores
    CAP = 768
    nc = build_program(S, NSEQ, CAP)
    maps = make_inmaps(inputs, n_cores, NSEQ, CAP)
    res = run_bass_kernel_spmd(nc, maps, core_ids=list(range(n_cores)))
    return np.concatenate([np.asarray(r["out"]) for r in res.results], axis=0).astype(np.float32)
```

```python
import math
from contextlib import ExitStack

import numpy as np
import concourse.bass as bass
import concourse.mybir as mybir
from concourse.bass_utils import run_bass_kernel_spmd

F32 = mybir.dt.float32
BF16 = mybir.dt.bfloat16
I32 = mybir.dt.int32
AF = mybir.ActivationFunctionType
ALU = mybir.AluOpType
AX = mybir.AxisListType

D = 1024
KC = 8
MEM_LEN = 256
EPS = 1e-6
N_EXP = 32
FF = 512
IN_COLS = 6664
OFF_QA, OFF_KA, OFF_VA, OFF_QB, OFF_KB, OFF_VB, OFF_FB, OFF_QC, OFF_G = 0, 512, 1024, 1536, 2048, 2560, 3072, 3080, 3592
ROPE_THETA = 500000.0
LAMBDA_INIT = 0.8 - 0.6 * math.exp(0.0)

C_ID, C_MASK, C_TLE, C_TLT, C_B64, C_ROPE, C_INVF, C_ONE = 0, 128, 256, 384, 512, 640, 768, 769
C_EB = 769 + 128
NCONST = C_EB + 32
R_AG, R_MG, R_FG, R_BR, R_SUB, R_FB, R_LAM = 0, 1024, 2048, 3072, 3108, 3236, 3244
NROWB = 3244 + 256
P_DQ, P_DK, P_FQ, P_FK, P_MQ, P_MK, P_SUB = 0, 1, 2, 3, 4, 5, 6
NCOLP = 7

ND_SEMS = 20


class Buf:
    __slots__ = ("w", "cw", "r")

    def __init__(self):
        self.w = {}
        self.cw = {}
        self.r = {}


class T:
    def __init__(self, h):
        self.h = h
        self.b = Buf()
        self.subs = {}

    def __getitem__(self, k):
        return self.h[k]

    def sub(self, key):
        b = self.subs.get(key)
        if b is None:
            b = self.subs[key] = Buf()
        return b


def _b(x):
    return x.b if isinstance(x, T) else x


class Eng:
    def __init__(self, k, eng, name, skip_self=False):
        self.eng = eng
        self.name = name
        self.key = k.newsem(name)
        self.cnt = 0
        self.waited = {}
        self.skip_self = skip_self
        self.dkeys = None
        self.dn = 0


class K:
    def __init__(self, nc):
        self.nc = nc
        self.es = ExitStack()
        self.sems = []
        self.PE = Eng(self, nc.tensor, "pe", skip_self=True)
        self.ACT = Eng(self, nc.scalar, "act")
        self.DVE = Eng(self, nc.vector, "dve")
        self.POOL = Eng(self, nc.gpsimd, "pool")
        self.SP = Eng(self, nc.sync, "sp")
        self.engs = [self.PE, self.ACT, self.DVE, self.POOL, self.SP]
        for q in (self.SP, self.POOL):
            q.dkeys = [self.newsem(f"d{q.name}{i}") for i in range(ND_SEMS)]
        self.uid = 0

    def newsem(self, name):
        h = self.es.enter_context(self.nc.semaphore(name))
        self.sems.append(h)
        return len(self.sems) - 1

    def name(self, base):
        self.uid += 1
        return f"{base}_{self.uid}"

    def sb(self, sc, name, shape, dt):
        return T(sc.enter_context(self.nc.sbuf_tensor(self.name(name), list(shape), dt)))

    def ps(self, sc, name, shape, dt):
        return T(sc.enter_context(self.nc.psum_tensor(self.name(name), list(shape), dt)))

    def _wait(self, E, deps):
        for k, v in deps.items():
            if E.skip_self and k == E.key:
                continue
            if E.waited.get(k, 0) >= v:
                continue
            E.eng.wait_ge(self.sems[k], v)
            E.waited[k] = v

    @staticmethod
    def _deps(r, w, cw):
        d = {}

        def add(m):
            for k, v in m.items():
                if d.get(k, 0) < v:
                    d[k] = v

        for b in r:
            add(b.w)
            add(b.cw)
        for b in w:
            add(b.w)
            add(b.cw)
            add(b.r)
        for b in cw:
            add(b.w)
            add(b.r)
        return d

    @staticmethod
    def _record(ev, r, w, cw):
        k, v = ev
        for b in r:
            if b.r.get(k, 0) < v:
                b.r[k] = v
        for b in w:
            b.w = {k: v}
            b.cw = {}
            b.r = {}
        for b in cw:
            if b.cw.get(k, 0) < v:
                b.cw[k] = v

    def op(self, E, fn, r=(), w=(), cw=()):
        r = [_b(x) for x in r]
        w = [_b(x) for x in w]
        cw = [_b(x) for x in cw]
        self._wait(E, self._deps(r, w, cw))
        inst = fn(E.eng)
        E.cnt += 1
        inst.then_inc(self.sems[E.key], 1)
        self._record((E.key, E.cnt), r, w, cw)

    def dma(self, Q, out, in_, r=(), w=(), cw=(), scatter_idx=None, gather_idx=None, bounds=None, **kw):
        r = [_b(x) for x in r]
        w = [_b(x) for x in w]
        cw = [_b(x) for x in cw]
        self._wait(Q, self._deps(r, w, cw))
        i = Q.dn
        Q.dn += 1
        slot, use = i % ND_SEMS, i // ND_SEMS
        k = Q.dkeys[slot]
        if use > 0 and Q.waited.get(k, 0) < 16 * use:
            Q.eng.wait_ge(self.sems[k], 16 * use)
            Q.waited[k] = 16 * use
        if bounds is not None:
            if not hasattr(self, "_breg"):
                self._breg = {}
            if bounds not in self._breg:
                self._breg[bounds] = Q.eng.to_reg(bounds)
            bounds = self._breg[bounds]
        if scatter_idx is not None:
            inst = Q.eng.indirect_dma_start(out=out, out_offset=bass.IndirectOffsetOnAxis(ap=scatter_idx, axis=0),
                                            in_=in_, in_offset=None, bounds_check=bounds, oob_is_err=False)
        elif gather_idx is not None:
            inst = Q.eng.indirect_dma_start(out=out, out_offset=None, in_=in_,
                                            in_offset=bass.IndirectOffsetOnAxis(ap=gather_idx, axis=0),
                                            bounds_check=bounds, oob_is_err=False)
        else:
            inst = Q.eng.dma_start(out=out, in_=in_, **kw)
        inst.then_inc(self.sems[k], 16)
        self._record((k, 16 * (use + 1)), r, w, cw)

    def barrier(self, engs=None):
        tot = {}
        for E in self.engs:
            if E.cnt:
                tot[E.key] = E.cnt
            if E.dkeys:
                for s, k in enumerate(E.dkeys):
                    uses = (E.dn - s + ND_SEMS - 1) // ND_SEMS if E.dn > s else 0
                    if uses:
                        tot[k] = 16 * uses
        for E in (engs or self.engs):
            d = dict(tot)
            d.pop(E.key, None)
            sk = E.skip_self
            E.skip_self = False
            self._wait(E, d)
            E.skip_self = sk


def build_program(S, NSEQ, CAP, dbg=None):
    nc = bass.Bass("TRN2", target_bir_lowering=False)
    k = K(nc)
    k.es.enter_context(nc.allow_low_precision("bf16 operands / fp32 accumulation by design"))
    PE, ACT, DVE, POOL, SP = k.PE, k.ACT, k.DVE, k.POOL, k.SP
    NT = S // 128
    NB = S // 512
    NTT = NSEQ * NT
    NSLOT = N_EXP * CAP
    EB = 384 if CAP % 384 == 0 else 128
    dt = nc.dram_tensor

    x_d = dt("x", [NSEQ, S, D], F32, kind="ExternalInput").ap()
    mem_d = dt("mem", [NSEQ, MEM_LEN, D], F32, kind="ExternalInput").ap()
    pos_d = dt("pos", [NSEQ, S], I32, kind="ExternalInput").ap()
    consts_d = dt("consts", [128, NCONST], F32, kind="ExternalInput").ap()
    rowb_d = dt("rowb", [128, NROWB], F32, kind="ExternalInput").ap()
    colp_d = dt("colp", [128, NCOLP], F32, kind="ExternalInput").ap()
    w_in_d = dt("w_in", [D, IN_COLS], F32, kind="ExternalInput").ap()
    w_kv_d = dt("w_mem_kv", [D, 1024], F32, kind="ExternalInput").ap()
    w_o_d = [dt(n, [512, D], F32, kind="ExternalInput").ap() for n in ("w_o_diff", "w_o_fox", "w_o_mem")]
    w_out_d = dt("w_out", [D, D], F32, kind="ExternalInput").ap()
    w_r_d = dt("w_router", [D, 36], F32, kind="ExternalInput").ap()
    w_up_d = dt("w_up", [N_EXP, D, 2 * FF], F32, kind="ExternalInput").ap()
    w_dn_d = dt("w_down", [N_EXP, FF, D], F32, kind="ExternalInput").ap()
    out_d = dt("out", [NSEQ, S, D], F32, kind="ExternalOutput").ap()
    yT_d = dt("yT_scr", [NSEQ, 12, 128, S], BF16).ap()
    g_d = dt("g_scr", [NSEQ, 24, 128, S], BF16).ap()
    xs_d = dt("xs_scr", [NSLOT, D], BF16).ap()
    ys_d = dt("ys_scr", [NSLOT, D], BF16).ap()
    B_yT, B_g, B_xs, B_ys, B_out = Buf(), Buf(), Buf(), Buf(), Buf()
    dbg_d = {}
    if dbg:
        for n, (shape, dty) in dbg.items():
            dbg_d[n] = dt(n, list(shape), dty, kind="ExternalOutput").ap()

    top = k.es
    cf = k.sb(top, "cf", [128, NCONST], F32)
    cb = k.sb(top, "cb", [128, NCONST], BF16)
    rowb = k.sb(top, "rowb", [128, NROWB], F32)
    colp = k.sb(top, "colp", [128, NCOLP], F32)
    lam = k.sb(top, "lam", [128, 4], F32)
    wr = k.sb(top, "wr", [128, KC, 36], F32)
    rt_d1 = k.sb(top, "rt_d1", [128, NTT], I32)
    rt_d2 = k.sb(top, "rt_d2", [128, NTT], I32)
    rt_w = k.sb(top, "rt_w", [128, NTT, 2], F32)
    cnt = k.sb(top, "cnt", [128, N_EXP], F32)
    banks = [k.ps(top, f"bank{i}", [128, 512], F32) for i in range(8)]

    k.dma(SP, cf[:], consts_d[:, :], w=[cf])
    k.dma(SP, rowb[:], rowb_d[:, :], w=[rowb])
    k.dma(SP, colp[:], colp_d[:, :], w=[colp])
    k.dma(SP, wr[:], w_r_d.rearrange("(kc p) c -> p kc c", p=128), w=[wr])
    k.op(DVE, lambda e: e.tensor_copy(out=cb[:], in_=cf[:]), r=[cf], w=[cb])
    k.op(POOL, lambda e: e.memset(cnt[:], 0.0), w=[cnt])
    ident_b = cb[:, C_ID:C_ID + 128]
    ident_f = cf[:, C_ID:C_ID + 128]

    with ExitStack() as sc:
        t = k.sb(sc, "lt", [128, 2, 64], F32)
        s2 = k.sb(sc, "ls", [128, 2], F32)
        lp = rowb[:, R_LAM:R_LAM + 256].rearrange("p (a b c) -> p a b c", a=2, b=2)
        k.op(DVE, lambda e: e.tensor_tensor(out=t[:], in0=lp[:, :, 0, :], in1=lp[:, :, 1, :], op=ALU.mult), r=[rowb], w=[t])
        k.op(DVE, lambda e: e.reduce_sum(out=s2[:], in_=t[:], axis=AX.X), r=[t], w=[s2])
        k.op(ACT, lambda e: e.activation(out=s2[:], in_=s2[:], func=AF.Exp), r=[s2], w=[s2])
        k.op(DVE, lambda e: e.scalar_tensor_tensor(out=lam[:, 0:1], in0=s2[:, 1:2], scalar=-LAMBDA_INIT, in1=s2[:, 0:1],
                                                   op0=ALU.add, op1=ALU.subtract), r=[s2], w=[lam])
        k.op(DVE, lambda e: e.tensor_scalar(out=lam[:, 1:2], in0=colp[:, P_SUB:P_SUB + 1], scalar1=1.0 - LAMBDA_INIT,
                                            scalar2=None, op0=ALU.mult), r=[colp, lam], w=[lam])
        k.barrier()

    bank_rr = [0]

    def rms_rstd(E_ok, out_ap, in_ap, n, rbufs, wbufs):
        k.op(ACT, lambda e: e.activation(out=out_ap, in_=in_ap, func=AF.Ln, scale=1.0 / n, bias=EPS),
             r=rbufs, w=wbufs)
        k.op(ACT, lambda e: e.activation(out=out_ap, in_=out_ap, func=AF.Exp, scale=-0.5), r=wbufs, w=wbufs)

    def load_w(sc_t, src_ap, rows_kc, ncols):
        k.dma(POOL, sc_t[:, 0:rows_kc, 0:ncols], src_ap.rearrange("(kc p) c -> p kc c", p=128), w=[sc_t])

    sc_cache = {}

    def norm_tokens_to_T(sc, src_ap_fn, ntiles, gcol0, dstT, tag, every4=None):
        if "nb" not in sc_cache:
            sc_cache["nb"] = ([k.sb(sc, f"xt{i}", [128, D], F32) for i in range(4)],
                              [k.sb(sc, f"xn{i}", [128, D], BF16) for i in range(4)],
                              [k.sb(sc, f"st{i}", [128, 2], F32) for i in range(4)],
                              k.sb(sc, "junk", [128, D], BF16))
        xts, xn, st, junk = sc_cache["nb"]
        for t in range(ntiles):
            xt, xb, s_ = xts[t % 4], xn[t % 4], st[t % 4]
            if every4 is not None and t % 4 == 0:
                every4(t // 4)
            k.dma(SP, xt[:], src_ap_fn(t), w=[xt])
            k.op(ACT, lambda e: e.activation(out=junk[:], in_=xt[:], func=AF.Square, accum_out=s_[:, 0:1]), r=[xt], w=[junk, s_])
            rms_rstd(None, s_[:, 1:2], s_[:, 0:1], D, [s_], [s_])
            k.op(DVE, lambda e: e.scalar_tensor_tensor(out=xb[:], in0=xt[:], scalar=s_[:, 1:2], in1=rowb[:, gcol0:gcol0 + D],
                                                       op0=ALU.mult, op1=ALU.mult), r=[xt, s_, rowb], w=[xb])
            bk = banks[6 + (t % 2)]
            bkb = bk.h.bitcast(BF16)

            def tr(e):
                for c in range(KC):
                    i = e.transpose(out=bkb[:, c * 128:(c + 1) * 128], in_=xb[:, c * 128:(c + 1) * 128], identity=ident_b)
                return i
            k.op(PE, tr, r=[xb, cb], w=[bk])
            k.op(DVE, lambda e: e.tensor_copy(out=dstT[:, :, t * 128:(t + 1) * 128],
                                              in_=bkb[:, 0:1024].rearrange("p (c t) -> p c t", c=KC)), r=[bk], w=[dstT.sub(t // 4)])

    def proj_fm(hT, hbuf, ws, wcol0, M, tok0, ntok, bank):
        def f(e):
            for c in range(KC):
                i = e.matmul(out=bank[0:M, 0:ntok], lhsT=ws[:, c, wcol0:wcol0 + M], rhs=hT[:, c, tok0:tok0 + ntok],
                             start=(c == 0), stop=(c == KC - 1))
            return i
        k.op(PE, f, r=[hbuf, ws], w=[bank])

    for seq in range(NSEQ):
        with ExitStack() as sc:
            hT = k.sb(sc, "hT", [128, KC, S], BF16)
            memT = k.sb(sc, "memT", [128, KC, MEM_LEN], BF16)
            ropeC = k.sb(sc, "ropeC", [128, S], BF16)
            ropeS = k.sb(sc, "ropeS", [128, S], BF16)
            with ExitStack() as s1:
                sc_cache.clear()
                RW = 512
                posi = k.sb(s1, "posi", [128, RW], I32)
                ang = k.sb(s1, "ang", [128, RW], F32)
                kk = k.sb(s1, "kk", [128, RW], F32)
                ki = k.sb(s1, "ki", [128, RW], I32)
                ang2 = k.sb(s1, "ang2", [128, RW], F32)
                TWO_PI = 2.0 * math.pi

                def wrap(a, shift):
                    k.op(DVE, lambda e: e.tensor_scalar(out=kk[:], in0=a[:], scalar1=shift, scalar2=1.0 / TWO_PI,
                                                        op0=ALU.add, op1=ALU.mult), r=[a], w=[kk])
                    k.op(DVE, lambda e: e.tensor_copy(out=ki[:], in_=kk[:]), r=[kk], w=[ki])
                    k.op(DVE, lambda e: e.tensor_copy(out=kk[:], in_=ki[:]), r=[ki], w=[kk])
                    k.op(DVE, lambda e: e.scalar_tensor_tensor(out=kk[:], in0=kk[:], scalar=-TWO_PI, in1=a[:],
                                                               op0=ALU.mult, op1=ALU.add), r=[kk, a], w=[kk])
                    if shift != 0.0:
                        k.op(DVE, lambda e: e.tensor_scalar(out=kk[:], in0=kk[:], scalar1=shift, scalar2=None, op0=ALU.add),
                             r=[kk], w=[kk])
                    k.op(DVE, lambda e: e.tensor_scalar(out=a[:], in0=kk[:], scalar1=math.pi, scalar2=-TWO_PI,
                                                        op0=ALU.is_gt, op1=ALU.mult), r=[kk], w=[a])
                    k.op(DVE, lambda e: e.tensor_tensor(out=kk[:], in0=kk[:], in1=a[:], op=ALU.add), r=[kk, a], w=[kk])
                    k.op(DVE, lambda e: e.tensor_scalar(out=a[:], in0=kk[:], scalar1=-math.pi, scalar2=TWO_PI,
                                                        op0=ALU.is_lt, op1=ALU.mult), r=[kk], w=[a])
                    k.op(DVE, lambda e: e.tensor_tensor(out=a[:], in0=kk[:], in1=a[:], op=ALU.add), r=[kk, a], w=[a])
                    k.op(DVE, lambda e: e.tensor_scalar(out=a[:], in0=a[:], scalar1=3.14159, scalar2=-3.14159,
                                                        op0=ALU.min, op1=ALU.max), r=[a], w=[a])

                def rope_block(bi):
                    r0_ = bi * RW
                    k.dma(SP, posi[:], pos_d[seq:seq + 1, r0_:r0_ + RW].partition_broadcast(128), w=[posi])
                    k.op(DVE, lambda e: e.tensor_copy(out=ang[:], in_=posi[:]), r=[posi], w=[ang])
                    k.op(DVE, lambda e: e.tensor_scalar(out=ang[:], in0=ang[:], scalar1=cf[:, C_INVF:C_INVF + 1], scalar2=None,
                                                        op0=ALU.mult), r=[ang, cf], w=[ang])
                    wrap(ang, 0.0)
                    k.op(ACT, lambda e: e.activation(out=ropeS[:, r0_:r0_ + RW], in_=ang[:], func=AF.Sin), r=[ang], w=[ropeS])
                    k.op(ACT, lambda e: e.activation(out=ang2[:], in_=ang[:], func=AF.Abs), r=[ang], w=[ang2])
                    k.op(DVE, lambda e: e.tensor_scalar(out=ang2[:], in0=ang2[:], scalar1=-1.0, scalar2=math.pi / 2.0,
                                                        op0=ALU.mult, op1=ALU.add), r=[ang2], w=[ang2])
                    k.op(ACT, lambda e: e.activation(out=ropeC[:, r0_:r0_ + RW], in_=ang2[:], func=AF.Sin), r=[ang2], w=[ropeC])
                norm_tokens_to_T(s1, lambda t: x_d[seq, t * 128:(t + 1) * 128, :], NT, R_AG, hT, "x")
                norm_tokens_to_T(s1, lambda t: mem_d[seq, t * 128:(t + 1) * 128, :], MEM_LEN // 128, R_MG, memT, "m")
                for bi_ in range(S // RW):
                    rope_block(bi_)
                k.barrier()
            QB = k.sb(sc, "QB", [128, S], BF16)
            KB = k.sb(sc, "KB", [128, S], BF16)
            QB2 = k.sb(sc, "QB2", [128, S], BF16)
            dacc = [k.sb(sc, f"dacc{i}", [128, 512], F32) for i in range(4)]
            VB = k.sb(sc, "VB", [128, NT, 128], BF16)
            WS = [k.sb(sc, f"WS{i}", [128, KC, 512], BF16) for i in range(3)]
            PT = [k.sb(sc, f"PT{i}", [128, 512], BF16) for i in range(4)]
            sq = [k.sb(sc, f"sq{i}", [128, 512], BF16) for i in range(3)]
            rstd = [k.sb(sc, f"rstd{i}", [128, 512], F32) for i in range(3)]
            qn = [k.sb(sc, f"qn{i}", [128, 512], BF16) for i in range(3)]
            tmpf = [k.sb(sc, f"tmpf{i}", [128, 512], F32) for i in range(3)]
            ytb = [k.sb(sc, f"ytb{i}", [128, 512], BF16) for i in range(2)]
            rr = [k.sb(sc, f"rr{i}", [128, 2, 2, 2], F32) for i in range(4)]
            ncum = k.sb(sc, "ncum", [128, NT, 8], F32)
            ncum8 = k.sb(sc, "ncum8", [128, 64 + NT * 8], BF16)
            nl = k.sb(sc, "nl", [128, NT, 8], F32)
            offs = k.sb(sc, "offs", [128, NT, 8], F32)
            memK = k.sb(sc, "memK", [128, 4, MEM_LEN], BF16)
            memV = k.sb(sc, "memV", [128, 2, 4, 128], BF16)
            dtm = [k.sb(sc, f"dtm{i}", [128, 512], F32) for i in range(3)]

            hbufs = [hT.sub(i) for i in range((NT + 3) // 4)]

            def qk_proj(ws, wcol0, M, src, sbufs, ntok_total, dst, dbuf, gcolidx, blkn, rope, ctr=[0], split=None):
                for t0 in range(0, ntok_total, 512):
                    n = min(512, ntok_total - t0)
                    i = ctr[0] % 3
                    ctr[0] += 1
                    bk = banks[(6, 7, 3)[i]]
                    bk2 = banks[(4, 5, 2)[i]]
                    proj_fm(src, sbufs[t0 // 512] if len(sbufs) > 1 else sbufs[0], ws, wcol0, M, t0, n, bk)
                    k.op(ACT, lambda e: e.activation(out=sq[i][0:M, 0:n], in_=bk[0:M, 0:n], func=AF.Square), r=[bk], w=[sq[i]])
                    ones = cb[0:M, C_B64:C_B64 + M] if blkn == 64 else cb[0:M, C_ONE:C_ONE + M]
                    k.op(PE, lambda e: e.matmul(out=bk2[0:M, 0:n], lhsT=ones, rhs=sq[i][0:M, 0:n], start=True, stop=True),
                         r=[sq[i], cb], w=[bk2])
                    rms_rstd(None, rstd[i][0:M, 0:n], bk2[0:M, 0:n], blkn, [bk2], [rstd[i]])
                    out_ap = dst[0:M, t0:t0 + n] if not rope else qn[i][0:M, 0:n]
                    obuf = [dbuf] if not rope else [qn[i]]
                    k.op(DVE, lambda e: e.scalar_tensor_tensor(out=out_ap, in0=bk[0:M, 0:n], scalar=colp[0:M, gcolidx:gcolidx + 1],
                                                               in1=rstd[i][0:M, 0:n], op0=ALU.mult, op1=ALU.mult),
                         r=[bk, colp, rstd[i]], w=obuf)
                    if rope:
                        k.op(PE, lambda e: e.matmul(out=bk2[0:M, 0:n], lhsT=cb[0:M, C_ROPE:C_ROPE + M], rhs=qn[i][0:M, 0:n],
                                                    start=True, stop=True), r=[qn[i], cb], w=[bk2])
                        k.op(DVE, lambda e: e.tensor_tensor(out=tmpf[i][0:M, 0:n], in0=bk2[0:M, 0:n], in1=ropeS[0:M, t0:t0 + n],
                                                            op=ALU.mult), r=[bk2, ropeS], w=[tmpf[i]])
                        k.op(DVE, lambda e: e.tensor_tensor(out=rstd[i][0:M, 0:n], in0=qn[i][0:M, 0:n], in1=ropeC[0:M, t0:t0 + n],
                                                            op=ALU.mult), r=[qn[i], ropeC], w=[rstd[i]])
                        if split is None:
                            k.op(DVE, lambda e: e.tensor_tensor(out=dst[0:M, t0:t0 + n], in0=rstd[i][0:M, 0:n], in1=tmpf[i][0:M, 0:n],
                                                                op=ALU.add), r=[rstd[i], tmpf[i]], w=[dbuf])
                        else:
                            for (dd, lo) in ((dst, 0), (split, 64)):
                                k.op(DVE, lambda e: e.tensor_tensor(out=dd[lo:lo + 64, t0:t0 + n], in0=rstd[i][lo:lo + 64, 0:n],
                                                                     in1=tmpf[i][lo:lo + 64, 0:n], op=ALU.add),
                                     r=[rstd[i], tmpf[i]], w=[dd])

            def v_proj(ws, wcol0, dv, src, sbufs, ntiles, dstV, vbuf, ones_col, vctr=[0]):
                if ones_col is not None:
                    k.op(POOL, lambda e: e.memset(dstV[:, 0:ntiles, ones_col:ones_col + 1], 1.0), w=[vbuf])
                for t0 in range(0, ntiles, 4):
                    nt_ = min(4, ntiles - t0)
                    bk = banks[6 + vctr[0] % 2]
                    vctr[0] += 1

                    def f(e):
                        for tt in range(nt_):
                            for c in range(KC):
                                i = e.matmul(out=bk[:, tt * 128:tt * 128 + dv], lhsT=src[:, c, (t0 + tt) * 128:(t0 + tt + 1) * 128],
                                             rhs=ws[:, c, wcol0:wcol0 + dv], start=(c == 0), stop=(c == KC - 1))
                        return i
                    k.op(PE, f, r=[sbufs[min(t0 // 4, len(sbufs) - 1)], ws], w=[bk])
                    k.op(DVE, lambda e: e.tensor_copy(out=dstV[:, t0:t0 + nt_, 0:dv],
                                                      in_=bk[:, 0:nt_ * 128].rearrange("p (t c) -> p t c", c=128)[:, :, 0:dv]),
                         r=[bk], w=[vbuf])

            sctr = [0]
            pctr = [0]
            pend = []

            def flush_pv(keep=0):
                while len(pend) > keep:
                    pend.pop(0)()

            def attn_fm(qb, Kr, krow0, kT, kbuf, qT, qbuf, V, vbuf, nk_total, causal, scale, bias_fn, bias_bufs, numb, denb):
                q0 = qb * 512
                nk = 4 * qb + 4 if causal else nk_total
                for j in range(nk):
                    r_ = j - 4 * qb if causal else -1
                    c0 = 128 * r_ if r_ > 0 else 0
                    sbk = banks[sctr[0] % 3]
                    sctr[0] += 1
                    pt = PT[pctr[0] % 4]
                    pctr[0] += 1

                    def qk(e, j=j, r_=r_, c0=c0, sbk=sbk):
                        i = e.matmul(out=sbk[:, c0:512], lhsT=kT[krow0:krow0 + Kr, j * 128:(j + 1) * 128],
                                     rhs=qT[krow0:krow0 + Kr, q0 + c0:q0 + 512], start=True, stop=True)
                        if r_ >= 0:
                            i = e.matmul(out=sbk[:, c0:c0 + 128], lhsT=ident_b, rhs=cb[:, C_MASK:C_MASK + 128],
                                         start=False, stop=True, skip_group_check=True)
                        return i
                    k.op(PE, qk, r=[kbuf, qbuf, cb], w=[sbk])
                    if bias_fn is None:
                        k.op(ACT, lambda e: e.activation(out=pt[:, c0:512], in_=sbk[:, c0:512], func=AF.Exp, scale=scale),
                             r=[sbk], w=[pt])
                    else:
                        k.op(ACT, lambda e: e.activation(out=pt[:, c0:512], in_=sbk[:, c0:512], func=AF.Exp, scale=scale,
                                                         bias=bias_fn(j)), r=[sbk] + bias_bufs, w=[pt])

                    def pv(e, j=j, c0=c0, pt=pt, nk=nk):
                        i = e.matmul(out=numb[:, c0:512], lhsT=V(j), rhs=pt[:, c0:512], start=(j == 0), stop=(j == nk - 1),
                                     skip_group_check=True)
                        if denb is not None:
                            i = e.matmul(out=denb[:, c0:512], lhsT=cb[:, C_ONE:C_ONE + 128], rhs=pt[:, c0:512], start=(j == 0),
                                         stop=(j == nk - 1), skip_group_check=True)
                        return i
                    wb = [numb] + ([denb] if denb is not None else [])
                    pend.append(lambda pv=pv, pt=pt, wb=wb: k.op(PE, pv, r=[pt, vbuf, cb], w=wb))
                    flush_pv(2)

            def store_yT(ytile, chunk_idx, qb):
                k.dma(SP, yT_d[seq, chunk_idx, :, qb * 512:(qb + 1) * 512], ytile[:], r=[ytile], cw=[B_yT])

            load_w(WS[0], w_in_d[:, OFF_QA:OFF_QA + 512], KC, 512)
            load_w(WS[1], w_in_d[:, OFF_KA:OFF_KA + 512], KC, 512)
            load_w(WS[2], w_in_d[:, OFF_VA:OFF_VA + 512], KC, 512)
            uc = 0
            dtail = []
            k.op(POOL, lambda e: e.memset(QB[64:128, :], 0.0), w=[QB])
            k.op(POOL, lambda e: e.memset(QB2[0:64, :], 0.0), w=[QB2])
            for h in range(4):
                qk_proj(WS[0], h * 128, 128, hT, hbufs, S, QB, QB, P_DQ, 64, True, split=QB2)
                qk_proj(WS[1], h * 128, 128, hT, hbufs, S, KB, KB, P_DK, 64, True)
                v_proj(WS[2], h * 128, 128, hT, hbufs, NT, VB, VB, None)
                for qb in range(NB):
                    nb_ = [banks[3], banks[5]]
                    db_ = [banks[4], banks[6]]
                    for m in range(2):
                        qsel = QB if m == 0 else QB2
                        attn_fm(qb, 128, 0, KB, KB, qsel, qsel, lambda j: VB[:, j, 0:128], VB, NT, True, 0.125, None, [],
                                nb_[m], db_[m])
                    flush_pv(0)
                    if dtail:
                        dtail.pop(0)()
                    t0_, t1_ = dtm[uc % 2], dtm[2]
                    yt_ = ytb[uc % 2]
                    rs_ = rstd[uc % 2]
                    sq_ = sq[uc % 2]
                    uc += 1
                    k.op(DVE, lambda e: e.tensor_copy(out=dacc[0][:], in_=nb_[0][:]), r=[nb_[0]], w=[dacc[0]])
                    k.op(DVE, lambda e: e.tensor_copy(out=dacc[1][:], in_=db_[0][:]), r=[db_[0]], w=[dacc[1]])
                    k.op(DVE, lambda e: e.tensor_copy(out=dacc[2][:], in_=nb_[1][:]), r=[nb_[1]], w=[dacc[2]])
                    k.op(DVE, lambda e: e.tensor_copy(out=dacc[3][:], in_=db_[1][:]), r=[db_[1]], w=[dacc[3]])
                    k.op(DVE, lambda e: e.reciprocal(out=dacc[1][:], in_=dacc[1][:]), r=[dacc[1]], w=[dacc[1]])
                    k.op(DVE, lambda e: e.tensor_tensor(out=t0_[:], in0=dacc[0][:], in1=dacc[1][:], op=ALU.mult), r=[dacc[0], dacc[1]], w=[t0_])
                    k.op(DVE, lambda e: e.reciprocal(out=dacc[3][:], in_=dacc[3][:]), r=[dacc[3]], w=[dacc[3]])
                    k.op(DVE, lambda e: e.tensor_tensor(out=t1_[:], in0=dacc[2][:], in1=dacc[3][:], op=ALU.mult), r=[dacc[2], dacc[3]], w=[t1_])
                    k.op(DVE, lambda e: e.scalar_tensor_tensor(out=t0_[:], in0=t1_[:], scalar=lam[:, 0:1], in1=t0_[:],
                                                               op0=ALU.mult, op1=ALU.add), r=[t1_, t0_, lam], w=[t0_])
                    k.op(POOL, lambda e: e.tensor_tensor(out=sq_[:], in0=t0_[:], in1=t0_[:], op=ALU.mult), r=[t0_], w=[sq_])

                    def tail(t0_=t0_, yt_=yt_, rs_=rs_, sq_=sq_, h=h, qb=qb):
                        bk7 = banks[7]
                        k.op(PE, lambda e: e.matmul(out=bk7[:], lhsT=cb[:, C_ONE:C_ONE + 128], rhs=sq_[:], start=True, stop=True),
                             r=[sq_, cb], w=[bk7])
                        rms_rstd(None, rs_[:], bk7[:], 128, [bk7], [rs_])
                        k.op(DVE, lambda e: e.scalar_tensor_tensor(out=yt_[:], in0=t0_[:], scalar=lam[:, 1:2], in1=rs_[:],
                                                                   op0=ALU.mult, op1=ALU.mult), r=[t0_, rs_, lam], w=[yt_])
                        store_yT(yt_, 0 + h, qb)
                    dtail.append(tail)
                while dtail:
                    dtail.pop(0)()

            load_w(WS[0], w_in_d[:, OFF_QB:OFF_QB + 512], KC, 512)
            load_w(WS[1], w_in_d[:, OFF_KB:OFF_KB + 512], KC, 512)
            load_w(WS[2], w_in_d[:, OFF_VB:OFF_VB + 512], KC, 512)
            with ExitStack() as s2:
                wfb = k.sb(s2, "wfb", [128, KC, 8], BF16)
                load_w(wfb, w_in_d[:, OFF_FB:OFF_FB + 8], KC, 8)
                for t in range(NT):
                    bk = banks[6 + t % 2]

                    def f(e):
                        for c in range(KC):
                            i = e.matmul(out=bk[:, 0:8], lhsT=hT[:, c, t * 128:(t + 1) * 128], rhs=wfb[:, c, :],
                                         start=(c == 0), stop=(c == KC - 1))
                        return i
                    k.op(PE, f, r=[hbufs[t // 4], wfb], w=[bk])
                    k.op(DVE, lambda e: e.tensor_tensor(out=nl[:, t, :], in0=bk[:, 0:8], in1=rowb[:, R_FB:R_FB + 8], op=ALU.add),
                         r=[bk, rowb], w=[nl.sub(t)])
                nlb = [nl.sub(t) for t in range(NT)]
                k.op(ACT, lambda e: e.activation(out=nl[:], in_=nl[:], func=AF.Exp, scale=-1.0), r=nlb, w=[nl] + nlb)
                k.op(ACT, lambda e: e.activation(out=nl[:], in_=nl[:], func=AF.Ln, bias=1.0), r=[nl], w=[nl])
                nlf = nl[:].rearrange("p t h -> p (t h)")
                bkA, bkB = banks[6], banks[7]
                for c0 in range(0, NT * 8, 512):
                    n = min(512, NT * 8 - c0)
                    k.op(PE, lambda e: e.matmul(out=bkA[:, 0:n], lhsT=cf[:, C_TLE:C_TLE + 128], rhs=nlf[:, c0:c0 + n], start=True, stop=True),
                         r=[nl, cf], w=[bkA])
                    k.op(PE, lambda e: e.matmul(out=bkB[:, 0:n], lhsT=cf[:, C_ONE:C_ONE + 128], rhs=nlf[:, c0:c0 + n], start=True, stop=True),
                         r=[nl, cf], w=[bkB])
                    k.op(DVE, lambda e: e.tensor_copy(out=ncum[:].rearrange("p t h -> p (t h)")[:, c0:c0 + n], in_=bkA[:, 0:n]),
                         r=[bkA], w=[ncum])
                    k.op(DVE, lambda e: e.tensor_copy(out=offs[:].rearrange("p t h -> p (t h)")[:, c0:c0 + n], in_=bkB[:, 0:n]),
                         r=[bkB], w=[offs])
                k.op(POOL, lambda e: e.memset(nl[:, 0, :], 0.0), w=[nl])
                for t in range(1, NT):
                    k.op(DVE, lambda e: e.tensor_tensor(out=nl[:, t, :], in0=nl[:, t - 1, :], in1=offs[:, t - 1, :], op=ALU.add),
                         r=[nl, offs], w=[nl])
                k.op(DVE, lambda e: e.tensor_tensor(out=ncum[:], in0=ncum[:], in1=nl[:], op=ALU.add), r=[ncum, nl], w=[ncum])
                k.op(POOL, lambda e: e.memset(ncum8[:, 0:64], 0.0), w=[ncum8])
                k.op(DVE, lambda e: e.tensor_scalar(out=ncum8[:, 64:64 + NT * 8], in0=ncum[:].rearrange("p t h -> p (t h)"),
                                                    scalar1=-8.0, scalar2=None, op0=ALU.mult), r=[ncum], w=[ncum8])
                k.barrier()
            QP, KP = QB2, ropeC
            VPv = ropeS[:].rearrange("p (t c) -> p t c", c=128)
            for pr in range(4):
                qk_proj(WS[0], pr * 128, 128, hT, hbufs, S, QP, QP, P_FQ, 64, False)
                qk_proj(WS[1], pr * 128, 128, hT, hbufs, S, KP, KP, P_FK, 64, False)
                for hh in range(2):
                    h = 2 * pr + hh
                    vlo, olo = (0, 64) if hh == 0 else (64, 0)
                    k.dma(SP, QB[0:64, :], QP[64 * hh:64 * hh + 64, :], r=[QP], w=[QB])
                    k.dma(SP, KB[0:64, :], KP[64 * hh:64 * hh + 64, :], r=[KP], w=[KB])
                    k.op(POOL, lambda e: e.memset(KB[64:65, :], 1.0), w=[KB])
                    for t0 in range(0, NT, 4):
                        bk = banks[6 + (t0 // 4) % 2]

                        def f(e):
                            for tt in range(4):
                                c = (t0 + tt) * 8 + h
                                i = e.matmul(out=bk[0:65, tt * 128:(tt + 1) * 128], lhsT=ncum8[:, c:c + 65], rhs=ident_b,
                                             start=True, stop=True)
                            return i
                        k.op(PE, f, r=[ncum8, cb], w=[bk])
                        k.op(DVE, lambda e: e.tensor_copy(out=QB[64:65, t0 * 128:(t0 + 4) * 128], in_=bk[64:65, 0:512]), r=[bk], w=[QB])
                    if hh == 0:
                        v_proj(WS[2], pr * 128, 128, hT, hbufs, NT, VPv, ropeS, None)
                    k.op(DVE, lambda e: e.tensor_copy(out=VB[:, :, vlo:vlo + 64], in_=VPv[:, :, hh * 64:(hh + 1) * 64]), r=[ropeS], w=[VB])
                    k.op(POOL, lambda e: e.memset(VB[:, :, olo:olo + 64], 1.0), w=[VB])
                    for qb in range(NB):
                        accb = banks[3 + (qb % 2)]
                        attn_fm(qb, 65, 0, KB, KB, QB, QB, lambda j: VB[:, j, 0:128], VB, NT, True, 0.125,
                                lambda j: ncum[:, j, h:h + 1], [ncum], accb, None)
                        flush_pv(0)
                        dsb = dacc[qb % 2]
                        dsh = dacc[2 + qb % 2]
                        nsb = tmpf[qb % 2]
                        k.op(DVE, lambda e: e.tensor_copy(out=dsb[olo:olo + 64, :], in_=accb[olo:olo + 64, :]), r=[accb], w=[dsb])
                        k.op(DVE, lambda e: e.tensor_copy(out=nsb[vlo:vlo + 64, :], in_=accb[vlo:vlo + 64, :]), r=[accb], w=[nsb])
                        k.dma(SP, dsh[vlo:vlo + 64, :], dsb[olo:olo + 64, :], r=[dsb], w=[dsh])
                        k.op(DVE, lambda e: e.reciprocal(out=dsh[vlo:vlo + 64, :], in_=dsh[vlo:vlo + 64, :]), r=[dsh], w=[dsh])
                        yt_ = ytb[uc % 2]
                        uc += 1
                        k.op(DVE, lambda e: e.tensor_tensor(out=yt_[vlo:vlo + 64, :], in0=nsb[vlo:vlo + 64, :],
                                                            in1=dsh[vlo:vlo + 64, :], op=ALU.mult), r=[nsb, dsh], w=[yt_])
                        k.dma(SP, yT_d[seq, 4 + pr, vlo:vlo + 64, qb * 512:(qb + 1) * 512], yt_[vlo:vlo + 64, :], r=[yt_], cw=[B_yT])

            load_w(WS[0], w_in_d[:, OFF_QC:OFF_QC + 512], KC, 512)
            load_w(WS[1], w_kv_d[:, 0:512], KC, 512)
            load_w(WS[2], w_kv_d[:, 512:1024], KC, 512)
            mbufs = [memT.sub(0)]
            for h in range(4):
                qk_proj(WS[1], h * 128, 128, memT, mbufs, MEM_LEN, memK[:, h, :], memK.sub(h), P_MK, 128, False)
            for h in range(4):
                v_proj(WS[2], h * 128, 128, memT, mbufs, MEM_LEN // 128, memV[:, :, h, :], memV.sub(h), None)
            for h in range(4):
                qk_proj(WS[0], h * 128, 128, hT, hbufs, S, QB, QB, P_MQ, 128, False)
                for qb in range(NB):
                    numb, denb = banks[3 + 2 * (qb % 2)], banks[4 + 2 * (qb % 2)]
                    attn_fm(qb, 128, 0, memK[:, h, :], memK.sub(h), QB, QB, lambda j: memV[:, j, h, 0:128], memV.sub(h),
                            MEM_LEN // 128, False, 128.0 ** -0.5, None, [], numb, denb)
                    flush_pv(0)
                    rd = tmpf[qb % 2]
                    yt_ = ytb[uc % 2]
                    uc += 1
                    k.op(DVE, lambda e: e.reciprocal(out=rd[:], in_=denb[:]), r=[denb], w=[rd])
                    k.op(DVE, lambda e: e.tensor_tensor(out=yt_[:], in0=numb[:], in1=rd[:], op=ALU.mult), r=[numb, rd], w=[yt_])
                    store_yT(yt_, 8 + h, qb)

            gctr = 0
            for grp in range(6):
                ws = WS[grp % 3]
                load_w(ws, w_in_d[:, OFF_G + grp * 512:OFF_G + (grp + 1) * 512], KC, 512)
                for c4 in range(4):
                    for tb in range(NB):
                        i = gctr % 2
                        gctr += 1
                        bk = banks[6 + i]
                        proj_fm(hT, hbufs[tb], ws, c4 * 128, 128, tb * 512, 512, bk)
                        k.op(ACT, lambda e: e.activation(out=qn[i][:], in_=bk[:], func=AF.Sigmoid), r=[bk], w=[qn[i]])
                        k.dma(SP, g_d[seq, grp * 4 + c4, :, tb * 512:(tb + 1) * 512], qn[i][:], r=[qn[i]], cw=[B_g])
            if dbg and "dbg_yT" in dbg_d and seq == 0:
                pass
            k.barrier()

        phase_merge(k, nc, seq, S, NT, NB, NSEQ, CAP, banks, cf, cb, rowb, wr, rt_d1, rt_d2, rt_w, cnt,
                    x_d, out_d, yT_d, g_d, xs_d, w_o_d, w_out_d, B_yT, B_g, B_xs, B_out, ident_f, load_w)

    phase_experts(k, nc, CAP, EB, banks, cb, ident_b, xs_d, ys_d, w_up_d, w_dn_d, B_xs, B_ys, load_w)
    phase_combine(k, nc, S, NT, NSEQ, CAP, rt_d1, rt_d2, rt_w, out_d, ys_d, B_ys, B_out)
    k.barrier([SP])
    k.es.close()
    return nc


def phase_merge(k, nc, seq, S, NT, NB, NSEQ, CAP, banks, cf, cb, rowb, wr, rt_d1, rt_d2, rt_w, cnt,
                x_d, out_d, yT_d, g_d, xs_d, w_o_d, w_out_d, B_yT, B_g, B_xs, B_out, ident_f, load_w):
    PE, ACT, DVE, POOL, SP = k.PE, k.ACT, k.DVE, k.POOL, k.SP
    NSLOT = N_EXP * CAP
    with ExitStack() as sc:
        Wo = [k.sb(sc, f"Wo{b}", [128, 4, D], BF16) for b in range(3)]
        Wout = k.sb(sc, "Wout", [128, KC, D], BF16)
        for b in range(3):
            load_w(Wo[b], w_o_d[b][:, :], 4, D)
        load_w(Wout, w_out_d[:, :], KC, D)
        ytb = [k.sb(sc, f"mytb{i}", [128, 12, 512], BF16) for i in range(2)]
        gts = [k.sb(sc, f"gts{i}", [128, 3, 512], BF16) for i in range(2)]
        mt = [k.sb(sc, f"mt{i}", [128, 512], F32) for i in range(3)]
        mT = [k.sb(sc, f"mT{i}", [128, KC, 512], BF16) for i in range(2)]
        xt = [k.sb(sc, f"mxt{i}", [128, D], F32) for i in range(4)]
        x1 = [k.sb(sc, f"x1{i}", [128, D], F32) for i in range(4)]
        h2f = [k.sb(sc, f"h2f{i}", [128, D], F32) for i in range(4)]
        h2b = [k.sb(sc, f"h2b{i}", [128, D], BF16) for i in range(10)]
        h2T = k.sb(sc, "h2T", [128, KC, 128], F32)
        junk = k.sb(sc, "mjunk", [128, D], BF16)
        st = [k.sb(sc, f"mst{i}", [128, 8], F32) for i in range(4)]
        lgs = [k.sb(sc, f"lg{i}", [128, 36], F32) for i in range(2)]
        sm = k.sb(sc, "sm", [128, 16], F32)
        m8 = k.sb(sc, "m8", [128, 8], F32)
        esel = k.sb(sc, "esel", [128, 8], F32)
        hot = k.sb(sc, "hot", [128, 2, 8], F32)
        e48 = k.sb(sc, "e48", [128, 4, 8], F32)
        M12 = k.sb(sc, "M12", [128, 2, 32], F32)
        Msum = k.sb(sc, "Msum", [128, 32], BF16)
        posb = k.sb(sc, "posb", [128, 32], F32)
        dsc = k.sb(sc, "dsc", [128, 2, 32], F32)
        dfl = k.sb(sc, "dfl", [128, 2], F32)
        lg4 = k.sb(sc, "lg4", [128, 4, 36], F32)
        sm4 = k.sb(sc, "sm4", [128, 5, 4], F32)
        gh4 = k.sb(sc, "gh4", [128, 4, 4], F32)
        ge4 = k.sb(sc, "ge4", [128, 4, 4], F32)
        e448 = k.sb(sc, "e448", [128, 4, 4, 8], F32)
        esel4 = k.sb(sc, "esel4", [128, 4, 8], F32)
        m84 = k.sb(sc, "m84", [128, 4, 8], F32)
        hot4 = k.sb(sc, "hot4", [128, 2, 4, 8], F32)
        M124 = k.sb(sc, "M124", [128, 2, 4, 32], F32)
        Msum4 = k.sb(sc, "Msum4", [128, 4, 32], BF16)
        pos4 = k.sb(sc, "pos4", [128, 4, 32], F32)
        dsc4 = k.sb(sc, "dsc4", [128, 2, 4, 32], F32)
        dfl4 = k.sb(sc, "dfl4", [128, 2, 4], F32)
        hb4 = [None] * 4
        zc = 0
        pend_back = []
        pendB = []
        for tb in range(NB):
            yb = ytb[tb % 2]
            k.dma(SP, yb[:], yT_d[seq, :, :, tb * 512:(tb + 1) * 512].rearrange("c p t -> p c t"), r=[B_yT], w=[yb])
            mTb = mT[tb % 2]
            for t in range(4):
                k.dma(SP, xt[t][:], x_d[seq, tb * 512 + t * 128:tb * 512 + (t + 1) * 128, :], w=[xt[t]])
            for j in range(KC):
                gt_ = gts[j % 2]
                k.dma(SP, gt_[:], g_d[seq].rearrange("(b j) p t -> p b j t", b=3)[:, :, j, tb * 512:(tb + 1) * 512], r=[B_g], w=[gt_])
                zb = [banks[3 * (zc % 2) + b] for b in range(3)]
                zc += 1
                for b in range(3):
                    def f(e, b=b):
                        for c in range(4):
                            i = e.matmul(out=zb[b][:], lhsT=Wo[b][:, c, j * 128:(j + 1) * 128], rhs=yb[:, b * 4 + c, :],
                                         start=(c == 0), stop=(c == 3))
                        return i
                    k.op(PE, f, r=[Wo[b], yb], w=[zb[b]])
                    k.op(DVE, lambda e: e.tensor_tensor(out=mt[b][:], in0=zb[b][:], in1=gt_[:, b, :], op=ALU.mult),
                         r=[zb[b], gt_], w=[mt[b]])
                k.op(DVE, lambda e: e.tensor_tensor(out=mt[0][:], in0=mt[0][:], in1=mt[1][:], op=ALU.add), r=[mt[0], mt[1]], w=[mt[0]])
                k.op(DVE, lambda e: e.tensor_tensor(out=mTb[:, j, :], in0=mt[0][:], in1=mt[2][:], op=ALU.add),
                     r=[mt[0], mt[2]], w=[mTb])
            if pend_back:
                pend_back.pop(0)()
            for t in range(4):
                gt = seq * NT + tb * 4 + t
                tok0 = tb * 512 + t * 128
                xt_, x1_, hf, hb, s_ = xt[t], x1[t], h2f[t], h2b[(tb * 4 + t) % 10], st[t]
                for half in range(2):
                    bk = banks[6 + half]

                    def f(e, half=half):
                        for c in range(KC):
                            i = e.matmul(out=bk[:], lhsT=mTb[:, c, t * 128:(t + 1) * 128], rhs=Wout[:, c, half * 512:(half + 1) * 512],
                                         start=(c == 0), stop=(c == KC - 1))
                        return i
                    k.op(PE, f, r=[mTb, Wout], w=[bk])
                    k.op(DVE, lambda e: e.tensor_tensor(out=x1_[:, half * 512:(half + 1) * 512], in0=bk[:],
                                                        in1=xt_[:, half * 512:(half + 1) * 512], op=ALU.add), r=[bk, xt_], w=[x1_])
                k.dma(SP, out_d[seq, tok0:tok0 + 128, :], x1_[:], r=[x1_], cw=[B_out])
                k.op(ACT, lambda e: e.activation(out=junk[:], in_=x1_[:], func=AF.Square, accum_out=s_[:, 0:1]), r=[x1_], w=[junk, s_])
                k.op(ACT, lambda e: e.activation(out=s_[:, 1:2], in_=s_[:, 0:1], func=AF.Ln, scale=1.0 / D, bias=EPS), r=[s_], w=[s_])
                k.op(ACT, lambda e: e.activation(out=s_[:, 1:2], in_=s_[:, 1:2], func=AF.Exp, scale=-0.5), r=[s_], w=[s_])
            for t in range(4):
                x1_, hf, hb, s_ = x1[t], h2f[t], h2b[(tb * 4 + t) % 10], st[t]
                k.op(DVE, lambda e: e.scalar_tensor_tensor(out=hf[:], in0=x1_[:], scalar=s_[:, 1:2], in1=rowb[:, R_FG:R_FG + D],
                                                           op0=ALU.mult, op1=ALU.mult), r=[x1_, s_, rowb], w=[hf])
                k.op(ACT, lambda e: e.activation(out=hb[:], in_=hf[:], func=AF.Copy), r=[hf], w=[hb])
            for t in range(4):
                gt = seq * NT + tb * 4 + t
                hf, hb, s_ = h2f[t], h2b[(tb * 4 + t) % 10], st[t]
                lg = lgs[t % 2]

                def stageB(gt=gt, hb=hb, s_=s_, lg=lg, hf=hf):
                    for hh in range(2):
                        bk = banks[hh]

                        def tr(e, hh=hh):
                            for c in range(4):
                                cc = hh * 4 + c
                                i = e.transpose(out=bk[:, c * 128:(c + 1) * 128], in_=hf[:, cc * 128:(cc + 1) * 128], identity=ident_f)
                            return i
                        k.op(PE, tr, r=[hf, cf], w=[bk])
                        k.op(ACT, lambda e: e.activation(out=h2T[:, hh * 4:(hh + 1) * 4, :], in_=bk[:].rearrange("p (c t) -> p c t", c=4),
                                                         func=AF.Copy), r=[bk], w=[h2T])
                    bkl = banks[2]

                    def fl(e):
                        for c in range(KC):
                            i = e.matmul(out=bkl[:, 0:36], lhsT=h2T[:, c, :], rhs=wr[:, c, :], start=(c == 0), stop=(c == KC - 1))
                        return i
                    k.op(PE, fl, r=[h2T, wr], w=[bkl])
                    k.op(DVE, lambda e: e.tensor_tensor(out=lg[:], in0=bkl[:, 0:36], in1=rowb[:, R_BR:R_BR + 36], op=ALU.add),
                         r=[bkl, rowb], w=[lg])
                    k.op(DVE, lambda e: e.tensor_copy(out=lg4[:, gt % 4, :], in_=lg[:]), r=[lg], w=[lg4])
                    hb4[gt % 4] = hb
                    if gt % 4 != 3:
                        return
                    g0 = gt - 3
                    hbs = list(hb4)

                    def back(g0=g0, hbs=hbs):
                        GL = lg4[:, :, 0:4]
                        k.op(DVE, lambda e: e.reduce_max(out=sm4[:, 0, :], in_=GL, axis=AX.X), r=[lg4], w=[sm4])
                        k.op(DVE, lambda e: e.tensor_tensor(out=gh4[:], in0=GL, in1=sm4[:, 0, :].unsqueeze(2).to_broadcast([128, 4, 4]),
                                                            op=ALU.subtract), r=[lg4, sm4], w=[gh4])
                        k.op(ACT, lambda e: e.activation(out=ge4[:], in_=gh4[:], func=AF.Exp), r=[gh4], w=[ge4])
                        k.op(DVE, lambda e: e.reduce_sum(out=sm4[:, 1, :], in_=ge4[:], axis=AX.X), r=[ge4], w=[sm4])
                        k.op(DVE, lambda e: e.tensor_scalar(out=gh4[:], in0=gh4[:], scalar1=0.0, scalar2=None, op0=ALU.is_equal),
                             r=[gh4, ge4], w=[gh4])
                        k.op(DVE, lambda e: e.tensor_tensor(out=e448[:], in0=lg4[:, :, 4:36].rearrange("p t (g j) -> p t g j", g=4),
                                                            in1=gh4[:].unsqueeze(3).to_broadcast([128, 4, 4, 8]), op=ALU.mult),
                             r=[lg4, gh4], w=[e448])
                        k.op(DVE, lambda e: e.reduce_sum(out=esel4[:], in_=e448[:].rearrange("p t g j -> p t j g"), axis=AX.X),
                             r=[e448], w=[esel4])
                        for t_ in range(4):
                            k.op(DVE, lambda e: e.max(out=m84[:, t_, :], in_=esel4[:, t_, :]), r=[esel4], w=[m84])
                        for q in range(2):
                            k.op(DVE, lambda e: e.tensor_tensor(out=hot4[:, q, :, :], in0=esel4[:],
                                                                in1=m84[:, :, q:q + 1].to_broadcast([128, 4, 8]), op=ALU.is_equal),
                                 r=[esel4, m84], w=[hot4])
                        k.op(DVE, lambda e: e.tensor_tensor(out=sm4[:, 2, :], in0=m84[:, :, 1], in1=m84[:, :, 0], op=ALU.subtract),
                             r=[m84, sm4], w=[sm4])
                        k.op(ACT, lambda e: e.activation(out=sm4[:, 3, :], in_=sm4[:, 2, :], func=AF.Exp), r=[sm4], w=[sm4])
                        k.op(DVE, lambda e: e.scalar_tensor_tensor(out=sm4[:, 4, :], in0=sm4[:, 3, :], scalar=1.0, in1=sm4[:, 1, :],
                                                                   op0=ALU.add, op1=ALU.mult), r=[sm4], w=[sm4])
                        k.op(DVE, lambda e: e.reciprocal(out=rt_w[:, g0:g0 + 4, 0], in_=sm4[:, 4, :]), r=[sm4], w=[rt_w.sub(g0)])
                        k.op(DVE, lambda e: e.tensor_tensor(out=rt_w[:, g0:g0 + 4, 1], in0=rt_w[:, g0:g0 + 4, 0], in1=sm4[:, 3, :], op=ALU.mult),
                             r=[sm4, rt_w.sub(g0)], w=[rt_w.sub(g0)])
                        for q in range(2):
                            k.op(DVE, lambda e: e.tensor_tensor(out=M124[:, q, :, :].rearrange("p t (g j) -> p t g j", g=4),
                                                                in0=gh4[:].unsqueeze(3).to_broadcast([128, 4, 4, 8]),
                                                                in1=hot4[:, q, :, :].unsqueeze(2).to_broadcast([128, 4, 4, 8]), op=ALU.mult),
                                 r=[gh4, hot4], w=[M124])
                        k.op(DVE, lambda e: e.tensor_tensor(out=Msum4[:], in0=M124[:, 0, :, :], in1=M124[:, 1, :, :], op=ALU.add),
                             r=[M124], w=[Msum4])
                        bkp = banks[3]
                        MS = Msum4[:].rearrange("p t e -> p (t e)")

                        def fp(e):
                            e.matmul(out=bkp[:, 0:128], lhsT=cb[:, C_TLT:C_TLT + 128], rhs=MS, start=True, stop=True)
                            return e.matmul(out=bkp[:, 128:256], lhsT=cb[:, C_ONE:C_ONE + 128], rhs=MS, start=False, stop=True,
                                            skip_group_check=True)
                        k.op(PE, fp, r=[Msum4, cb], w=[bkp])
                        for t_ in range(4):
                            k.op(DVE, lambda e: e.tensor_tensor(out=pos4[:, t_, :], in0=bkp[:, t_ * 32:(t_ + 1) * 32], in1=cnt[:], op=ALU.add),
                                 r=[bkp, cnt], w=[pos4])
                            k.op(DVE, lambda e: e.tensor_tensor(out=cnt[:], in0=bkp[:, 128 + t_ * 32:128 + (t_ + 1) * 32], in1=cnt[:], op=ALU.add),
                                 r=[bkp, cnt], w=[cnt])
                        k.op(DVE, lambda e: e.tensor_scalar(out=pos4[:], in0=pos4[:], scalar1=float(CAP - 1), scalar2=None, op0=ALU.min),
                             r=[pos4], w=[pos4])
                        k.op(DVE, lambda e: e.tensor_tensor(out=pos4[:], in0=pos4[:], in1=cf[:, C_EB:C_EB + 32].unsqueeze(1).to_broadcast([128, 4, 32]),
                                                            op=ALU.add), r=[pos4, cf], w=[pos4])
                        for q in range(2):
                            k.op(DVE, lambda e: e.tensor_tensor(out=dsc4[:, q, :, :], in0=M124[:, q, :, :], in1=pos4[:], op=ALU.mult),
                                 r=[M124, pos4], w=[dsc4])
                        k.op(DVE, lambda e: e.reduce_sum(out=dfl4[:], in_=dsc4[:], axis=AX.X), r=[dsc4], w=[dfl4])
                        k.op(DVE, lambda e: e.tensor_copy(out=rt_d1[:, g0:g0 + 4], in_=dfl4[:, 0, :]), r=[dfl4], w=[rt_d1.sub(g0)])
                        k.op(DVE, lambda e: e.tensor_copy(out=rt_d2[:, g0:g0 + 4], in_=dfl4[:, 1, :]), r=[dfl4], w=[rt_d2.sub(g0)])
                        for t_ in range(4):
                            g_ = g0 + t_
                            k.dma(POOL, xs_d[:, :], hbs[t_][:], r=[hbs[t_], rt_d1.sub(g0)], cw=[B_xs], scatter_idx=rt_d1[:, g_:g_ + 1],
                                  bounds=NSLOT - 1)
                            k.dma(POOL, xs_d[:, :], hbs[t_][:], r=[hbs[t_], rt_d2.sub(g0)], cw=[B_xs], scatter_idx=rt_d2[:, g_:g_ + 1],
                                  bounds=NSLOT - 1)
                    pend_back.append(back)
                stageB()
        while pendB:
            pendB.pop(0)()
        while pend_back:
            pend_back.pop(0)()
        k.barrier()


def phase_experts(k, nc, CAP, EB, banks, cb, ident_b, xs_d, ys_d, w_up_d, w_dn_d, B_xs, B_ys, load_w):
    PE, ACT, DVE, POOL, SP = k.PE, k.ACT, k.DVE, k.POOL, k.SP
    NTB = EB // 128
    with ExitStack() as sc:
        wup = [k.sb(sc, f"wup{i}", [128, KC, 2 * FF], BF16) for i in range(3)]
        wdn = [k.sb(sc, f"wdn{i}", [128, 4, D], BF16) for i in range(3)]
        xr = [k.sb(sc, f"xr{i}", [128, NTB, D], BF16) for i in range(2)]
        xsT = [k.sb(sc, f"xsT{i}", [128, KC, EB], BF16) for i in range(2)]
        actT = [k.sb(sc, f"actT{i}", [128, 4, EB], BF16) for i in range(2)]
        sil = [k.sb(sc, f"sil{i}", [128, EB], F32) for i in range(2)]
        yt = [k.sb(sc, f"eyt{i}", [128, D], BF16) for i in range(2)]
        it = 0
        cc = 0
        blocks = [(ex, blk) for ex in range(N_EXP) for blk in range(CAP // EB)]

        def load_rows(i):
            ex_, blk_ = blocks[i]
            s0_ = ex_ * CAP + blk_ * EB
            k.dma(SP, xr[i % 2][:], xs_d[s0_:s0_ + EB, :].rearrange("(t p) d -> p t d", p=128), r=[B_xs], w=[xr[i % 2]])
        load_rows(0)
        for ex in range(N_EXP):
            wu, wd = wup[ex % 3], wdn[ex % 3]
            if ex == 0:
                for e0 in range(2):
                    load_w(wup[e0], w_up_d[e0], KC, 2 * FF)
                    load_w(wdn[e0], w_dn_d[e0], 4, D)
            if ex + 2 < N_EXP:
                load_w(wup[(ex + 2) % 3], w_up_d[ex + 2], KC, 2 * FF)
                load_w(wdn[(ex + 2) % 3], w_dn_d[ex + 2], 4, D)
            for blk in range(CAP // EB):
                s0 = ex * CAP + blk * EB
                xr_, xT, aT = xr[it % 2], xsT[it % 2], actT[it % 2]
                it += 1
                for t in range(NTB):
                    bk = banks[6 + t % 2]
                    bkb = bk.h.bitcast(BF16)

                    def tr(e, t=t):
                        for c in range(KC):
                            i = e.transpose(out=bkb[:, c * 128:(c + 1) * 128], in_=xr_[:, t, c * 128:(c + 1) * 128], identity=ident_b)
                        return i
                    k.op(PE, tr, r=[xr_, cb], w=[bk])
                    k.op(DVE, lambda e: e.tensor_copy(out=xT[:, :, t * 128:(t + 1) * 128],
                                                      in_=bkb[:, 0:1024].rearrange("p (c t) -> p c t", c=KC)), r=[bk], w=[xT])
                if it < len(blocks):
                    load_rows(it)
                for fc in range(4):
                    ba, bb = banks[(cc % 2) * 2], banks[(cc % 2) * 2 + 1]
                    cc += 1
                    for (bk, col0) in ((ba, fc * 128), (bb, FF + fc * 128)):
                        def f(e, bk=bk, col0=col0):
                            for c in range(KC):
                                i = e.matmul(out=bk[:, 0:EB], lhsT=wu[:, c, col0:col0 + 128], rhs=xT[:, c, :], start=(c == 0), stop=(c == KC - 1))
                            return i
                        k.op(PE, f, r=[wu, xT], w=[bk])
                    sl = sil[fc % 2]
                    k.op(ACT, lambda e: e.activation(out=sl[:], in_=ba[:, 0:EB], func=AF.Silu), r=[ba], w=[sl])
                    k.op(DVE, lambda e: e.tensor_tensor(out=aT[:, fc, :], in0=bb[:, 0:EB], in1=sl[:], op=ALU.mult), r=[bb, sl], w=[aT])
                for t in range(NTB):
                    y_ = yt[t % 2]
                    for half in range(2):
                        bk = banks[4 + half]

                        def f(e, half=half, t=t):
                            for c in range(4):
                                i = e.matmul(out=bk[:], lhsT=aT[:, c, t * 128:(t + 1) * 128], rhs=wd[:, c, half * 512:(half + 1) * 512],
                                             start=(c == 0), stop=(c == 3))
                            return i
                        k.op(PE, f, r=[aT, wd], w=[bk])
                        if half == 0:
                            k.op(ACT, lambda e: e.activation(out=y_[:, 0:512], in_=bk[:], func=AF.Copy), r=[bk], w=[y_])
                        else:
                            k.op(DVE, lambda e: e.tensor_copy(out=y_[:, 512:1024], in_=bk[:]), r=[bk], w=[y_])
                    k.dma(SP, ys_d[s0 + t * 128:s0 + (t + 1) * 128, :], y_[:], r=[y_], cw=[B_ys])
        k.barrier()


def phase_combine(k, nc, S, NT, NSEQ, CAP, rt_d1, rt_d2, rt_w, out_d, ys_d, B_ys, B_out):
    PE, ACT, DVE, POOL, SP = k.PE, k.ACT, k.DVE, k.POOL, k.SP
    NSLOT = N_EXP * CAP
    B_fin = Buf()
    with ExitStack() as sc:
        xt = [k.sb(sc, f"cxt{i}", [128, D], F32) for i in range(4)]
        y1 = [k.sb(sc, f"cy1{i}", [128, D], BF16) for i in range(4)]
        y2 = [k.sb(sc, f"cy2{i}", [128, D], BF16) for i in range(4)]
        o1 = [k.sb(sc, f"co1{i}", [128, D], F32) for i in range(4)]
        NTT_ = NSEQ * NT

        def fetch(g):
            sq_, t_ = divmod(g, NT)
            ii = g % 4
            k.dma(SP, xt[ii][:], out_d[sq_, t_ * 128:(t_ + 1) * 128, :], r=[B_out], w=[xt[ii]])
            k.dma(POOL, y1[ii][:], ys_d[:, :], r=[B_ys], w=[y1[ii]], gather_idx=rt_d1[:, g:g + 1], bounds=NSLOT - 1)
            k.dma(POOL, y2[ii][:], ys_d[:, :], r=[B_ys], w=[y2[ii]], gather_idx=rt_d2[:, g:g + 1], bounds=NSLOT - 1)
        for g in range(min(3, NTT_)):
            fetch(g)
        for gt in range(NTT_):
            seq, t = divmod(gt, NT)
            i = gt % 4
            if gt + 3 < NTT_:
                fetch(gt + 3)
            k.op(DVE, lambda e: e.scalar_tensor_tensor(out=o1[i][:], in0=y1[i][:], scalar=rt_w[:, gt, 0:1], in1=xt[i][:],
                                                       op0=ALU.mult, op1=ALU.add), r=[y1[i], xt[i], rt_w.sub(gt)], w=[o1[i]])
            k.op(DVE, lambda e: e.scalar_tensor_tensor(out=o1[i][:], in0=y2[i][:], scalar=rt_w[:, gt, 1:2], in1=o1[i][:],
                                                       op0=ALU.mult, op1=ALU.add), r=[y2[i], o1[i], rt_w.sub(gt)], w=[o1[i]])
            k.dma(SP, out_d[seq, t * 128:(t + 1) * 128, :], o1[i][:], r=[o1[i]], cw=[B_fin])
        k.barrier()


def make_consts(CAP):
    c = np.zeros((128, NCONST), np.float32)
    p = np.arange(128)[:, None]
    q = np.arange(128)[None, :]
    c[:, C_ID:C_ID + 128] = (p == q)
    c[:, C_MASK:C_MASK + 128] = np.where(p > q, -30000.0, 0.0)
    c[:, C_TLE:C_TLE + 128] = (p <= q)
    c[:, C_TLT:C_TLT + 128] = (p < q)
    c[:, C_B64:C_B64 + 128] = (p // 64 == q // 64)
    rope = np.zeros((128, 128), np.float32)
    for base in (0, 64):
        for d in range(8):
            rope[base + d + 8, base + d] = -1.0
            rope[base + d, base + d + 8] = 1.0
    c[:, C_ROPE:C_ROPE + 128] = rope
    dd = np.arange(128) % 64
    inv = np.where(dd < 16, ROPE_THETA ** (-(2.0 * (dd % 8)) / 16.0), 0.0)
    c[:, C_INVF] = inv.astype(np.float32)
    c[:, C_ONE:C_ONE + 128] = 1.0
    c[:, C_EB:C_EB + 32] = (np.arange(32) * CAP)[None, :]
    return c


def make_inmaps(inputs, n_cores, NSEQ, CAP):
    f = lambda a: np.ascontiguousarray(np.asarray(a, dtype=np.float32))
    row = np.concatenate([f(inputs["attn_norm_g"])[0], f(inputs["mem_norm_g"])[0], f(inputs["ffn_norm_g"])[0],
                          f(inputs["b_router_group"])[0], f(inputs["b_router_expert"])[0], f(inputs["diff_subln_g"])[0],
                          f(inputs["fox_forget_b"])[0], f(inputs["diff_lambda"])[0].ravel()])
    assert row.shape[0] == NROWB
    rowb = np.ascontiguousarray(np.broadcast_to(row[None, :], (128, NROWB)))
    i64 = np.arange(128) % 64
    colp = np.stack([f(inputs["diff_qnorm_g"])[0][i64], f(inputs["diff_knorm_g"])[0][i64], f(inputs["fox_qnorm_g"])[0][i64],
                     f(inputs["fox_knorm_g"])[0][i64], f(inputs["mem_qnorm_g"])[0], f(inputs["mem_knorm_g"])[0],
                     f(inputs["diff_subln_g"])[0]], axis=1)
    colp = np.ascontiguousarray(colp.astype(np.float32))
    shared = {
        "consts": make_consts(CAP), "rowb": rowb, "colp": colp,
        "w_in": f(inputs["w_in"])[0], "w_mem_kv": f(inputs["w_mem_kv"])[0],
        "w_o_diff": f(inputs["w_o_diff"])[0], "w_o_fox": f(inputs["w_o_fox"])[0], "w_o_mem": f(inputs["w_o_mem"])[0],
        "w_out": f(inputs["w_out"])[0],
        "w_router": np.ascontiguousarray(np.concatenate([f(inputs["w_router_group"])[0], f(inputs["w_router_expert"])[0]], axis=1)),
        "w_up": f(inputs["w_up"])[0], "w_down": f(inputs["w_down"])[0],
    }
    x = f(inputs["x"])
    mem = f(inputs["mem"])
    pos = np.ascontiguousarray(np.asarray(inputs["positions"], dtype=np.int32))
    maps = []
    for c in range(n_cores):
        m = dict(shared)
        m["x"] = np.ascontiguousarray(x[c * NSEQ:(c + 1) * NSEQ])
        m["mem"] = np.ascontiguousarray(mem[c * NSEQ:(c + 1) * NSEQ])
        m["pos"] = np.ascontiguousarray(pos[c * NSEQ:(c + 1) * NSEQ])
        maps.append(m)
    return maps


def kernel(**inputs):
    B, S, _ = np.asarray(inputs["x"]).shape
    n_cores = 8
    NSEQ = B // n_cores
    CAP = 768
    nc = build_program(S, NSEQ, CAP)
    maps = make_inmaps(inputs, n_cores, NSEQ, CAP)
    res = run_bass_kernel_spmd(nc, maps, core_ids=list(range(n_cores)))
    return np.concatenate([np.asarray(r["out"]) for r in res.results], axis=0).astype(np.float32)
```

```python
import math
from contextlib import ExitStack

import numpy as np
import concourse.bass as bass
import concourse.mybir as mybir
from concourse.bass_utils import run_bass_kernel_spmd

F32 = mybir.dt.float32
BF16 = mybir.dt.bfloat16
I32 = mybir.dt.int32
AF = mybir.ActivationFunctionType
ALU = mybir.AluOpType
AX = mybir.AxisListType

D = 1024
KC = 8
MEM_LEN = 256
EPS = 1e-6
N_EXP = 32
FF = 512
IN_COLS = 6664
OFF_QA, OFF_KA, OFF_VA, OFF_QB, OFF_KB, OFF_VB, OFF_FB, OFF_QC, OFF_G = 0, 512, 1024, 1536, 2048, 2560, 3072, 3080, 3592
ROPE_THETA = 500000.0
LAMBDA_INIT = 0.8 - 0.6 * math.exp(0.0)

C_ID, C_MASK, C_TLE, C_TLT, C_B64, C_ROPE, C_INVF, C_ONE = 0, 128, 256, 384, 512, 640, 768, 769
C_EB = 769 + 128
NCONST = C_EB + 32
R_AG, R_MG, R_FG, R_BR, R_SUB, R_FB, R_LAM = 0, 1024, 2048, 3072, 3108, 3236, 3244
NROWB = 3244 + 256
P_DQ, P_DK, P_FQ, P_FK, P_MQ, P_MK, P_SUB = 0, 1, 2, 3, 4, 5, 6
NCOLP = 7

ND_SEMS = 20


class Buf:
    __slots__ = ("w", "cw", "r")

    def __init__(self):
        self.w = {}
        self.cw = {}
        self.r = {}


class T:
    def __init__(self, h):
        self.h = h
        self.b = Buf()
        self.subs = {}

    def __getitem__(self, k):
        return self.h[k]

    def sub(self, key):
        b = self.subs.get(key)
        if b is None:
            b = self.subs[key] = Buf()
        return b


def _b(x):
    return x.b if isinstance(x, T) else x


class Eng:
    def __init__(self, k, eng, name, skip_self=False):
        self.eng = eng
        self.name = name
        self.key = k.newsem(name)
        self.cnt = 0
        self.waited = {}
        self.skip_self = skip_self
        self.dkeys = None
        self.dn = 0


class K:
    def __init__(self, nc):
        self.nc = nc
        self.es = ExitStack()
        self.sems = []
        self.PE = Eng(self, nc.tensor, "pe", skip_self=True)
        self.ACT = Eng(self, nc.scalar, "act")
        self.DVE = Eng(self, nc.vector, "dve")
        self.POOL = Eng(self, nc.gpsimd, "pool")
        self.SP = Eng(self, nc.sync, "sp")
        self.engs = [self.PE, self.ACT, self.DVE, self.POOL, self.SP]
        for q in (self.SP, self.POOL):
            q.dkeys = [self.newsem(f"d{q.name}{i}") for i in range(ND_SEMS)]
        self.uid = 0

    def newsem(self, name):
        h = self.es.enter_context(self.nc.semaphore(name))
        self.sems.append(h)
        return len(self.sems) - 1

    def name(self, base):
        self.uid += 1
        return f"{base}_{self.uid}"

    def sb(self, sc, name, shape, dt):
        return T(sc.enter_context(self.nc.sbuf_tensor(self.name(name), list(shape), dt)))

    def ps(self, sc, name, shape, dt):
        return T(sc.enter_context(self.nc.psum_tensor(self.name(name), list(shape), dt)))

    def _wait(self, E, deps):
        for k, v in deps.items():
            if E.skip_self and k == E.key:
                continue
            if E.waited.get(k, 0) >= v:
                continue
            E.eng.wait_ge(self.sems[k], v)
            E.waited[k] = v

    @staticmethod
    def _deps(r, w, cw):
        d = {}

        def add(m):
            for k, v in m.items():
                if d.get(k, 0) < v:
                    d[k] = v

        for b in r:
            add(b.w)
            add(b.cw)
        for b in w:
            add(b.w)
            add(b.cw)
            add(b.r)
        for b in cw:
            add(b.w)
            add(b.r)
        return d

    @staticmethod
    def _record(ev, r, w, cw):
        k, v = ev
        for b in r:
            if b.r.get(k, 0) < v:
                b.r[k] = v
        for b in w:
            b.w = {k: v}
            b.cw = {}
            b.r = {}
        for b in cw:
            if b.cw.get(k, 0) < v:
                b.cw[k] = v

    def op(self, E, fn, r=(), w=(), cw=()):
        r = [_b(x) for x in r]
        w = [_b(x) for x in w]
        cw = [_b(x) for x in cw]
        self._wait(E, self._deps(r, w, cw))
        inst = fn(E.eng)
        E.cnt += 1
        inst.then_inc(self.sems[E.key], 1)
        self._record((E.key, E.cnt), r, w, cw)

    def dma(self, Q, out, in_, r=(), w=(), cw=(), scatter_idx=None, gather_idx=None, bounds=None, **kw):
        r = [_b(x) for x in r]
        w = [_b(x) for x in w]
        cw = [_b(x) for x in cw]
        self._wait(Q, self._deps(r, w, cw))
        i = Q.dn
        Q.dn += 1
        slot, use = i % ND_SEMS, i // ND_SEMS
        k = Q.dkeys[slot]
        if use > 0 and Q.waited.get(k, 0) < 16 * use:
            Q.eng.wait_ge(self.sems[k], 16 * use)
            Q.waited[k] = 16 * use
        if bounds is not None:
            if not hasattr(self, "_breg"):
                self._breg = {}
            if bounds not in self._breg:
                self._breg[bounds] = Q.eng.to_reg(bounds)
            bounds = self._breg[bounds]
        if scatter_idx is not None:
            inst = Q.eng.indirect_dma_start(out=out, out_offset=bass.IndirectOffsetOnAxis(ap=scatter_idx, axis=0),
                                            in_=in_, in_offset=None, bounds_check=bounds, oob_is_err=False)
        elif gather_idx is not None:
            inst = Q.eng.indirect_dma_start(out=out, out_offset=None, in_=in_,
                                            in_offset=bass.IndirectOffsetOnAxis(ap=gather_idx, axis=0),
                                            bounds_check=bounds, oob_is_err=False)
        else:
            inst = Q.eng.dma_start(out=out, in_=in_, **kw)
        inst.then_inc(self.sems[k], 16)
        self._record((k, 16 * (use + 1)), r, w, cw)

    def barrier(self, engs=None):
        tot = {}
        for E in self.engs:
            if E.cnt:
                tot[E.key] = E.cnt
            if E.dkeys:
                for s, k in enumerate(E.dkeys):
                    uses = (E.dn - s + ND_SEMS - 1) // ND_SEMS if E.dn > s else 0
                    if uses:
                        tot[k] = 16 * uses
        for E in (engs or self.engs):
            d = dict(tot)
            d.pop(E.key, None)
            sk = E.skip_self
            E.skip_self = False
            self._wait(E, d)
            E.skip_self = sk


def build_program(S, NSEQ, CAP, dbg=None):
    nc = bass.Bass("TRN2", target_bir_lowering=False)
    k = K(nc)
    k.es.enter_context(nc.allow_low_precision("bf16 operands / fp32 accumulation by design"))
    PE, ACT, DVE, POOL, SP = k.PE, k.ACT, k.DVE, k.POOL, k.SP
    NT = S // 128
    NB = S // 512
    NTT = NSEQ * NT
    NSLOT = N_EXP * CAP
    EB = 384 if CAP % 384 == 0 else 128
    dt = nc.dram_tensor

    x_d = dt("x", [NSEQ, S, D], F32, kind="ExternalInput").ap()
    mem_d = dt("mem", [NSEQ, MEM_LEN, D], F32, kind="ExternalInput").ap()
    pos_d = dt("pos", [NSEQ, S], I32, kind="ExternalInput").ap()
    consts_d = dt("consts", [128, NCONST], F32, kind="ExternalInput").ap()
    rowb_d = dt("rowb", [128, NROWB], F32, kind="ExternalInput").ap()
    colp_d = dt("colp", [128, NCOLP], F32, kind="ExternalInput").ap()
    w_in_d = dt("w_in", [D, IN_COLS], F32, kind="ExternalInput").ap()
    w_kv_d = dt("w_mem_kv", [D, 1024], F32, kind="ExternalInput").ap()
    w_o_d = [dt(n, [512, D], F32, kind="ExternalInput").ap() for n in ("w_o_diff", "w_o_fox", "w_o_mem")]
    w_out_d = dt("w_out", [D, D], F32, kind="ExternalInput").ap()
    w_r_d = dt("w_router", [D, 36], F32, kind="ExternalInput").ap()
    w_up_d = dt("w_up", [N_EXP, D, 2 * FF], F32, kind="ExternalInput").ap()
    w_dn_d = dt("w_down", [N_EXP, FF, D], F32, kind="ExternalInput").ap()
    out_d = dt("out", [NSEQ, S, D], F32, kind="ExternalOutput").ap()
    yT_d = dt("yT_scr", [NSEQ, 12, 128, S], BF16).ap()
    g_d = dt("g_scr", [NSEQ, 24, 128, S], BF16).ap()
    xs_d = dt("xs_scr", [NSLOT, D], BF16).ap()
    ys_d = dt("ys_scr", [NSLOT, D], BF16).ap()
    B_yT, B_g, B_xs, B_ys, B_out = Buf(), Buf(), Buf(), Buf(), Buf()
    dbg_d = {}
    if dbg:
        for n, (shape, dty) in dbg.items():
            dbg_d[n] = dt(n, list(shape), dty, kind="ExternalOutput").ap()

    top = k.es
    cf = k.sb(top, "cf", [128, NCONST], F32)
    cb = k.sb(top, "cb", [128, NCONST], BF16)
    rowb = k.sb(top, "rowb", [128, NROWB], F32)
    colp = k.sb(top, "colp", [128, NCOLP], F32)
    lam = k.sb(top, "lam", [128, 4], F32)
    wr = k.sb(top, "wr", [128, KC, 36], F32)
    rt_d1 = k.sb(top, "rt_d1", [128, NTT], I32)
    rt_d2 = k.sb(top, "rt_d2", [128, NTT], I32)
    rt_w = k.sb(top, "rt_w", [128, NTT, 2], F32)
    cnt = k.sb(top, "cnt", [128, N_EXP], F32)
    banks = [k.ps(top, f"bank{i}", [128, 512], F32) for i in range(8)]

    k.dma(SP, cf[:], consts_d[:, :], w=[cf])
    k.dma(SP, rowb[:], rowb_d[:, :], w=[rowb])
    k.dma(SP, colp[:], colp_d[:, :], w=[colp])
    k.dma(SP, wr[:], w_r_d.rearrange("(kc p) c -> p kc c", p=128), w=[wr])
    k.op(DVE, lambda e: e.tensor_copy(out=cb[:], in_=cf[:]), r=[cf], w=[cb])
    k.op(POOL, lambda e: e.memset(cnt[:], 0.0), w=[cnt])
    ident_b = cb[:, C_ID:C_ID + 128]
    ident_f = cf[:, C_ID:C_ID + 128]

    with ExitStack() as sc:
        t = k.sb(sc, "lt", [128, 2, 64], F32)
        s2 = k.sb(sc, "ls", [128, 2], F32)
        lp = rowb[:, R_LAM:R_LAM + 256].rearrange("p (a b c) -> p a b c", a=2, b=2)
        k.op(DVE, lambda e: e.tensor_tensor(out=t[:], in0=lp[:, :, 0, :], in1=lp[:, :, 1, :], op=ALU.mult), r=[rowb], w=[t])
        k.op(DVE, lambda e: e.reduce_sum(out=s2[:], in_=t[:], axis=AX.X), r=[t], w=[s2])
        k.op(ACT, lambda e: e.activation(out=s2[:], in_=s2[:], func=AF.Exp), r=[s2], w=[s2])
        k.op(DVE, lambda e: e.scalar_tensor_tensor(out=lam[:, 0:1], in0=s2[:, 1:2], scalar=-LAMBDA_INIT, in1=s2[:, 0:1],
                                                   op0=ALU.add, op1=ALU.subtract), r=[s2], w=[lam])
        k.op(DVE, lambda e: e.tensor_scalar(out=lam[:, 1:2], in0=colp[:, P_SUB:P_SUB + 1], scalar1=1.0 - LAMBDA_INIT,
                                            scalar2=None, op0=ALU.mult), r=[colp, lam], w=[lam])
        k.barrier()

    bank_rr = [0]

    def rms_rstd(E_ok, out_ap, in_ap, n, rbufs, wbufs):
        k.op(ACT, lambda e: e.activation(out=out_ap, in_=in_ap, func=AF.Ln, scale=1.0 / n, bias=EPS),
             r=rbufs, w=wbufs)
        k.op(ACT, lambda e: e.activation(out=out_ap, in_=out_ap, func=AF.Exp, scale=-0.5), r=wbufs, w=wbufs)

    def load_w(sc_t, src_ap, rows_kc, ncols):
        k.dma(POOL, sc_t[:, 0:rows_kc, 0:ncols], src_ap.rearrange("(kc p) c -> p kc c", p=128), w=[sc_t])

    sc_cache = {}

    def norm_tokens_to_T(sc, src_ap_fn, ntiles, gcol0, dstT, tag, every4=None):
        if "nb" not in sc_cache:
            sc_cache["nb"] = ([k.sb(sc, f"xt{i}", [128, D], F32) for i in range(4)],
                              [k.sb(sc, f"xn{i}", [128, D], BF16) for i in range(4)],
                              [k.sb(sc, f"st{i}", [128, 2], F32) for i in range(4)],
                              k.sb(sc, "junk", [128, D], BF16))
        xts, xn, st, junk = sc_cache["nb"]
        for t in range(ntiles):
            xt, xb, s_ = xts[t % 4], xn[t % 4], st[t % 4]
            if every4 is not None and t % 4 == 0:
                every4(t // 4)
            k.dma(SP, xt[:], src_ap_fn(t), w=[xt])
            k.op(ACT, lambda e: e.activation(out=junk[:], in_=xt[:], func=AF.Square, accum_out=s_[:, 0:1]), r=[xt], w=[junk, s_])
            rms_rstd(None, s_[:, 1:2], s_[:, 0:1], D, [s_], [s_])
            k.op(DVE, lambda e: e.scalar_tensor_tensor(out=xb[:], in0=xt[:], scalar=s_[:, 1:2], in1=rowb[:, gcol0:gcol0 + D],
                                                       op0=ALU.mult, op1=ALU.mult), r=[xt, s_, rowb], w=[xb])
            bk = banks[6 + (t % 2)]
            bkb = bk.h.bitcast(BF16)

            def tr(e):
                for c in range(KC):
                    i = e.transpose(out=bkb[:, c * 128:(c + 1) * 128], in_=xb[:, c * 128:(c + 1) * 128], identity=ident_b)
                return i
            k.op(PE, tr, r=[xb, cb], w=[bk])
            k.op(DVE, lambda e: e.tensor_copy(out=dstT[:, :, t * 128:(t + 1) * 128],
                                              in_=bkb[:, 0:1024].rearrange("p (c t) -> p c t", c=KC)), r=[bk], w=[dstT.sub(t // 4)])

    def proj_fm(hT, hbuf, ws, wcol0, M, tok0, ntok, bank):
        def f(e):
            for c in range(KC):
                i = e.matmul(out=bank[0:M, 0:ntok], lhsT=ws[:, c, wcol0:wcol0 + M], rhs=hT[:, c, tok0:tok0 + ntok],
                             start=(c == 0), stop=(c == KC - 1))
            return i
        k.op(PE, f, r=[hbuf, ws], w=[bank])

    for seq in range(NSEQ):
        with ExitStack() as sc:
            hT = k.sb(sc, "hT", [128, KC, S], BF16)
            memT = k.sb(sc, "memT", [128, KC, MEM_LEN], BF16)
            ropeC = k.sb(sc, "ropeC", [128, S], BF16)
            ropeS = k.sb(sc, "ropeS", [128, S], BF16)
            with ExitStack() as s1:
                sc_cache.clear()
                RW = 512
                posi = k.sb(s1, "posi", [128, RW], I32)
                ang = k.sb(s1, "ang", [128, RW], F32)
                kk = k.sb(s1, "kk", [128, RW], F32)
                ki = k.sb(s1, "ki", [128, RW], I32)
                ang2 = k.sb(s1, "ang2", [128, RW], F32)
                TWO_PI = 2.0 * math.pi

                def wrap(a, shift):
                    k.op(DVE, lambda e: e.tensor_scalar(out=kk[:], in0=a[:], scalar1=shift, scalar2=1.0 / TWO_PI,
                                                        op0=ALU.add, op1=ALU.mult), r=[a], w=[kk])
                    k.op(DVE, lambda e: e.tensor_copy(out=ki[:], in_=kk[:]), r=[kk], w=[ki])
                    k.op(DVE, lambda e: e.tensor_copy(out=kk[:], in_=ki[:]), r=[ki], w=[kk])
                    k.op(DVE, lambda e: e.scalar_tensor_tensor(out=kk[:], in0=kk[:], scalar=-TWO_PI, in1=a[:],
                                                               op0=ALU.mult, op1=ALU.add), r=[kk, a], w=[kk])
                    if shift != 0.0:
                        k.op(DVE, lambda e: e.tensor_scalar(out=kk[:], in0=kk[:], scalar1=shift, scalar2=None, op0=ALU.add),
                             r=[kk], w=[kk])
                    k.op(DVE, lambda e: e.tensor_scalar(out=a[:], in0=kk[:], scalar1=math.pi, scalar2=-TWO_PI,
                                                        op0=ALU.is_gt, op1=ALU.mult), r=[kk], w=[a])
                    k.op(DVE, lambda e: e.tensor_tensor(out=kk[:], in0=kk[:], in1=a[:], op=ALU.add), r=[kk, a], w=[kk])
                    k.op(DVE, lambda e: e.tensor_scalar(out=a[:], in0=kk[:], scalar1=-math.pi, scalar2=TWO_PI,
                                                        op0=ALU.is_lt, op1=ALU.mult), r=[kk], w=[a])
                    k.op(DVE, lambda e: e.tensor_tensor(out=a[:], in0=kk[:], in1=a[:], op=ALU.add), r=[kk, a], w=[a])
                    k.op(DVE, lambda e: e.tensor_scalar(out=a[:], in0=a[:], scalar1=3.14159, scalar2=-3.14159,
                                                        op0=ALU.min, op1=ALU.max), r=[a], w=[a])

                def rope_block(bi):
                    r0_ = bi * RW
                    k.dma(SP, posi[:], pos_d[seq:seq + 1, r0_:r0_ + RW].partition_broadcast(128), w=[posi])
                    k.op(DVE, lambda e: e.tensor_copy(out=ang[:], in_=posi[:]), r=[posi], w=[ang])
                    k.op(DVE, lambda e: e.tensor_scalar(out=ang[:], in0=ang[:], scalar1=cf[:, C_INVF:C_INVF + 1], scalar2=None,
                                                        op0=ALU.mult), r=[ang, cf], w=[ang])
                    wrap(ang, 0.0)
                    k.op(ACT, lambda e: e.activation(out=ropeS[:, r0_:r0_ + RW], in_=ang[:], func=AF.Sin), r=[ang], w=[ropeS])
                    k.op(ACT, lambda e: e.activation(out=ang2[:], in_=ang[:], func=AF.Abs), r=[ang], w=[ang2])
                    k.op(DVE, lambda e: e.tensor_scalar(out=ang2[:], in0=ang2[:], scalar1=-1.0, scalar2=math.pi / 2.0,
                                                        op0=ALU.mult, op1=ALU.add), r=[ang2], w=[ang2])
                    k.op(ACT, lambda e: e.activation(out=ropeC[:, r0_:r0_ + RW], in_=ang2[:], func=AF.Sin), r=[ang2], w=[ropeC])
                norm_tokens_to_T(s1, lambda t: x_d[seq, t * 128:(t + 1) * 128, :], NT, R_AG, hT, "x", every4=rope_block)
                norm_tokens_to_T(s1, lambda t: mem_d[seq, t * 128:(t + 1) * 128, :], MEM_LEN // 128, R_MG, memT, "m")
                k.barrier()
            QB = k.sb(sc, "QB", [128, S], BF16)
            KB = k.sb(sc, "KB", [128, S], BF16)
            QB2 = k.sb(sc, "QB2", [128, S], BF16)
            dacc = [k.sb(sc, f"dacc{i}", [128, 512], F32) for i in range(4)]
            VB = k.sb(sc, "VB", [128, NT, 128], BF16)
            WS = [k.sb(sc, f"WS{i}", [128, KC, 512], BF16) for i in range(3)]
            PT = [k.sb(sc, f"PT{i}", [128, 512], BF16) for i in range(4)]
            sq = [k.sb(sc, f"sq{i}", [128, 512], BF16) for i in range(3)]
            rstd = [k.sb(sc, f"rstd{i}", [128, 512], F32) for i in range(3)]
            qn = [k.sb(sc, f"qn{i}", [128, 512], BF16) for i in range(3)]
            tmpf = [k.sb(sc, f"tmpf{i}", [128, 512], F32) for i in range(3)]
            ytb = [k.sb(sc, f"ytb{i}", [128, 512], BF16) for i in range(2)]
            rr = [k.sb(sc, f"rr{i}", [128, 2, 2, 2], F32) for i in range(4)]
            ncum = k.sb(sc, "ncum", [128, NT, 8], F32)
            ncum8 = k.sb(sc, "ncum8", [128, 64 + NT * 8], BF16)
            nl = k.sb(sc, "nl", [128, NT, 8], F32)
            offs = k.sb(sc, "offs", [128, NT, 8], F32)
            memK = k.sb(sc, "memK", [128, 4, MEM_LEN], BF16)
            memV = k.sb(sc, "memV", [128, 2, 4, 128], BF16)
            dtm = [k.sb(sc, f"dtm{i}", [128, 512], F32) for i in range(3)]

            hbufs = [hT.sub(i) for i in range((NT + 3) // 4)]

            def qk_proj(ws, wcol0, M, src, sbufs, ntok_total, dst, dbuf, gcolidx, blkn, rope, ctr=[0], split=None):
                for t0 in range(0, ntok_total, 512):
                    n = min(512, ntok_total - t0)
                    i = ctr[0] % 3
                    ctr[0] += 1
                    bk = banks[(6, 7, 3)[i]]
                    bk2 = banks[(4, 5, 2)[i]]
                    proj_fm(src, sbufs[t0 // 512] if len(sbufs) > 1 else sbufs[0], ws, wcol0, M, t0, n, bk)
                    k.op(ACT, lambda e: e.activation(out=sq[i][0:M, 0:n], in_=bk[0:M, 0:n], func=AF.Square), r=[bk], w=[sq[i]])
                    ones = cb[0:M, C_B64:C_B64 + M] if blkn == 64 else cb[0:M, C_ONE:C_ONE + M]
                    k.op(PE, lambda e: e.matmul(out=bk2[0:M, 0:n], lhsT=ones, rhs=sq[i][0:M, 0:n], start=True, stop=True),
                         r=[sq[i], cb], w=[bk2])
                    rms_rstd(None, rstd[i][0:M, 0:n], bk2[0:M, 0:n], blkn, [bk2], [rstd[i]])
                    out_ap = dst[0:M, t0:t0 + n] if not rope else qn[i][0:M, 0:n]
                    obuf = [dbuf] if not rope else [qn[i]]
                    k.op(DVE, lambda e: e.scalar_tensor_tensor(out=out_ap, in0=bk[0:M, 0:n], scalar=colp[0:M, gcolidx:gcolidx + 1],
                                                               in1=rstd[i][0:M, 0:n], op0=ALU.mult, op1=ALU.mult),
                         r=[bk, colp, rstd[i]], w=obuf)
                    if rope:
                        k.op(PE, lambda e: e.matmul(out=bk2[0:M, 0:n], lhsT=cb[0:M, C_ROPE:C_ROPE + M], rhs=qn[i][0:M, 0:n],
                                                    start=True, stop=True), r=[qn[i], cb], w=[bk2])
                        k.op(DVE, lambda e: e.tensor_tensor(out=tmpf[i][0:M, 0:n], in0=bk2[0:M, 0:n], in1=ropeS[0:M, t0:t0 + n],
                                                            op=ALU.mult), r=[bk2, ropeS], w=[tmpf[i]])
                        k.op(DVE, lambda e: e.tensor_tensor(out=rstd[i][0:M, 0:n], in0=qn[i][0:M, 0:n], in1=ropeC[0:M, t0:t0 + n],
                                                            op=ALU.mult), r=[qn[i], ropeC], w=[rstd[i]])
                        if split is None:
                            k.op(DVE, lambda e: e.tensor_tensor(out=dst[0:M, t0:t0 + n], in0=rstd[i][0:M, 0:n], in1=tmpf[i][0:M, 0:n],
                                                                op=ALU.add), r=[rstd[i], tmpf[i]], w=[dbuf])
                        else:
                            for (dd, lo) in ((dst, 0), (split, 64)):
                                k.op(DVE, lambda e: e.tensor_tensor(out=dd[lo:lo + 64, t0:t0 + n], in0=rstd[i][lo:lo + 64, 0:n],
                                                                     in1=tmpf[i][lo:lo + 64, 0:n], op=ALU.add),
                                     r=[rstd[i], tmpf[i]], w=[dd])

            def v_proj(ws, wcol0, dv, src, sbufs, ntiles, dstV, vbuf, ones_col, vctr=[0]):
                if ones_col is not None:
                    k.op(POOL, lambda e: e.memset(dstV[:, 0:ntiles, ones_col:ones_col + 1], 1.0), w=[vbuf])
                for t0 in range(0, ntiles, 4):
                    nt_ = min(4, ntiles - t0)
                    bk = banks[6 + vctr[0] % 2]
                    vctr[0] += 1

                    def f(e):
                        for tt in range(nt_):
                            for c in range(KC):
                                i = e.matmul(out=bk[:, tt * 128:tt * 128 + dv], lhsT=src[:, c, (t0 + tt) * 128:(t0 + tt + 1) * 128],
                                             rhs=ws[:, c, wcol0:wcol0 + dv], start=(c == 0), stop=(c == KC - 1))
                        return i
                    k.op(PE, f, r=[sbufs[min(t0 // 4, len(sbufs) - 1)], ws], w=[bk])
                    k.op(DVE, lambda e: e.tensor_copy(out=dstV[:, t0:t0 + nt_, 0:dv],
                                                      in_=bk[:, 0:nt_ * 128].rearrange("p (t c) -> p t c", c=128)[:, :, 0:dv]),
                         r=[bk], w=[vbuf])

            sctr = [0]
            pctr = [0]
            pend = []

            def flush_pv(keep=0):
                while len(pend) > keep:
                    pend.pop(0)()

            def attn_fm(qb, Kr, krow0, kT, kbuf, qT, qbuf, V, vbuf, nk_total, causal, scale, bias_fn, bias_bufs, numb, denb):
                q0 = qb * 512
                nk = 4 * qb + 4 if causal else nk_total
                for j in range(nk):
                    r_ = j - 4 * qb if causal else -1
                    c0 = 128 * r_ if r_ > 0 else 0
                    sbk = banks[sctr[0] % 3]
                    sctr[0] += 1
                    pt = PT[pctr[0] % 4]
                    pctr[0] += 1

                    def qk(e, j=j, r_=r_, c0=c0, sbk=sbk):
                        i = e.matmul(out=sbk[:, c0:512], lhsT=kT[krow0:krow0 + Kr, j * 128:(j + 1) * 128],
                                     rhs=qT[krow0:krow0 + Kr, q0 + c0:q0 + 512], start=True, stop=True)
                        if r_ >= 0:
                            i = e.matmul(out=sbk[:, c0:c0 + 128], lhsT=ident_b, rhs=cb[:, C_MASK:C_MASK + 128],
                                         start=False, stop=True, skip_group_check=True)
                        return i
                    k.op(PE, qk, r=[kbuf, qbuf, cb], w=[sbk])
                    if bias_fn is None:
                        k.op(ACT, lambda e: e.activation(out=pt[:, c0:512], in_=sbk[:, c0:512], func=AF.Exp, scale=scale),
                             r=[sbk], w=[pt])
                    else:
                        k.op(ACT, lambda e: e.activation(out=pt[:, c0:512], in_=sbk[:, c0:512], func=AF.Exp, scale=scale,
                                                         bias=bias_fn(j)), r=[sbk] + bias_bufs, w=[pt])

                    def pv(e, j=j, c0=c0, pt=pt, nk=nk):
                        i = e.matmul(out=numb[:, c0:512], lhsT=V(j), rhs=pt[:, c0:512], start=(j == 0), stop=(j == nk - 1),
                                     skip_group_check=True)
                        if denb is not None:
                            i = e.matmul(out=denb[:, c0:512], lhsT=cb[:, C_ONE:C_ONE + 128], rhs=pt[:, c0:512], start=(j == 0),
                                         stop=(j == nk - 1), skip_group_check=True)
                        return i
                    wb = [numb] + ([denb] if denb is not None else [])
                    pend.append(lambda pv=pv, pt=pt, wb=wb: k.op(PE, pv, r=[pt, vbuf, cb], w=wb))
                    flush_pv(3)

            def store_yT(ytile, chunk_idx, qb):
                k.dma(SP, yT_d[seq, chunk_idx, :, qb * 512:(qb + 1) * 512], ytile[:], r=[ytile], cw=[B_yT])

            load_w(WS[0], w_in_d[:, OFF_QA:OFF_QA + 512], KC, 512)
            load_w(WS[1], w_in_d[:, OFF_KA:OFF_KA + 512], KC, 512)
            load_w(WS[2], w_in_d[:, OFF_VA:OFF_VA + 512], KC, 512)
            uc = 0
            dtail = []
            k.op(POOL, lambda e: e.memset(QB[64:128, :], 0.0), w=[QB])
            k.op(POOL, lambda e: e.memset(QB2[0:64, :], 0.0), w=[QB2])
            for h in range(4):
                qk_proj(WS[0], h * 128, 128, hT, hbufs, S, QB, QB, P_DQ, 64, True, split=QB2)
                qk_proj(WS[1], h * 128, 128, hT, hbufs, S, KB, KB, P_DK, 64, True)
                v_proj(WS[2], h * 128, 128, hT, hbufs, NT, VB, VB, None)
                for qb in range(NB):
                    nb_ = [banks[3], banks[5]]
                    db_ = [banks[4], banks[6]]
                    for m in range(2):
                        qsel = QB if m == 0 else QB2
                        attn_fm(qb, 128, 0, KB, KB, qsel, qsel, lambda j: VB[:, j, 0:128], VB, NT, True, 0.125, None, [],
                                nb_[m], db_[m])
                    flush_pv(0)
                    if dtail:
                        dtail.pop(0)()
                    t0_, t1_ = dtm[uc % 2], dtm[2]
                    yt_ = ytb[uc % 2]
                    rs_ = rstd[uc % 2]
                    sq_ = sq[uc % 2]
                    uc += 1
                    k.op(DVE, lambda e: e.tensor_copy(out=dacc[0][:], in_=nb_[0][:]), r=[nb_[0]], w=[dacc[0]])
                    k.op(DVE, lambda e: e.tensor_copy(out=dacc[1][:], in_=db_[0][:]), r=[db_[0]], w=[dacc[1]])
                    k.op(DVE, lambda e: e.tensor_copy(out=dacc[2][:], in_=nb_[1][:]), r=[nb_[1]], w=[dacc[2]])
                    k.op(DVE, lambda e: e.tensor_copy(out=dacc[3][:], in_=db_[1][:]), r=[db_[1]], w=[dacc[3]])
                    k.op(DVE, lambda e: e.reciprocal(out=dacc[1][:], in_=dacc[1][:]), r=[dacc[1]], w=[dacc[1]])
                    k.op(DVE, lambda e: e.tensor_tensor(out=t0_[:], in0=dacc[0][:], in1=dacc[1][:], op=ALU.mult), r=[dacc[0], dacc[1]], w=[t0_])
                    k.op(DVE, lambda e: e.reciprocal(out=dacc[3][:], in_=dacc[3][:]), r=[dacc[3]], w=[dacc[3]])
                    k.op(DVE, lambda e: e.tensor_tensor(out=t1_[:], in0=dacc[2][:], in1=dacc[3][:], op=ALU.mult), r=[dacc[2], dacc[3]], w=[t1_])
                    k.op(DVE, lambda e: e.scalar_tensor_tensor(out=t0_[:], in0=t1_[:], scalar=lam[:, 0:1], in1=t0_[:],
                                                               op0=ALU.mult, op1=ALU.add), r=[t1_, t0_, lam], w=[t0_])
                    k.op(POOL, lambda e: e.tensor_tensor(out=sq_[:], in0=t0_[:], in1=t0_[:], op=ALU.mult), r=[t0_], w=[sq_])

                    def tail(t0_=t0_, yt_=yt_, rs_=rs_, sq_=sq_, h=h, qb=qb):
                        bk7 = banks[7]
                        k.op(PE, lambda e: e.matmul(out=bk7[:], lhsT=cb[:, C_ONE:C_ONE + 128], rhs=sq_[:], start=True, stop=True),
                             r=[sq_, cb], w=[bk7])
                        rms_rstd(None, rs_[:], bk7[:], 128, [bk7], [rs_])
                        k.op(DVE, lambda e: e.scalar_tensor_tensor(out=yt_[:], in0=t0_[:], scalar=lam[:, 1:2], in1=rs_[:],
                                                                   op0=ALU.mult, op1=ALU.mult), r=[t0_, rs_, lam], w=[yt_])
                        store_yT(yt_, 0 + h, qb)
                    dtail.append(tail)
                while dtail:
                    dtail.pop(0)()

            load_w(WS[0], w_in_d[:, OFF_QB:OFF_QB + 512], KC, 512)
            load_w(WS[1], w_in_d[:, OFF_KB:OFF_KB + 512], KC, 512)
            load_w(WS[2], w_in_d[:, OFF_VB:OFF_VB + 512], KC, 512)
            with ExitStack() as s2:
                wfb = k.sb(s2, "wfb", [128, KC, 8], BF16)
                load_w(wfb, w_in_d[:, OFF_FB:OFF_FB + 8], KC, 8)
                for t in range(NT):
                    bk = banks[6 + t % 2]

                    def f(e):
                        for c in range(KC):
                            i = e.matmul(out=bk[:, 0:8], lhsT=hT[:, c, t * 128:(t + 1) * 128], rhs=wfb[:, c, :],
                                         start=(c == 0), stop=(c == KC - 1))
                        return i
                    k.op(PE, f, r=[hbufs[t // 4], wfb], w=[bk])
                    k.op(DVE, lambda e: e.tensor_tensor(out=nl[:, t, :], in0=bk[:, 0:8], in1=rowb[:, R_FB:R_FB + 8], op=ALU.add),
                         r=[bk, rowb], w=[nl.sub(t)])
                nlb = [nl.sub(t) for t in range(NT)]
                k.op(ACT, lambda e: e.activation(out=nl[:], in_=nl[:], func=AF.Exp, scale=-1.0), r=nlb, w=[nl] + nlb)
                k.op(ACT, lambda e: e.activation(out=nl[:], in_=nl[:], func=AF.Ln, bias=1.0), r=[nl], w=[nl])
                nlf = nl[:].rearrange("p t h -> p (t h)")
                bkA, bkB = banks[6], banks[7]
                for c0 in range(0, NT * 8, 512):
                    n = min(512, NT * 8 - c0)
                    k.op(PE, lambda e: e.matmul(out=bkA[:, 0:n], lhsT=cf[:, C_TLE:C_TLE + 128], rhs=nlf[:, c0:c0 + n], start=True, stop=True),
                         r=[nl, cf], w=[bkA])
                    k.op(PE, lambda e: e.matmul(out=bkB[:, 0:n], lhsT=cf[:, C_ONE:C_ONE + 128], rhs=nlf[:, c0:c0 + n], start=True, stop=True),
                         r=[nl, cf], w=[bkB])
                    k.op(DVE, lambda e: e.tensor_copy(out=ncum[:].rearrange("p t h -> p (t h)")[:, c0:c0 + n], in_=bkA[:, 0:n]),
                         r=[bkA], w=[ncum])
                    k.op(DVE, lambda e: e.tensor_copy(out=offs[:].rearrange("p t h -> p (t h)")[:, c0:c0 + n], in_=bkB[:, 0:n]),
                         r=[bkB], w=[offs])
                k.op(POOL, lambda e: e.memset(nl[:, 0, :], 0.0), w=[nl])
                for t in range(1, NT):
                    k.op(DVE, lambda e: e.tensor_tensor(out=nl[:, t, :], in0=nl[:, t - 1, :], in1=offs[:, t - 1, :], op=ALU.add),
                         r=[nl, offs], w=[nl])
                k.op(DVE, lambda e: e.tensor_tensor(out=ncum[:], in0=ncum[:], in1=nl[:], op=ALU.add), r=[ncum, nl], w=[ncum])
                k.op(POOL, lambda e: e.memset(ncum8[:, 0:64], 0.0), w=[ncum8])
                k.op(DVE, lambda e: e.tensor_scalar(out=ncum8[:, 64:64 + NT * 8], in0=ncum[:].rearrange("p t h -> p (t h)"),
                                                    scalar1=-8.0, scalar2=None, op0=ALU.mult), r=[ncum], w=[ncum8])
                k.barrier()
            QP, KP = QB2, ropeC
            VPv = ropeS[:].rearrange("p (t c) -> p t c", c=128)
            for pr in range(4):
                qk_proj(WS[0], pr * 128, 128, hT, hbufs, S, QP, QP, P_FQ, 64, False)
                qk_proj(WS[1], pr * 128, 128, hT, hbufs, S, KP, KP, P_FK, 64, False)
                for hh in range(2):
                    h = 2 * pr + hh
                    vlo, olo = (0, 64) if hh == 0 else (64, 0)
                    k.dma(SP, QB[0:64, :], QP[64 * hh:64 * hh + 64, :], r=[QP], w=[QB])
                    k.dma(SP, KB[0:64, :], KP[64 * hh:64 * hh + 64, :], r=[KP], w=[KB])
                    k.op(POOL, lambda e: e.memset(KB[64:65, :], 1.0), w=[KB])
                    for t0 in range(0, NT, 4):
                        bk = banks[6 + (t0 // 4) % 2]

                        def f(e):
                            for tt in range(4):
                                c = (t0 + tt) * 8 + h
                                i = e.matmul(out=bk[0:65, tt * 128:(tt + 1) * 128], lhsT=ncum8[:, c:c + 65], rhs=ident_b,
                                             start=True, stop=True)
                            return i
                        k.op(PE, f, r=[ncum8, cb], w=[bk])
                        k.op(DVE, lambda e: e.tensor_copy(out=QB[64:65, t0 * 128:(t0 + 4) * 128], in_=bk[64:65, 0:512]), r=[bk], w=[QB])
                    if hh == 0:
                        v_proj(WS[2], pr * 128, 128, hT, hbufs, NT, VPv, ropeS, None)
                    k.op(DVE, lambda e: e.tensor_copy(out=VB[:, :, vlo:vlo + 64], in_=VPv[:, :, hh * 64:(hh + 1) * 64]), r=[ropeS], w=[VB])
                    k.op(POOL, lambda e: e.memset(VB[:, :, olo:olo + 64], 1.0), w=[VB])
                    for qb in range(NB):
                        accb = banks[3 + (qb % 2)]
                        attn_fm(qb, 65, 0, KB, KB, QB, QB, lambda j: VB[:, j, 0:128], VB, NT, True, 0.125,
                                lambda j: ncum[:, j, h:h + 1], [ncum], accb, None)
                        flush_pv(0)
                        dsb = dacc[qb % 2]
                        dsh = dacc[2 + qb % 2]
                        nsb = tmpf[qb % 2]
                        k.op(DVE, lambda e: e.tensor_copy(out=dsb[olo:olo + 64, :], in_=accb[olo:olo + 64, :]), r=[accb], w=[dsb])
                        k.op(DVE, lambda e: e.tensor_copy(out=nsb[vlo:vlo + 64, :], in_=accb[vlo:vlo + 64, :]), r=[accb], w=[nsb])
                        k.dma(SP, dsh[vlo:vlo + 64, :], dsb[olo:olo + 64, :], r=[dsb], w=[dsh])
                        k.op(DVE, lambda e: e.reciprocal(out=dsh[vlo:vlo + 64, :], in_=dsh[vlo:vlo + 64, :]), r=[dsh], w=[dsh])
                        yt_ = ytb[uc % 2]
                        uc += 1
                        k.op(DVE, lambda e: e.tensor_tensor(out=yt_[vlo:vlo + 64, :], in0=nsb[vlo:vlo + 64, :],
                                                            in1=dsh[vlo:vlo + 64, :], op=ALU.mult), r=[nsb, dsh], w=[yt_])
                        k.dma(SP, yT_d[seq, 4 + pr, vlo:vlo + 64, qb * 512:(qb + 1) * 512], yt_[vlo:vlo + 64, :], r=[yt_], cw=[B_yT])

            load_w(WS[0], w_in_d[:, OFF_QC:OFF_QC + 512], KC, 512)
            load_w(WS[1], w_kv_d[:, 0:512], KC, 512)
            load_w(WS[2], w_kv_d[:, 512:1024], KC, 512)
            mbufs = [memT.sub(0)]
            for h in range(4):
                qk_proj(WS[1], h * 128, 128, memT, mbufs, MEM_LEN, memK[:, h, :], memK.sub(h), P_MK, 128, False)
            for h in range(4):
                v_proj(WS[2], h * 128, 128, memT, mbufs, MEM_LEN // 128, memV[:, :, h, :], memV.sub(h), None)
            for h in range(4):
                qk_proj(WS[0], h * 128, 128, hT, hbufs, S, QB, QB, P_MQ, 128, False)
                for qb in range(NB):
                    numb, denb = banks[3 + 2 * (qb % 2)], banks[4 + 2 * (qb % 2)]
                    attn_fm(qb, 128, 0, memK[:, h, :], memK.sub(h), QB, QB, lambda j: memV[:, j, h, 0:128], memV.sub(h),
                            MEM_LEN // 128, False, 128.0 ** -0.5, None, [], numb, denb)
                    flush_pv(0)
                    rd = tmpf[qb % 2]
                    yt_ = ytb[uc % 2]
                    uc += 1
                    k.op(DVE, lambda e: e.reciprocal(out=rd[:], in_=denb[:]), r=[denb], w=[rd])
                    k.op(DVE, lambda e: e.tensor_tensor(out=yt_[:], in0=numb[:], in1=rd[:], op=ALU.mult), r=[numb, rd], w=[yt_])
                    store_yT(yt_, 8 + h, qb)

            gctr = 0
            for grp in range(6):
                ws = WS[grp % 3]
                load_w(ws, w_in_d[:, OFF_G + grp * 512:OFF_G + (grp + 1) * 512], KC, 512)
                for c4 in range(4):
                    for tb in range(NB):
                        i = gctr % 2
                        gctr += 1
                        bk = banks[6 + i]
                        proj_fm(hT, hbufs[tb], ws, c4 * 128, 128, tb * 512, 512, bk)
                        k.op(ACT, lambda e: e.activation(out=qn[i][:], in_=bk[:], func=AF.Sigmoid), r=[bk], w=[qn[i]])
                        k.dma(SP, g_d[seq, grp * 4 + c4, :, tb * 512:(tb + 1) * 512], qn[i][:], r=[qn[i]], cw=[B_g])
            if dbg and "dbg_yT" in dbg_d and seq == 0:
                pass
            k.barrier()

        phase_merge(k, nc, seq, S, NT, NB, NSEQ, CAP, banks, cf, cb, rowb, wr, rt_d1, rt_d2, rt_w, cnt,
                    x_d, out_d, yT_d, g_d, xs_d, w_o_d, w_out_d, B_yT, B_g, B_xs, B_out, ident_f, load_w)

    phase_experts(k, nc, CAP, EB, banks, cb, ident_b, xs_d, ys_d, w_up_d, w_dn_d, B_xs, B_ys, load_w)
    phase_combine(k, nc, S, NT, NSEQ, CAP, rt_d1, rt_d2, rt_w, out_d, ys_d, B_ys, B_out)
    k.barrier([SP])
    k.es.close()
    return nc


def phase_merge(k, nc, seq, S, NT, NB, NSEQ, CAP, banks, cf, cb, rowb, wr, rt_d1, rt_d2, rt_w, cnt,
                x_d, out_d, yT_d, g_d, xs_d, w_o_d, w_out_d, B_yT, B_g, B_xs, B_out, ident_f, load_w):
    PE, ACT, DVE, POOL, SP = k.PE, k.ACT, k.DVE, k.POOL, k.SP
    NSLOT = N_EXP * CAP
    with ExitStack() as sc:
        Wo = [k.sb(sc, f"Wo{b}", [128, 4, D], BF16) for b in range(3)]
        Wout = k.sb(sc, "Wout", [128, KC, D], BF16)
        for b in range(3):
            load_w(Wo[b], w_o_d[b][:, :], 4, D)
        load_w(Wout, w_out_d[:, :], KC, D)
        ytb = [k.sb(sc, f"mytb{i}", [128, 12, 512], BF16) for i in range(2)]
        gts = [k.sb(sc, f"gts{i}", [128, 3, 512], BF16) for i in range(2)]
        mt = [k.sb(sc, f"mt{i}", [128, 512], F32) for i in range(3)]
        mT = [k.sb(sc, f"mT{i}", [128, KC, 512], BF16) for i in range(2)]
        xt = [k.sb(sc, f"mxt{i}", [128, D], F32) for i in range(4)]
        x1 = [k.sb(sc, f"x1{i}", [128, D], F32) for i in range(4)]
        h2f = [k.sb(sc, f"h2f{i}", [128, D], F32) for i in range(4)]
        h2b = [k.sb(sc, f"h2b{i}", [128, D], BF16) for i in range(10)]
        h2T = k.sb(sc, "h2T", [128, KC, 128], F32)
        junk = k.sb(sc, "mjunk", [128, D], BF16)
        st = [k.sb(sc, f"mst{i}", [128, 8], F32) for i in range(4)]
        lgs = [k.sb(sc, f"lg{i}", [128, 36], F32) for i in range(2)]
        sm = k.sb(sc, "sm", [128, 16], F32)
        m8 = k.sb(sc, "m8", [128, 8], F32)
        esel = k.sb(sc, "esel", [128, 8], F32)
        hot = k.sb(sc, "hot", [128, 2, 8], F32)
        e48 = k.sb(sc, "e48", [128, 4, 8], F32)
        M12 = k.sb(sc, "M12", [128, 2, 32], F32)
        Msum = k.sb(sc, "Msum", [128, 32], BF16)
        posb = k.sb(sc, "posb", [128, 32], F32)
        dsc = k.sb(sc, "dsc", [128, 2, 32], F32)
        dfl = k.sb(sc, "dfl", [128, 2], F32)
        lg4 = k.sb(sc, "lg4", [128, 4, 36], F32)
        sm4 = k.sb(sc, "sm4", [128, 5, 4], F32)
        gh4 = k.sb(sc, "gh4", [128, 4, 4], F32)
        ge4 = k.sb(sc, "ge4", [128, 4, 4], F32)
        e448 = k.sb(sc, "e448", [128, 4, 4, 8], F32)
        esel4 = k.sb(sc, "esel4", [128, 4, 8], F32)
        m84 = k.sb(sc, "m84", [128, 4, 8], F32)
        hot4 = k.sb(sc, "hot4", [128, 2, 4, 8], F32)
        M124 = k.sb(sc, "M124", [128, 2, 4, 32], F32)
        Msum4 = k.sb(sc, "Msum4", [128, 4, 32], BF16)
        pos4 = k.sb(sc, "pos4", [128, 4, 32], F32)
        dsc4 = k.sb(sc, "dsc4", [128, 2, 4, 32], F32)
        dfl4 = k.sb(sc, "dfl4", [128, 2, 4], F32)
        hb4 = [None] * 4
        zc = 0
        pend_back = []
        pendB = []
        for tb in range(NB):
            yb = ytb[tb % 2]
            k.dma(SP, yb[:], yT_d[seq, :, :, tb * 512:(tb + 1) * 512].rearrange("c p t -> p c t"), r=[B_yT], w=[yb])
            mTb = mT[tb % 2]
            for t in range(4):
                k.dma(SP, xt[t][:], x_d[seq, tb * 512 + t * 128:tb * 512 + (t + 1) * 128, :], w=[xt[t]])
            for j in range(KC):
                gt_ = gts[j % 2]
                k.dma(SP, gt_[:], g_d[seq].rearrange("(b j) p t -> p b j t", b=3)[:, :, j, tb * 512:(tb + 1) * 512], r=[B_g], w=[gt_])
                zb = [banks[3 * (zc % 2) + b] for b in range(3)]
                zc += 1
                for b in range(3):
                    def f(e, b=b):
                        for c in range(4):
                            i = e.matmul(out=zb[b][:], lhsT=Wo[b][:, c, j * 128:(j + 1) * 128], rhs=yb[:, b * 4 + c, :],
                                         start=(c == 0), stop=(c == 3))
                        return i
                    k.op(PE, f, r=[Wo[b], yb], w=[zb[b]])
                    k.op(DVE, lambda e: e.tensor_tensor(out=mt[b][:], in0=zb[b][:], in1=gt_[:, b, :], op=ALU.mult),
                         r=[zb[b], gt_], w=[mt[b]])
                k.op(DVE, lambda e: e.tensor_tensor(out=mt[0][:], in0=mt[0][:], in1=mt[1][:], op=ALU.add), r=[mt[0], mt[1]], w=[mt[0]])
                k.op(DVE, lambda e: e.tensor_tensor(out=mTb[:, j, :], in0=mt[0][:], in1=mt[2][:], op=ALU.add),
                     r=[mt[0], mt[2]], w=[mTb])
            if pend_back:
                pend_back.pop(0)()
            for t in range(4):
                gt = seq * NT + tb * 4 + t
                tok0 = tb * 512 + t * 128
                xt_, x1_, hf, hb, s_ = xt[t], x1[t], h2f[t], h2b[(tb * 4 + t) % 10], st[t]
                for half in range(2):
                    bk = banks[6 + half]

                    def f(e, half=half):
                        for c in range(KC):
                            i = e.matmul(out=bk[:], lhsT=mTb[:, c, t * 128:(t + 1) * 128], rhs=Wout[:, c, half * 512:(half + 1) * 512],
                                         start=(c == 0), stop=(c == KC - 1))
                        return i
                    k.op(PE, f, r=[mTb, Wout], w=[bk])
                    k.op(DVE, lambda e: e.tensor_tensor(out=x1_[:, half * 512:(half + 1) * 512], in0=bk[:],
                                                        in1=xt_[:, half * 512:(half + 1) * 512], op=ALU.add), r=[bk, xt_], w=[x1_])
                k.dma(SP, out_d[seq, tok0:tok0 + 128, :], x1_[:], r=[x1_], cw=[B_out])
                k.op(ACT, lambda e: e.activation(out=junk[:], in_=x1_[:], func=AF.Square, accum_out=s_[:, 0:1]), r=[x1_], w=[junk, s_])
                k.op(ACT, lambda e: e.activation(out=s_[:, 1:2], in_=s_[:, 0:1], func=AF.Ln, scale=1.0 / D, bias=EPS), r=[s_], w=[s_])
                k.op(ACT, lambda e: e.activation(out=s_[:, 1:2], in_=s_[:, 1:2], func=AF.Exp, scale=-0.5), r=[s_], w=[s_])
            for t in range(4):
                x1_, hf, hb, s_ = x1[t], h2f[t], h2b[(tb * 4 + t) % 10], st[t]
                k.op(DVE, lambda e: e.scalar_tensor_tensor(out=hf[:], in0=x1_[:], scalar=s_[:, 1:2], in1=rowb[:, R_FG:R_FG + D],
                                                           op0=ALU.mult, op1=ALU.mult), r=[x1_, s_, rowb], w=[hf])
                k.op(ACT, lambda e: e.activation(out=hb[:], in_=hf[:], func=AF.Copy), r=[hf], w=[hb])
            for t in range(4):
                gt = seq * NT + tb * 4 + t
                hf, hb, s_ = h2f[t], h2b[(tb * 4 + t) % 10], st[t]
                lg = lgs[t % 2]

                def stageB(gt=gt, hb=hb, s_=s_, lg=lg, hf=hf):
                    for hh in range(2):
                        bk = banks[hh]

                        def tr(e, hh=hh):
                            for c in range(4):
                                cc = hh * 4 + c
                                i = e.transpose(out=bk[:, c * 128:(c + 1) * 128], in_=hf[:, cc * 128:(cc + 1) * 128], identity=ident_f)
                            return i
                        k.op(PE, tr, r=[hf, cf], w=[bk])
                        k.op(ACT, lambda e: e.activation(out=h2T[:, hh * 4:(hh + 1) * 4, :], in_=bk[:].rearrange("p (c t) -> p c t", c=4),
                                                         func=AF.Copy), r=[bk], w=[h2T])
                    bkl = banks[2]

                    def fl(e):
                        for c in range(KC):
                            i = e.matmul(out=bkl[:, 0:36], lhsT=h2T[:, c, :], rhs=wr[:, c, :], start=(c == 0), stop=(c == KC - 1))
                        return i
                    k.op(PE, fl, r=[h2T, wr], w=[bkl])
                    k.op(DVE, lambda e: e.tensor_tensor(out=lg[:], in0=bkl[:, 0:36], in1=rowb[:, R_BR:R_BR + 36], op=ALU.add),
                         r=[bkl, rowb], w=[lg])
                    k.op(DVE, lambda e: e.tensor_copy(out=lg4[:, gt % 4, :], in_=lg[:]), r=[lg], w=[lg4])
                    hb4[gt % 4] = hb
                    if gt % 4 != 3:
                        return
                    g0 = gt - 3
                    hbs = list(hb4)

                    def back(g0=g0, hbs=hbs):
                        GL = lg4[:, :, 0:4]
                        k.op(DVE, lambda e: e.reduce_max(out=sm4[:, 0, :], in_=GL, axis=AX.X), r=[lg4], w=[sm4])
                        k.op(DVE, lambda e: e.tensor_tensor(out=gh4[:], in0=GL, in1=sm4[:, 0, :].unsqueeze(2).to_broadcast([128, 4, 4]),
                                                            op=ALU.subtract), r=[lg4, sm4], w=[gh4])
                        k.op(ACT, lambda e: e.activation(out=ge4[:], in_=gh4[:], func=AF.Exp), r=[gh4], w=[ge4])
                        k.op(DVE, lambda e: e.reduce_sum(out=sm4[:, 1, :], in_=ge4[:], axis=AX.X), r=[ge4], w=[sm4])
                        k.op(DVE, lambda e: e.tensor_scalar(out=gh4[:], in0=gh4[:], scalar1=0.0, scalar2=None, op0=ALU.is_equal),
                             r=[gh4, ge4], w=[gh4])
                        k.op(DVE, lambda e: e.tensor_tensor(out=e448[:], in0=lg4[:, :, 4:36].rearrange("p t (g j) -> p t g j", g=4),
                                                            in1=gh4[:].unsqueeze(3).to_broadcast([128, 4, 4, 8]), op=ALU.mult),
                             r=[lg4, gh4], w=[e448])
                        k.op(DVE, lambda e: e.reduce_sum(out=esel4[:], in_=e448[:].rearrange("p t g j -> p t j g"), axis=AX.X),
                             r=[e448], w=[esel4])
                        for t_ in range(4):
                            k.op(DVE, lambda e: e.max(out=m84[:, t_, :], in_=esel4[:, t_, :]), r=[esel4], w=[m84])
                        for q in range(2):
                            k.op(DVE, lambda e: e.tensor_tensor(out=hot4[:, q, :, :], in0=esel4[:],
                                                                in1=m84[:, :, q:q + 1].to_broadcast([128, 4, 8]), op=ALU.is_equal),
                                 r=[esel4, m84], w=[hot4])
                        k.op(DVE, lambda e: e.tensor_tensor(out=sm4[:, 2, :], in0=m84[:, :, 1], in1=m84[:, :, 0], op=ALU.subtract),
                             r=[m84, sm4], w=[sm4])
                        k.op(ACT, lambda e: e.activation(out=sm4[:, 3, :], in_=sm4[:, 2, :], func=AF.Exp), r=[sm4], w=[sm4])
                        k.op(DVE, lambda e: e.scalar_tensor_tensor(out=sm4[:, 4, :], in0=sm4[:, 3, :], scalar=1.0, in1=sm4[:, 1, :],
                                                                   op0=ALU.add, op1=ALU.mult), r=[sm4], w=[sm4])
                        k.op(DVE, lambda e: e.reciprocal(out=rt_w[:, g0:g0 + 4, 0], in_=sm4[:, 4, :]), r=[sm4], w=[rt_w.sub(g0)])
                        k.op(DVE, lambda e: e.tensor_tensor(out=rt_w[:, g0:g0 + 4, 1], in0=rt_w[:, g0:g0 + 4, 0], in1=sm4[:, 3, :], op=ALU.mult),
                             r=[sm4, rt_w.sub(g0)], w=[rt_w.sub(g0)])
                        for q in range(2):
                            k.op(DVE, lambda e: e.tensor_tensor(out=M124[:, q, :, :].rearrange("p t (g j) -> p t g j", g=4),
                                                                in0=gh4[:].unsqueeze(3).to_broadcast([128, 4, 4, 8]),
                                                                in1=hot4[:, q, :, :].unsqueeze(2).to_broadcast([128, 4, 4, 8]), op=ALU.mult),
                                 r=[gh4, hot4], w=[M124])
                        k.op(DVE, lambda e: e.tensor_tensor(out=Msum4[:], in0=M124[:, 0, :, :], in1=M124[:, 1, :, :], op=ALU.add),
                             r=[M124], w=[Msum4])
                        bkp = banks[3]
                        MS = Msum4[:].rearrange("p t e -> p (t e)")

                        def fp(e):
                            e.matmul(out=bkp[:, 0:128], lhsT=cb[:, C_TLT:C_TLT + 128], rhs=MS, start=True, stop=True)
                            return e.matmul(out=bkp[:, 128:256], lhsT=cb[:, C_ONE:C_ONE + 128], rhs=MS, start=False, stop=True,
                                            skip_group_check=True)
                        k.op(PE, fp, r=[Msum4, cb], w=[bkp])
                        for t_ in range(4):
                            k.op(DVE, lambda e: e.tensor_tensor(out=pos4[:, t_, :], in0=bkp[:, t_ * 32:(t_ + 1) * 32], in1=cnt[:], op=ALU.add),
                                 r=[bkp, cnt], w=[pos4])
                            k.op(DVE, lambda e: e.tensor_tensor(out=cnt[:], in0=bkp[:, 128 + t_ * 32:128 + (t_ + 1) * 32], in1=cnt[:], op=ALU.add),
                                 r=[bkp, cnt], w=[cnt])
                        k.op(DVE, lambda e: e.tensor_scalar(out=pos4[:], in0=pos4[:], scalar1=float(CAP - 1), scalar2=None, op0=ALU.min),
                             r=[pos4], w=[pos4])
                        k.op(DVE, lambda e: e.tensor_tensor(out=pos4[:], in0=pos4[:], in1=cf[:, C_EB:C_EB + 32].unsqueeze(1).to_broadcast([128, 4, 32]),
                                                            op=ALU.add), r=[pos4, cf], w=[pos4])
                        for q in range(2):
                            k.op(DVE, lambda e: e.tensor_tensor(out=dsc4[:, q, :, :], in0=M124[:, q, :, :], in1=pos4[:], op=ALU.mult),
                                 r=[M124, pos4], w=[dsc4])
                        k.op(DVE, lambda e: e.reduce_sum(out=dfl4[:], in_=dsc4[:], axis=AX.X), r=[dsc4], w=[dfl4])
                        k.op(DVE, lambda e: e.tensor_copy(out=rt_d1[:, g0:g0 + 4], in_=dfl4[:, 0, :]), r=[dfl4], w=[rt_d1.sub(g0)])
                        k.op(DVE, lambda e: e.tensor_copy(out=rt_d2[:, g0:g0 + 4], in_=dfl4[:, 1, :]), r=[dfl4], w=[rt_d2.sub(g0)])
                        for t_ in range(4):
                            g_ = g0 + t_
                            k.dma(POOL, xs_d[:, :], hbs[t_][:], r=[hbs[t_], rt_d1.sub(g0)], cw=[B_xs], scatter_idx=rt_d1[:, g_:g_ + 1],
                                  bounds=NSLOT - 1)
                            k.dma(POOL, xs_d[:, :], hbs[t_][:], r=[hbs[t_], rt_d2.sub(g0)], cw=[B_xs], scatter_idx=rt_d2[:, g_:g_ + 1],
                                  bounds=NSLOT - 1)
                    pend_back.append(back)
                stageB()
        while pendB:
            pendB.pop(0)()
        while pend_back:
            pend_back.pop(0)()
        k.barrier()


def phase_experts(k, nc, CAP, EB, banks, cb, ident_b, xs_d, ys_d, w_up_d, w_dn_d, B_xs, B_ys, load_w):
    PE, ACT, DVE, POOL, SP = k.PE, k.ACT, k.DVE, k.POOL, k.SP
    NTB = EB // 128
    with ExitStack() as sc:
        wup = [k.sb(sc, f"wup{i}", [128, KC, 2 * FF], BF16) for i in range(3)]
        wdn = [k.sb(sc, f"wdn{i}", [128, 4, D], BF16) for i in range(3)]
        xr = [k.sb(sc, f"xr{i}", [128, NTB, D], BF16) for i in range(2)]
        xsT = [k.sb(sc, f"xsT{i}", [128, KC, EB], BF16) for i in range(2)]
        actT = [k.sb(sc, f"actT{i}", [128, 4, EB], BF16) for i in range(2)]
        sil = [k.sb(sc, f"sil{i}", [128, EB], F32) for i in range(2)]
        yt = [k.sb(sc, f"eyt{i}", [128, D], BF16) for i in range(2)]
        it = 0
        cc = 0
        blocks = [(ex, blk) for ex in range(N_EXP) for blk in range(CAP // EB)]

        def load_rows(i):
            ex_, blk_ = blocks[i]
            s0_ = ex_ * CAP + blk_ * EB
            k.dma(SP, xr[i % 2][:], xs_d[s0_:s0_ + EB, :].rearrange("(t p) d -> p t d", p=128), r=[B_xs], w=[xr[i % 2]])
        load_rows(0)
        for ex in range(N_EXP):
            wu, wd = wup[ex % 3], wdn[ex % 3]
            if ex == 0:
                for e0 in range(2):
                    load_w(wup[e0], w_up_d[e0], KC, 2 * FF)
                    load_w(wdn[e0], w_dn_d[e0], 4, D)
            if ex + 2 < N_EXP:
                load_w(wup[(ex + 2) % 3], w_up_d[ex + 2], KC, 2 * FF)
                load_w(wdn[(ex + 2) % 3], w_dn_d[ex + 2], 4, D)
            for blk in range(CAP // EB):
                s0 = ex * CAP + blk * EB
                xr_, xT, aT = xr[it % 2], xsT[it % 2], actT[it % 2]
                it += 1
                for t in range(NTB):
                    bk = banks[6 + t % 2]
                    bkb = bk.h.bitcast(BF16)

                    def tr(e, t=t):
                        for c in range(KC):
                            i = e.transpose(out=bkb[:, c * 128:(c + 1) * 128], in_=xr_[:, t, c * 128:(c + 1) * 128], identity=ident_b)
                        return i
                    k.op(PE, tr, r=[xr_, cb], w=[bk])
                    k.op(DVE, lambda e: e.tensor_copy(out=xT[:, :, t * 128:(t + 1) * 128],
                                                      in_=bkb[:, 0:1024].rearrange("p (c t) -> p c t", c=KC)), r=[bk], w=[xT])
                if it < len(blocks):
                    load_rows(it)
                for fc in range(4):
                    ba, bb = banks[(cc % 2) * 2], banks[(cc % 2) * 2 + 1]
                    cc += 1
                    for (bk, col0) in ((ba, fc * 128), (bb, FF + fc * 128)):
                        def f(e, bk=bk, col0=col0):
                            for c in range(KC):
                                i = e.matmul(out=bk[:, 0:EB], lhsT=wu[:, c, col0:col0 + 128], rhs=xT[:, c, :], start=(c == 0), stop=(c == KC - 1))
                            return i
                        k.op(PE, f, r=[wu, xT], w=[bk])
                    sl = sil[fc % 2]
                    k.op(ACT, lambda e: e.activation(out=sl[:], in_=ba[:, 0:EB], func=AF.Silu), r=[ba], w=[sl])
                    k.op(DVE, lambda e: e.tensor_tensor(out=aT[:, fc, :], in0=bb[:, 0:EB], in1=sl[:], op=ALU.mult), r=[bb, sl], w=[aT])
                for t in range(NTB):
                    y_ = yt[t % 2]
                    for half in range(2):
                        bk = banks[4 + half]

                        def f(e, half=half, t=t):
                            for c in range(4):
                                i = e.matmul(out=bk[:], lhsT=aT[:, c, t * 128:(t + 1) * 128], rhs=wd[:, c, half * 512:(half + 1) * 512],
                                             start=(c == 0), stop=(c == 3))
                            return i
                        k.op(PE, f, r=[aT, wd], w=[bk])
                        if half == 0:
                            k.op(ACT, lambda e: e.activation(out=y_[:, 0:512], in_=bk[:], func=AF.Copy), r=[bk], w=[y_])
                        else:
                            k.op(DVE, lambda e: e.tensor_copy(out=y_[:, 512:1024], in_=bk[:]), r=[bk], w=[y_])
                    k.dma(SP, ys_d[s0 + t * 128:s0 + (t + 1) * 128, :], y_[:], r=[y_], cw=[B_ys])
        k.barrier()


def phase_combine(k, nc, S, NT, NSEQ, CAP, rt_d1, rt_d2, rt_w, out_d, ys_d, B_ys, B_out):
    PE, ACT, DVE, POOL, SP = k.PE, k.ACT, k.DVE, k.POOL, k.SP
    NSLOT = N_EXP * CAP
    B_fin = Buf()
    with ExitStack() as sc:
        xt = [k.sb(sc, f"cxt{i}", [128, D], F32) for i in range(4)]
        y1 = [k.sb(sc, f"cy1{i}", [128, D], BF16) for i in range(4)]
        y2 = [k.sb(sc, f"cy2{i}", [128, D], BF16) for i in range(4)]
        o1 = [k.sb(sc, f"co1{i}", [128, D], F32) for i in range(4)]
        NTT_ = NSEQ * NT

        def fetch(g):
            sq_, t_ = divmod(g, NT)
            ii = g % 4
            k.dma(SP, xt[ii][:], out_d[sq_, t_ * 128:(t_ + 1) * 128, :], r=[B_out], w=[xt[ii]])
            k.dma(POOL, y1[ii][:], ys_d[:, :], r=[B_ys], w=[y1[ii]], gather_idx=rt_d1[:, g:g + 1], bounds=NSLOT - 1)
            k.dma(POOL, y2[ii][:], ys_d[:, :], r=[B_ys], w=[y2[ii]], gather_idx=rt_d2[:, g:g + 1], bounds=NSLOT - 1)
        for g in range(min(3, NTT_)):
            fetch(g)
        for gt in range(NTT_):
            seq, t = divmod(gt, NT)
            i = gt % 4
            if gt + 3 < NTT_:
                fetch(gt + 3)
            k.op(DVE, lambda e: e.scalar_tensor_tensor(out=o1[i][:], in0=y1[i][:], scalar=rt_w[:, gt, 0:1], in1=xt[i][:],
                                                       op0=ALU.mult, op1=ALU.add), r=[y1[i], xt[i], rt_w.sub(gt)], w=[o1[i]])
            k.op(DVE, lambda e: e.scalar_tensor_tensor(out=o1[i][:], in0=y2[i][:], scalar=rt_w[:, gt, 1:2], in1=o1[i][:],
                                                       op0=ALU.mult, op1=ALU.add), r=[y2[i], o1[i], rt_w.sub(gt)], w=[o1[i]])
            k.dma(SP, out_d[seq, t * 128:(t + 1) * 128, :], o1[i][:], r=[o1[i]], cw=[B_fin])
        k.barrier()


def make_consts(CAP):
    c = np.zeros((128, NCONST), np.float32)
    p = np.arange(128)[:, None]
    q = np.arange(128)[None, :]
    c[:, C_ID:C_ID + 128] = (p == q)
    c[:, C_MASK:C_MASK + 128] = np.where(p > q, -30000.0, 0.0)
    c[:, C_TLE:C_TLE + 128] = (p <= q)
    c[:, C_TLT:C_TLT + 128] = (p < q)
    c[:, C_B64:C_B64 + 128] = (p // 64 == q // 64)
    rope = np.zeros((128, 128), np.float32)
    for base in (0, 64):
        for d in range(8):
            rope[base + d + 8, base + d] = -1.0
            rope[base + d, base + d + 8] = 1.0
    c[:, C_ROPE:C_ROPE + 128] = rope
    dd = np.arange(128) % 64
    inv = np.where(dd < 16, ROPE_THETA ** (-(2.0 * (dd % 8)) / 16.0), 0.0)
    c[:, C_INVF] = inv.astype(np.float32)
    c[:, C_ONE:C_ONE + 128] = 1.0
    c[:, C_EB:C_EB + 32] = (np.arange(32) * CAP)[None, :]
    return c


def make_inmaps(inputs, n_cores, NSEQ, CAP):
    f = lambda a: np.ascontiguousarray(np.asarray(a, dtype=np.float32))
    row = np.concatenate([f(inputs["attn_norm_g"])[0], f(inputs["mem_norm_g"])[0], f(inputs["ffn_norm_g"])[0],
                          f(inputs["b_router_group"])[0], f(inputs["b_router_expert"])[0], f(inputs["diff_subln_g"])[0],
                          f(inputs["fox_forget_b"])[0], f(inputs["diff_lambda"])[0].ravel()])
    assert row.shape[0] == NROWB
    rowb = np.ascontiguousarray(np.broadcast_to(row[None, :], (128, NROWB)))
    i64 = np.arange(128) % 64
    colp = np.stack([f(inputs["diff_qnorm_g"])[0][i64], f(inputs["diff_knorm_g"])[0][i64], f(inputs["fox_qnorm_g"])[0][i64],
                     f(inputs["fox_knorm_g"])[0][i64], f(inputs["mem_qnorm_g"])[0], f(inputs["mem_knorm_g"])[0],
                     f(inputs["diff_subln_g"])[0]], axis=1)
    colp = np.ascontiguousarray(colp.astype(np.float32))
    shared = {
        "consts": make_consts(CAP), "rowb": rowb, "colp": colp,
        "w_in": f(inputs["w_in"])[0], "w_mem_kv": f(inputs["w_mem_kv"])[0],
        "w_o_diff": f(inputs["w_o_diff"])[0], "w_o_fox": f(inputs["w_o_fox"])[0], "w_o_mem": f(inputs["w_o_mem"])[0],
        "w_out": f(inputs["w_out"])[0],
        "w_router": np.ascontiguousarray(np.concatenate([f(inputs["w_router_group"])[0], f(inputs["w_router_expert"])[0]], axis=1)),
        "w_up": f(inputs["w_up"])[0], "w_down": f(inputs["w_down"])[0],
    }
    x = f(inputs["x"])
    mem = f(inputs["mem"])
    pos = np.ascontiguousarray(np.asarray(inputs["positions"], dtype=np.int32))
    maps = []
    for c in range(n_cores):
        m = dict(shared)
        m["x"] = np.ascontiguousarray(x[c * NSEQ:(c + 1) * NSEQ])
        m["mem"] = np.ascontiguousarray(mem[c * NSEQ:(c + 1) * NSEQ])
        m["pos"] = np.ascontiguousarray(pos[c * NSEQ:(c + 1) * NSEQ])
        maps.append(m)
    return maps


def kernel(**inputs):
    B, S, _ = np.asarray(inputs["x"]).shape
    n_cores = 8
    NSEQ = B // n_cores
    CAP = 768
    nc = build_program(S, NSEQ, CAP)
    maps = make_inmaps(inputs, n_cores, NSEQ, CAP)
    res = run_bass_kernel_spmd(nc, maps, core_ids=list(range(n_cores)))
    return np.concatenate([np.asarray(r["out"]) for r in res.results], axis=0).astype(np.float32)
```
